# Optimizing a Trainium2 kernel written in Bass

```python
import jax
import jax.numpy as jnp
from jax import lax
import numpy as np

D_MODEL = 2048
BATCH = 8
SEQ = 2048
DEPTH = 2

GRID_W = 64
CTX_LEN = 256
HEAD_DIM = 128
ROPE_THETA = 10000.0
NORM_EPS = 1e-6
N_MOD = 6

A_Q_HEADS = 8
A_KV_HEADS = 2
Q_BLOCK = 128
B_HEADS = 8
RET_CHUNK = 128
RET_DECAY_BASE = 5.0
C_HEADS = 16
C_DK = 128
C_DV = D_MODEL // C_HEADS
HGRN_CHUNK = 16

A_WIDTH = A_Q_HEADS * HEAD_DIM
B_WIDTH = B_HEADS * HEAD_DIM
MIX_WIDTH = A_WIDTH + B_WIDTH
EVEN_KV_SIZES = (A_KV_HEADS * HEAD_DIM, A_KV_HEADS * HEAD_DIM, B_WIDTH, B_WIDTH)
EVEN_SIZES = EVEN_KV_SIZES + (A_WIDTH, B_WIDTH, B_WIDTH)
EVEN_IN = 2 * A_KV_HEADS * HEAD_DIM + 2 * B_WIDTH + A_WIDTH + 2 * B_WIDTH
C_KEY_WIDTH = C_HEADS * C_DK
C_VAL_WIDTH = C_HEADS * C_DV
ODD_STATE_SIZES = (C_KEY_WIDTH, C_KEY_WIDTH, C_VAL_WIDTH)
ODD_SIZES = ODD_STATE_SIZES + (C_KEY_WIDTH, C_VAL_WIDTH)
ODD_IN = 3 * C_KEY_WIDTH + 2 * C_VAL_WIDTH

N_EXPERTS = 32
TOP_K = 4
D_EXPERT = D_MODEL
SWIGLU_ALPHA = 1.702
SWIGLU_LIMIT = 7.0
MOE_BLOCK = 256

N_EVEN_LAYERS = (DEPTH + 1) // 2
N_ODD_LAYERS = DEPTH // 2

kernel_name = 'hybrid_dit_attn_retnet_hgrn2_moe'


def rms_norm(x, gain):
    xf = x.astype(jnp.float32)
    y = xf * lax.rsqrt(jnp.mean(xf * xf, axis=-1, keepdims=True) + NORM_EPS)
    return (y * gain.astype(jnp.float32)).astype(x.dtype)


def modulate(h, shift, scale):
    return h * (1 + scale) + shift


def split_cols(t, sizes):
    cuts = [int(v) for v in np.cumsum(sizes)[:-1]]
    return jnp.split(t, cuts, axis=-1)


def to_heads(t, n_heads):
    b, l, _ = t.shape
    return t.reshape(b, l, n_heads, -1).transpose(0, 2, 1, 3)


def from_heads(t):
    b, h, l, d = t.shape
    return t.transpose(0, 2, 1, 3).reshape(b, l, h * d)


def flip_seq(t):
    return jnp.flip(t, axis=2)


def axial_rope_tables(n_tokens):
    n_rows = n_tokens // GRID_W
    row = jnp.repeat(jnp.arange(n_rows, dtype=jnp.float32), GRID_W)
    col = jnp.tile(jnp.arange(GRID_W, dtype=jnp.float32), n_rows)
    n_freq = HEAD_DIM // 4
    inv_freq = ROPE_THETA ** (-jnp.arange(n_freq, dtype=jnp.float32) / n_freq)
    ang = jnp.concatenate([row[:, None] * inv_freq, col[:, None] * inv_freq], axis=-1)
    return jnp.cos(ang), jnp.sin(ang)


def apply_rope(t, cos, sin):
    half = t.shape[-1] // 2
    t1 = t[..., :half].astype(jnp.float32)
    t2 = t[..., half:].astype(jnp.float32)
    return jnp.concatenate([t1 * cos - t2 * sin, t1 * sin + t2 * cos], axis=-1).astype(t.dtype)


def gqa_attend(q, k, v):
    b, hq, lq, d = q.shape
    hkv = k.shape[1]
    qg = q.reshape(b, hkv, hq // hkv, lq, d)
    s = jnp.einsum('bkgqd,bksd->bkgqs', qg, k).astype(jnp.float32) * (d ** -0.5)
    p = jax.nn.softmax(s, axis=-1).astype(v.dtype)
    return jnp.einsum('bkgqs,bksd->bkgqd', p, v).reshape(b, hq, lq, d)


def blocked_attention(q, k, v):
    b, h, l, d = q.shape
    nb = l // Q_BLOCK
    qb = jnp.moveaxis(q.reshape(b, h, nb, Q_BLOCK, d), 2, 0)
    ob = lax.map(lambda blk: gqa_attend(blk, k, v), qb)
    return jnp.moveaxis(ob, 0, 2).reshape(b, h, l, d)


def retention_chunked(q, k, v, log_g, s0):
    b, h, l, dk = q.shape
    dv = v.shape[-1]
    n = l // RET_CHUNK
    idx = jnp.arange(RET_CHUNK, dtype=jnp.float32)
    diff = idx[:, None] - idx[None, :]
    intra = jnp.where(diff >= 0, jnp.exp(log_g[:, None, None] * jnp.maximum(diff, 0.0)), 0.0)
    q_decay = jnp.exp(log_g[:, None] * (idx + 1.0))[None, :, :, None]
    k_decay = jnp.exp(log_g[:, None] * (RET_CHUNK - 1.0 - idx))[None, :, :, None]
    chunk_decay = jnp.exp(log_g * RET_CHUNK)[None, :, None, None]

    def chunks(t):
        return jnp.moveaxis(t.reshape(b, h, n, RET_CHUNK, t.shape[-1]), 2, 0)

    def step(s, inp):
        qc, kc, vc = inp
        scores = jnp.einsum('bhnd,bhmd->bhnm', qc, kc) * intra
        o = jnp.einsum('bhnm,bhmv->bhnv', scores, vc) + jnp.einsum('bhnd,bhdv->bhnv', qc * q_decay, s)
        s = s * chunk_decay + jnp.einsum('bhmd,bhmv->bhdv', kc * k_decay, vc)
        return s, o

    s, o = lax.scan(step, s0, (chunks(q), chunks(k), chunks(v)))
    return jnp.moveaxis(o, 0, 2).reshape(b, h, l, dv), s


def retention_state(k, v, log_g):
    l = k.shape[2]
    w = jnp.exp(log_g[:, None] * (l - 1.0 - jnp.arange(l, dtype=jnp.float32)))
    return jnp.einsum('bhld,bhlv->bhdv', k * w[None, :, :, None], v)


def gla_chunked(q, k, v, log_f, s0):
    b, h, l, dk = q.shape
    dv = v.shape[-1]
    n = l // HGRN_CHUNK
    causal = jnp.tril(jnp.ones((HGRN_CHUNK, HGRN_CHUNK), dtype=bool))

    def chunks(t):
        return jnp.moveaxis(t.reshape(b, h, n, HGRN_CHUNK, t.shape[-1]), 2, 0)

    def step(s, inp):
        qc, kc, vc, gc = inp
        cum = jnp.cumsum(gc, axis=2)
        diff = cum[:, :, :, None, :] - cum[:, :, None, :, :]
        decay = jnp.exp(jnp.where(causal[:, :, None], diff, -jnp.inf))
        a = jnp.einsum('bhnd,bhmd,bhnmd->bhnm', qc, kc, decay)
        o = jnp.einsum('bhnm,bhmv->bhnv', a, vc) + jnp.einsum('bhnd,bhdv->bhnv', qc * jnp.exp(cum), s)
        last = cum[:, :, -1:, :]
        s = s * jnp.exp(last)[:, :, 0, :, None] + jnp.einsum('bhmd,bhmv->bhdv', kc * jnp.exp(last - cum), vc)
        return s, o

    s, o = lax.scan(step, s0, (chunks(q), chunks(k), chunks(v), chunks(log_f)))
    return jnp.moveaxis(o, 0, 2).reshape(b, h, l, dv), s


def gla_state(k, v, log_f):
    cum = jnp.cumsum(log_f, axis=2)
    return jnp.einsum('bhld,bhlv->bhdv', k * jnp.exp(cum[:, :, -1:] - cum), v)


def hgrn2_gates(f_raw, lb):
    fr = f_raw.astype(jnp.float32)
    log_f = jnp.logaddexp(jnp.log(lb), jnp.log1p(-lb) + jax.nn.log_sigmoid(fr))
    k = (1.0 - lb) * jax.nn.sigmoid(-fr)
    return k, log_f


def group_norm_gate(o, gate, gain, dtype):
    mu = jnp.mean(o, axis=-1, keepdims=True)
    var = jnp.mean(jnp.square(o - mu), axis=-1, keepdims=True)
    on = from_heads((o - mu) * lax.rsqrt(var + NORM_EPS))
    return (on * gain.astype(jnp.float32) * jax.nn.silu(gate.astype(jnp.float32))).astype(dtype)


def rms_norm_gate(o, gate, gain, dtype):
    on = from_heads(o * lax.rsqrt(jnp.mean(o * o, axis=-1, keepdims=True) + NORM_EPS))
    return (on * gain.astype(jnp.float32) * jax.nn.silu(gate.astype(jnp.float32))).astype(dtype)


def clamped_swiglu(hid):
    x_glu = jnp.minimum(hid[..., ::2], SWIGLU_LIMIT)
    x_lin = jnp.clip(hid[..., 1::2], -SWIGLU_LIMIT, SWIGLU_LIMIT)
    return x_glu * jax.nn.sigmoid(SWIGLU_ALPHA * x_glu) * (x_lin + 1)


def moe_ffn(h, w_router, b_router, w1, b1, w2, b2):
    n_tok, d = h.shape
    logits = (h @ w_router).astype(jnp.float32) + b_router.astype(jnp.float32)
    top_val, top_idx = lax.top_k(logits, TOP_K)
    gates = jax.nn.softmax(top_val, axis=-1)
    n_assign = n_tok * TOP_K
    n_blocks = -(-(n_assign + N_EXPERTS * (MOE_BLOCK - 1)) // MOE_BLOCK)
    n_rows = n_blocks * MOE_BLOCK
    flat_e = top_idx.reshape(-1)
    flat_tok = jnp.arange(n_assign, dtype=jnp.int32) // TOP_K
    order = jnp.argsort(flat_e)
    e_sorted = flat_e[order]
    counts = jnp.bincount(flat_e, length=N_EXPERTS)
    padded = (counts + MOE_BLOCK - 1) // MOE_BLOCK * MOE_BLOCK
    pad_end = jnp.cumsum(padded)
    pad_start = pad_end - padded
    start = jnp.cumsum(counts) - counts
    dest = pad_start[e_sorted] + jnp.arange(n_assign, dtype=jnp.int32) - start[e_sorted]
    row_tok = jnp.full((n_rows,), n_tok, jnp.int32).at[dest].set(flat_tok[order])
    row_gate = jnp.zeros((n_rows,), jnp.float32).at[dest].set(gates.reshape(-1)[order])
    block_start = jnp.arange(n_blocks, dtype=jnp.int32) * MOE_BLOCK
    block_e = jnp.minimum(jnp.searchsorted(pad_end, block_start, side='right'), N_EXPERTS - 1)
    h_pad = jnp.concatenate([h, jnp.zeros((1, d), h.dtype)], axis=0)

    def block_step(acc, blk):
        tok, gate, e = blk
        hid = h_pad[tok] @ w1[e] + b1[e]
        out = clamped_swiglu(hid) @ w2[e] + b2[e]
        return acc.at[tok].add(out.astype(jnp.float32) * gate[:, None]), None

    acc0 = jnp.zeros((n_tok + 1, d), jnp.float32)
    acc, _ = lax.scan(block_step, acc0, (row_tok.reshape(n_blocks, MOE_BLOCK), row_gate.reshape(n_blocks, MOE_BLOCK), block_e))
    return acc[:n_tok].astype(h.dtype)


def mixer_attention_retention(hx, hy, w_in, w_out, q_gain, k_gain, decay_exp, gn_gain, cos, sin, ctx_out):
    bsz = hx.shape[0]
    ak, av, bk, bv, aq, bq, bg = split_cols(hx @ w_in, EVEN_SIZES)
    y_sizes = EVEN_SIZES if ctx_out else EVEN_KV_SIZES
    y_parts = split_cols(hy @ w_in[:, :sum(y_sizes)], y_sizes)
    ak_y, av_y, bk_y, bv_y = y_parts[:4]

    qa = apply_rope(rms_norm(to_heads(aq, A_Q_HEADS), q_gain), cos, sin)
    ka = apply_rope(rms_norm(to_heads(ak, A_KV_HEADS), k_gain), cos, sin)
    ka_y = rms_norm(to_heads(ak_y, A_KV_HEADS), k_gain)
    va = to_heads(av, A_KV_HEADS)
    va_y = to_heads(av_y, A_KV_HEADS)
    k_all = jnp.concatenate([ka_y, ka], axis=2)
    v_all = jnp.concatenate([va_y, va], axis=2)
    att_x = from_heads(blocked_attention(qa, k_all, v_all))

    log_g = jnp.log1p(-jnp.exp2(-decay_exp.astype(jnp.float32)))
    k_scale = HEAD_DIM ** -0.5
    qr = apply_rope(to_heads(bq, B_HEADS), cos, sin).astype(jnp.float32)
    kr = apply_rope(to_heads(bk, B_HEADS), cos, sin).astype(jnp.float32) * k_scale
    vr = to_heads(bv, B_HEADS).astype(jnp.float32)
    kr_y = to_heads(bk_y, B_HEADS).astype(jnp.float32) * k_scale
    vr_y = to_heads(bv_y, B_HEADS).astype(jnp.float32)
    if ctx_out:
        aq_y, bq_y, bg_y = y_parts[4:]
        att_y = from_heads(gqa_attend(rms_norm(to_heads(aq_y, A_Q_HEADS), q_gain), ka_y, va_y))
        qr_y = to_heads(bq_y, B_HEADS).astype(jnp.float32)
        s0 = jnp.zeros((bsz, B_HEADS, HEAD_DIM, HEAD_DIM), jnp.float32)
        oyf, syf = retention_chunked(qr_y, kr_y, vr_y, log_g[0], s0)
        oyb, syb = retention_chunked(flip_seq(qr_y), flip_seq(kr_y), flip_seq(vr_y), log_g[1], s0)
        ret_y = group_norm_gate(oyf + flip_seq(oyb), bg_y, gn_gain, hy.dtype)
        out_y = jnp.concatenate([att_y, ret_y], axis=-1) @ w_out
    else:
        syf = retention_state(kr_y, vr_y, log_g[0])
        syb = retention_state(flip_seq(kr_y), flip_seq(vr_y), log_g[1])
        out_y = None
    oxf, _ = retention_chunked(qr, kr, vr, log_g[0], syf)
    oxb, _ = retention_chunked(flip_seq(qr), flip_seq(kr), flip_seq(vr), log_g[1], syb)
    ret_x = group_norm_gate(oxf + flip_seq(oxb), bg, gn_gain, hx.dtype)
    out_x = jnp.concatenate([att_x, ret_x], axis=-1) @ w_out
    return out_x, out_y


def mixer_hgrn2(hx, hy, w_in, w_out, lb, gn_gain, ctx_out):
    bsz = hx.shape[0]
    ffx, fbx, ix, qx, gx = split_cols(hx @ w_in, ODD_SIZES)
    y_sizes = ODD_SIZES if ctx_out else ODD_STATE_SIZES
    y_parts = split_cols(hy @ w_in[:, :sum(y_sizes)], y_sizes)

    def state_inputs(f_fwd, f_bwd, inp):
        k_f, lf_f = hgrn2_gates(f_fwd, lb)
        k_b, lf_b = hgrn2_gates(f_bwd, lb)
        return (to_heads(k_f, C_HEADS), to_heads(lf_f, C_HEADS), to_heads(k_b, C_HEADS), to_heads(lf_b, C_HEADS), to_heads(inp.astype(jnp.float32), C_HEADS))

    kxf, lxf, kxb, lxb, vx = state_inputs(ffx, fbx, ix)
    kyf, lyf, kyb, lyb, vy = state_inputs(y_parts[0], y_parts[1], y_parts[2])
    if ctx_out:
        s0 = jnp.zeros((bsz, C_HEADS, C_DK, C_DV), jnp.float32)
        qy = to_heads(jax.nn.silu(y_parts[3].astype(jnp.float32)), C_HEADS)
        oyf, syf = gla_chunked(qy, kyf, vy, lyf, s0)
        oyb, syb = gla_chunked(flip_seq(qy), flip_seq(kyb), flip_seq(vy), flip_seq(lyb), s0)
        out_y = rms_norm_gate(oyf + flip_seq(oyb), y_parts[4], gn_gain, hy.dtype) @ w_out
    else:
        syf = gla_state(kyf, vy, lyf)
        syb = gla_state(flip_seq(kyb), flip_seq(vy), flip_seq(lyb))
        out_y = None
    qh = to_heads(jax.nn.silu(qx.astype(jnp.float32)), C_HEADS)
    oxf, _ = gla_chunked(qh, kxf, vx, lxf, syf)
    oxb, _ = gla_chunked(flip_seq(qh), flip_seq(kxb), flip_seq(vx), flip_seq(lxb), syb)
    out_x = rms_norm_gate(oxf + flip_seq(oxb), gx, gn_gain, hx.dtype) @ w_out
    return out_x, out_y


def setup_inputs(seed: int = 0) -> dict:
    key = jax.random.key(seed)
    ks = jax.random.split(key, 25)

    def normal(k, shape, scale=1.0):
        return jax.random.normal(k, shape, jnp.float32) * scale

    def gain(k, shape):
        return 1.0 + normal(k, shape, 0.05)

    return {
        'x': normal(ks[0], (BATCH, SEQ, D_MODEL)),
        'c': normal(ks[1], (BATCH, D_MODEL)),
        'ctx': normal(ks[2], (BATCH, CTX_LEN, D_MODEL)),
        'c_ctx': normal(ks[3], (D_MODEL,)),
        'ada_w': normal(ks[4], (DEPTH, D_MODEL, N_MOD * D_MODEL), 0.5 * D_MODEL ** -0.5),
        'ada_b': normal(ks[5], (DEPTH, N_MOD * D_MODEL), 0.02),
        'norm_mix': gain(ks[6], (DEPTH, D_MODEL)),
        'norm_ffn': gain(ks[7], (DEPTH, D_MODEL)),
        'ab_w_in': normal(ks[8], (N_EVEN_LAYERS, D_MODEL, EVEN_IN), D_MODEL ** -0.5),
        'ab_w_out': normal(ks[9], (N_EVEN_LAYERS, MIX_WIDTH, D_MODEL), MIX_WIDTH ** -0.5),
        'a_q_norm': gain(ks[10], (N_EVEN_LAYERS, HEAD_DIM)),
        'a_k_norm': gain(ks[11], (N_EVEN_LAYERS, HEAD_DIM)),
        'b_decay_exp': RET_DECAY_BASE + jnp.arange(B_HEADS, dtype=jnp.float32) + normal(ks[12], (N_EVEN_LAYERS, 2, B_HEADS), 0.1),
        'b_gn': gain(ks[13], (N_EVEN_LAYERS, B_WIDTH)),
        'c_w_in': normal(ks[14], (N_ODD_LAYERS, D_MODEL, ODD_IN), D_MODEL ** -0.5),
        'c_w_out': normal(ks[15], (N_ODD_LAYERS, C_VAL_WIDTH, D_MODEL), C_VAL_WIDTH ** -0.5),
        'c_lb': normal(ks[16], (DEPTH, C_KEY_WIDTH), 0.1),
        'c_gn': gain(ks[17], (N_ODD_LAYERS, C_VAL_WIDTH)),
        'router_w': normal(ks[18], (DEPTH, D_MODEL, N_EXPERTS), D_MODEL ** -0.5),
        'router_b': normal(ks[19], (DEPTH, N_EXPERTS), 0.01),
        'exp_w1': normal(ks[20], (DEPTH, N_EXPERTS, D_MODEL, 2 * D_EXPERT), D_MODEL ** -0.5),
        'exp_b1': normal(ks[21], (DEPTH, N_EXPERTS, 2 * D_EXPERT), 0.02),
        'exp_w2': normal(ks[22], (DEPTH, N_EXPERTS, D_EXPERT, D_MODEL), D_EXPERT ** -0.5),
        'exp_b2': normal(ks[23], (DEPTH, N_EXPERTS, D_MODEL), 0.02),
        'norm_final': gain(ks[24], (D_MODEL,)),
    }


def reference(x, c, ctx, c_ctx, ada_w, ada_b, norm_mix, norm_ffn, ab_w_in, ab_w_out, a_q_norm, a_k_norm, b_decay_exp, b_gn, c_w_in, c_w_out, c_lb, c_gn, router_w, router_b, exp_w1, exp_b1, exp_w2, exp_b2, norm_final):
    bsz, n_lat, d = x.shape
    n_ctx = ctx.shape[1]
    cos, sin = axial_rope_tables(n_lat)
    lb_soft = jax.nn.softmax(c_lb.astype(jnp.float32), axis=0)
    lower_bounds = jnp.cumsum(lb_soft, axis=0) - lb_soft[0]
    y = ctx
    for l in range(DEPTH):
        ctx_out = l < DEPTH - 1
        j = l // 2
        mod_x = jnp.split((jax.nn.silu(c) @ ada_w[l] + ada_b[l])[:, None, :], N_MOD, axis=-1)
        mod_y = jnp.split(jax.nn.silu(c_ctx) @ ada_w[l] + ada_b[l], N_MOD, axis=-1)
        hx = modulate(rms_norm(x, norm_mix[l]), mod_x[0], mod_x[1])
        hy = modulate(rms_norm(y, norm_mix[l]), mod_y[0], mod_y[1])
        if l % 2 == 0:
            ox, oy = mixer_attention_retention(hx, hy, ab_w_in[j], ab_w_out[j], a_q_norm[j], a_k_norm[j], b_decay_exp[j], b_gn[j], cos, sin, ctx_out)
        else:
            ox, oy = mixer_hgrn2(hx, hy, c_w_in[j], c_w_out[j], lower_bounds[l], c_gn[j], ctx_out)
        x = x + mod_x[2] * ox
        hx = modulate(rms_norm(x, norm_ffn[l]), mod_x[3], mod_x[4])
        if ctx_out:
            y = y + mod_y[2] * oy
            hy = modulate(rms_norm(y, norm_ffn[l]), mod_y[3], mod_y[4])
            tokens = jnp.concatenate([hx.reshape(-1, d), hy.reshape(-1, d)], axis=0)
            ffn = moe_ffn(tokens, router_w[l], router_b[l], exp_w1[l], exp_b1[l], exp_w2[l], exp_b2[l])
            x = x + mod_x[5] * ffn[: bsz * n_lat].reshape(bsz, n_lat, d)
            y = y + mod_y[5] * ffn[bsz * n_lat:].reshape(bsz, n_ctx, d)
        else:
            ffn = moe_ffn(hx.reshape(-1, d), router_w[l], router_b[l], exp_w1[l], exp_b1[l], exp_w2[l], exp_b2[l])
            x = x + mod_x[5] * ffn.reshape(bsz, n_lat, d)
    return rms_norm(x, norm_final)
```

```python
import math
from contextlib import ExitStack

import numpy as np
import ml_dtypes
import concourse.bass as bass
import concourse.mybir as mybir
from concourse.bass_utils import run_bass_kernel_spmd

F32 = mybir.dt.float32
BF16 = mybir.dt.bfloat16
AF = mybir.ActivationFunctionType
ALU = mybir.AluOpType
AX = mybir.AxisListType

D = 2048
KC = 16
NCTX = 256
NX = 2048
NT = NCTX + NX
T = NT // 128
NE = 32
CAP = 384
EPS = 1e-6
N_CORES = 8

C_ID = 0
C_IOTA = 128
C_US = 512
C_RGE = 640
C_RLE = 768
C_MGE = 896
C_MLE = 1024
C_JP1 = 1152
C_CMJ = 1280
C_COL = 1408
C_ONE = 1412
C_OFF1 = 1540
C_OFF2 = 1572
CW = 1588


def make_consts():
    c = np.zeros((128, CW), np.float32)
    p = np.arange(128)[:, None].astype(np.float32)
    j = np.arange(128)[None, :].astype(np.float32)
    c[:, C_ID:C_ID + 128] = np.eye(128)
    c[:, C_IOTA:C_IOTA + 384] = np.arange(384)[None, :]
    c[:, C_US:C_US + 128] = (p < j)
    c[:, C_RGE:C_RGE + 128] = np.maximum(j - p, 0)
    c[:, C_RLE:C_RLE + 128] = np.maximum(p - j, 0)
    c[:, C_MGE:C_MGE + 128] = (j >= p)
    c[:, C_MLE:C_MLE + 128] = (j <= p)
    c[:, C_JP1:C_JP1 + 128] = j + 1
    c[:, C_CMJ:C_CMJ + 128] = 128 - j
    c[:, C_COL] = 127 - p[:, 0]
    c[:, C_COL + 1] = p[:, 0]
    c[:, C_COL + 2] = EPS
    c[:, C_COL + 3] = 1.0
    c[:, C_ONE:C_ONE + 128] = 1.0
    jj = np.arange(32)
    c[:, C_OFF1:C_OFF1 + 32] = 2 * p + (jj // 2) * 256 + jj % 2
    c[:, C_OFF2:C_OFF2 + 16] = 2 * p + (jj[:16] // 2) * 256 + jj[:16] % 2
    return c


def make_cossin():
    gw, hd = 64, 128
    n = NX
    row = np.repeat(np.arange(n // gw, dtype=np.float32), gw)
    col = np.tile(np.arange(gw, dtype=np.float32), n // gw)
    nf = hd // 4
    inv = (np.float32(10000.0) ** (-np.arange(nf, dtype=np.float32) / nf)).astype(np.float32)
    ang = np.concatenate([row[:, None] * inv, col[:, None] * inv], axis=-1).astype(np.float32)
    return np.concatenate([np.cos(ang), np.sin(ang)], axis=-1).astype(np.float32)


class Res:
    __slots__ = ("w", "r")

    def __init__(self):
        self.w = None
        self.r = []


class TL:
    def __init__(self, t):
        self.t = t
        self.r = Res()

    def __getitem__(self, k):
        return self.t[k]


class Eng:
    def __init__(self, e, name, sem):
        self.e = e
        self.name = name
        self.sem = sem
        self.n = 0
        self.seen = {}


def _res(x):
    return x if isinstance(x, Res) else x.r


class Prog:
    def __init__(self, nc, es):
        self.nc = nc

        def mk(e, name):
            return Eng(e, name, es.enter_context(nc.semaphore("s_" + name)))

        self.pe = mk(nc.tensor, "pe")
        self.dve = mk(nc.vector, "dve")
        self.act = mk(nc.scalar, "act")
        self.pool = mk(nc.gpsimd, "pool")
        self.sp = mk(nc.sync, "sp")
        self.engs = [self.pe, self.dve, self.act, self.pool, self.sp]
        self.NQ = 8
        self.dq = {}
        for E in (self.sp, self.act, self.pool):
            self.dq[E.name] = dict(
                sems=[es.enter_context(nc.semaphore("d_%s%d" % (E.name, i))) for i in range(self.NQ)], i=0)
        self.rr = 0

    def _wait(self, E, ev):
        sem, val = ev
        k = id(sem)
        if E.seen.get(k, 0) < val:
            E.e.wait_ge(sem, val)
            E.seen[k] = val

    def _deps(self, E, reads, writes):
        for b in reads:
            b = _res(b)
            if b.w is not None:
                self._wait(E, b.w)
        for b in writes:
            b = _res(b)
            if b.w is not None:
                self._wait(E, b.w)
            for ev in b.r:
                self._wait(E, ev)

    def _commit(self, ev, reads, writes):
        for b in reads:
            _res(b).r.append(ev)
        for b in writes:
            b = _res(b)
            b.w = ev
            b.r = []

    def op(self, E, fn, reads=(), writes=()):
        self._deps(E, reads, writes)
        ins = fn(E.e)
        E.n += 1
        ins.then_inc(E.sem, 1)
        ev = (E.sem, E.n)
        self._commit(ev, reads, writes)
        return ev

    def mm(self, fns, reads=(), writes=()):
        E = self.pe
        self._deps(E, reads, writes)
        ins = None
        for fn in fns:
            ins = fn(E.e)
        E.n += 1
        ins.then_inc(E.sem, 1)
        ev = (E.sem, E.n)
        self._commit(ev, reads, writes)
        return ev

    def dma(self, out, in_, reads=(), writes=(), E=None, fn=None, **kw):
        if E is None:
            E = self.sp
        q = self.dq[E.name]
        i = q["i"]
        q["i"] += 1
        sem = q["sems"][i % self.NQ]
        prev = 16 * (i // self.NQ)
        if prev:
            self._wait(E, (sem, prev))
        self._deps(E, reads, writes)
        if fn is not None:
            fn(E.e).then_inc(sem, 16)
        else:
            E.e.dma_start(out=out, in_=in_, **kw).then_inc(sem, 16)
        ev = (sem, prev + 16)
        self._commit(ev, reads, writes)
        return ev

    def barrier(self):
        evs = [(E.sem, E.n) for E in self.engs if E.n > 0]
        for q in self.dq.values():
            for k, sem in enumerate(q["sems"]):
                uses = (q["i"] - k + self.NQ - 1) // self.NQ
                if uses > 0:
                    evs.append((sem, 16 * uses))
        for E in self.engs:
            for ev in evs:
                if ev[0] is not E.sem:
                    self._wait(E, ev)


class Ctx:
    pass


def build_program(debug=None, stop_after=None, dummy=()):
    debug = debug or []
    nc = bass.Bass("TRN2", target_bir_lowering=False)
    G = Ctx()
    G.nc = nc
    G.debug = debug

    def din(name, shape, dt=F32):
        if name in dummy or ("exp_w1" in dummy and name[:3] in ("w1t", "w2t")):
            shape = [1] * len(shape)
        return nc.dram_tensor(name, list(shape), dt, kind="ExternalInput").ap()

    I = {}
    I["x"] = din("x", [NX, D])
    I["ctx"] = din("ctx", [NCTX, D])
    I["c"] = din("c", [1, D])
    I["c_ctx"] = din("c_ctx", [1, D])
    I["ada_w"] = din("ada_w", [2, D, 6 * D])
    I["ada_b"] = din("ada_b", [2, 6 * D])
    I["norm_mix"] = din("norm_mix", [2, D])
    I["norm_ffn"] = din("norm_ffn", [2, D])
    I["ab_w_in"] = din("ab_w_in", [D, 5632])
    I["ab_w_out"] = din("ab_w_out", [D, D])
    I["a_q_norm"] = din("a_q_norm", [1, 128])
    I["a_k_norm"] = din("a_k_norm", [1, 128])
    I["b_decay_exp"] = din("b_decay_exp", [1, 16])
    I["b_gn"] = din("b_gn", [1, 1024])
    I["c_w_in"] = din("c_w_in", [D, 10240])
    I["c_w_out"] = din("c_w_out", [D, D])
    I["c_lb"] = din("c_lb", [2, D])
    I["c_gn"] = din("c_gn", [1, D])
    I["router_w"] = din("router_w", [2, D, NE])
    I["router_b"] = din("router_b", [2, NE])
    for l_ in range(2):
        I["w1t%d" % l_] = din("w1t%d" % l_, [NE * 16 * 128 * 2, 2048])
        I["w2t%d" % l_] = din("w2t%d" % l_, [NE * 8 * 128 * 2, 2048])
    I["exp_b1"] = din("exp_b1", [2, NE, 2 * D])
    I["exp_b2"] = din("exp_b2", [2, NE, D])
    I["norm_final"] = din("norm_final", [1, D])
    I["consts"] = din("consts", [128, CW])
    I["cossin"] = din("cossin", [NX, 128])
    G.I = I
    G.out = nc.dram_tensor("out", [NX, D], F32, kind="ExternalOutput").ap()

    def dscr(name, shape, dt):
        kind = "ExternalOutput" if name in debug else "Internal"
        return nc.dram_tensor(name, list(shape), dt, kind=kind).ap()

    S = {}
    S["xres"] = dscr("xres", [NT, D], F32)
    S["modD"] = dscr("modD", [2, 2, 6 * D], F32)
    S["QTa"] = dscr("QTa", [8, 128, NT], BF16)
    S["KTa"] = dscr("KTa", [2, 128, NT], BF16)
    S["Va"] = dscr("Va", [NT, 256], BF16)
    S["QTb"] = dscr("QTb", [8, 128, NT], BF16)
    S["KTb"] = dscr("KTb", [8, 128, NT], BF16)
    S["Kb"] = dscr("Kb", [NT, 1024], BF16)
    S["Vb"] = dscr("Vb", [NT, 1024], BF16)
    S["Gt"] = dscr("Gt", [NT, 2048], BF16)
    S["mixT"] = dscr("mixT", [16, 128, NT], BF16)
    S["outE"] = dscr("outE", [68, 256, D], BF16)
    S["hTd"] = dscr("hTd", [KC, 128, NT], BF16)
    if "dbg_h2" in debug:
        S["dbg_h2"] = dscr("dbg_h2", [NT, D], BF16)
        S["dbg_route"] = dscr("dbg_route", [3, 128, T, NE], F32)
    G.S = S
    G.res = {k: Res() for k in S}
    G.out_res = Res()

    with ExitStack() as es:
        P = Prog(nc, es)
        G.P = P
        G.ps = [TL(es.enter_context(nc.psum_tensor("ps%d" % i, [128, 512], F32))) for i in range(8)]
        G.cst = TL(es.enter_context(nc.sbuf_tensor("cst", [128, CW], F32)))
        G.idb = TL(es.enter_context(nc.sbuf_tensor("idb", [128, 128], BF16)))
        G.oneb = TL(es.enter_context(nc.sbuf_tensor("oneb", [128, 128], BF16)))
        es.enter_context(nc.Block())
        P.dma(G.cst[:, :], I["consts"][:, :], writes=[G.cst])
        P.op(P.dve, lambda e: e.tensor_copy(G.idb[:, :], G.cst[:, C_ID:C_ID + 128]), reads=[G.cst], writes=[G.idb])
        P.op(P.dve, lambda e: e.tensor_copy(G.oneb[:, :], G.cst[:, C_ONE:C_ONE + 128]), reads=[G.cst], writes=[G.oneb])

        from_phases(G, stop_after)

        P.barrier()
    return nc


class V:
    def __init__(s, ap, r):
        s.ap = ap
        s.r = r

    def __getitem__(s, k):
        return s.ap[k]


class Stop(Exception):
    pass


def from_phases(G, stop_after):
    P = G.P
    G.stop = False
    with ExitStack() as es0:
        bigA = sb(G, es0, "bigA", [128, KC * NT], BF16)
        hT = V(bigA.t[:, :].rearrange("p (k n) -> p k n", k=KC), bigA.r)
        h2 = V(bigA.t[:, :].rearrange("p (t n) -> p t n", t=T), bigA.r)

        def ph(name, fn):
            if G.stop:
                return
            fn()
            P.barrier()
            if "hTd" in G.debug and name in ("norm0", "norm1"):
                P.dma(G.S["hTd"].rearrange("k p n -> p k n"), hT[:, :, :], reads=[hT], writes=[G.res["hTd"]])
                P.barrier()
            if stop_after == name:
                G.stop = True

        for l in range(2):
            ph("mods%d" % l, lambda: phase_mods(G, l))
        for l in range(2):
            tiles = list(range(T)) if l == 0 else list(range(2, T))
            ph("norm%d" % l, lambda: phase_norm(G, l, 1, list(range(T)), hT=hT))
            if l == 0:
                ph("inproj0", lambda: phase_inproj0(G, hT))
                ph("attn", lambda: phase_attn(G))
                ph("ret", lambda: phase_ret(G))
            else:
                ph("hgrn", lambda: phase_hgrn(G, hT))
            ph("outproj%d" % l, lambda: phase_outproj(G, l, hT, tiles))
            if G.stop:
                break
            with ExitStack() as esr:
                route = dict(mask=sb(G, esr, "rmask", [128, T, NE], F32), gate=sb(G, esr, "rgate", [128, T, NE], F32),
                             pos=sb(G, esr, "rpos", [128, T, NE], F32), dsel=sb(G, esr, "rdsel", [128, 68, T], F32),
                             gsel=sb(G, esr, "rgsel", [128, 68, T], F32))
                ph("norm%db" % l, lambda: phase_norm(G, l, 2, tiles, h2=h2, route=route))
                ph("moe%d" % l, lambda: phase_moe(G, l, tiles, h2, route, bigA))
        ph("final", lambda: phase_final(G))


_UID = [0]


def sb(G, es, name, shape, dt):
    _UID[0] += 1
    return TL(es.enter_context(G.nc.sbuf_tensor("%s_%d" % (name, _UID[0]), list(shape), dt)))


def phase_mods(G, l):
    P, I, S = G.P, G.I, G.S
    with ExitStack() as es:
        cin = sb(G, es, "cin", [32, 128], F32)
        cT = sb(G, es, "cT", [128, KC, 2], F32)
        wst = [sb(G, es, "mw%d" % i, [128, KC, 512], F32) for i in range(2)]
        bia = [sb(G, es, "mb%d" % i, [2, 512], F32) for i in range(2)]
        mro = [sb(G, es, "mr%d" % i, [2, 512], F32) for i in range(2)]
        P.dma(cin[0:16, :], I["c"].rearrange("o (k p) -> (o k) p", p=128), writes=[cin])
        P.dma(cin[16:32, :], I["c_ctx"].rearrange("o (k p) -> (o k) p", p=128), writes=[cin])
        P.op(P.act, lambda e: e.activation(out=cin[:, :], in_=cin[:, :], func=AF.Silu), reads=[cin], writes=[cin])
        ps = G.ps[0]
        P.mm([lambda e: e.transpose(ps[:, 0:32], cin[:, :], G.cst[0:32, C_ID:C_ID + 32])],
             reads=[cin, G.cst], writes=[ps])
        P.op(P.dve, lambda e: e.tensor_copy(cT[:, :, :].rearrange("p k o -> p o k"),
                                            ps[:, 0:32].rearrange("p (o k) -> p o k", o=2)),
             reads=[ps], writes=[cT])
        for g in range(24):
            w = wst[g % 2]
            b = bia[g % 2]
            m = mro[g % 2]
            pq = G.ps[1 + g % 2]
            P.dma(w[:, :, :], I["ada_w"][l, :, g * 512:(g + 1) * 512].rearrange("(k p) n -> p k n", p=128), writes=[w])
            P.dma(b[:, :], I["ada_b"][l, g * 512:(g + 1) * 512].partition_broadcast(2), writes=[b], E=P.act)
            P.mm([(lambda e, k=k: e.matmul(pq[0:2, :], lhsT=cT[:, k, :], rhs=w[:, k, :], start=(k == 0), stop=(k == KC - 1)))
                  for k in range(KC)], reads=[cT, w], writes=[pq])
            P.op(P.dve, lambda e: e.tensor_tensor(out=m[:, :], in0=pq[0:2, :], in1=b[:, :], op=ALU.add),
                 reads=[pq, b], writes=[m])
            P.dma(S["modD"][l, :, g * 512:(g + 1) * 512], m[:, :], reads=[m], writes=[G.res["modD"]], E=P.pool)


def bc_row(G, es, name, row_ap, n, E=None):
    t = sb(G, es, name, [128, n], F32)
    G.P.dma(t[:, :], row_ap.partition_broadcast(128), writes=[t], E=E)
    return t


def src_rows(G, l, which, t):
    I, S = G.I, G.S
    if l == 0 and which == 1:
        return (I["ctx"][t * 128:(t + 1) * 128, :] if t < 2 else I["x"][(t - 2) * 128:(t - 1) * 128, :]), None
    return S["xres"][t * 128:(t + 1) * 128, :], G.res["xres"]


def rstd_from_ss(G, ss, rs, n, inv_n):
    P = G.P
    P.op(P.act, lambda e: e.activation(out=rs[:, 0:n], in_=ss[:, 0:n], func=AF.Sqrt, scale=inv_n,
                                       bias=G.cst[:, C_COL + 2:C_COL + 3]), reads=[ss, G.cst], writes=[rs])
    P.op(P.dve, lambda e: e.reciprocal(rs[:, 0:n], rs[:, 0:n]), reads=[rs], writes=[rs])


def phase_norm(G, l, which, tiles, hT=None, h2=None, route=None):
    P, I, S = G.P, G.I, G.S
    gain = I["norm_mix"] if which == 1 else I["norm_ffn"]
    m0 = 0 if which == 1 else 3
    with ExitStack() as es:
        gb = bc_row(G, es, "gb", gain[l, :], D)
        Gm, Sh = [], []
        for r in range(2):
            sc = bc_row(G, es, "sc", S["modD"][l, r, (m0 + 1) * D:(m0 + 2) * D], D, E=P.act)
            P.op(P.dve, lambda e, sc=sc: e.scalar_tensor_tensor(out=sc[:, :], in0=sc[:, :], scalar=1.0, in1=gb[:, :],
                                                                op0=ALU.add, op1=ALU.mult), reads=[sc, gb], writes=[sc])
            Gm.append(sc)
            Sh.append(bc_row(G, es, "sh", S["modD"][l, r, m0 * D:(m0 + 1) * D], D, E=P.act))
        xt = [sb(G, es, "xt", [128, D], F32) for _ in range(2)]
        junk = sb(G, es, "junk", [128, D], BF16)
        hf = sb(G, es, "hf", [128, D], F32)
        hb = [sb(G, es, "hb", [128, D], BF16) for _ in range(2)]
        ss = sb(G, es, "ss", [128, 4], F32)
        rs = sb(G, es, "rs", [128, 4], F32)
        if which == 2:
            rw = sb(G, es, "rw", [128, KC, NE], F32)
            P.dma(rw[:, :, :], I["router_w"][l].rearrange("(k p) n -> p k n", p=128), writes=[rw])
            rb = bc_row(G, es, "rb", I["router_b"][l, :], NE)
            hTf = sb(G, es, "hTf", [128, KC, 128], F32)
            lg = sb(G, es, "lg", [128, NE], F32)
            top = sb(G, es, "top", [128, 8], F32)
            nm = sb(G, es, "nm", [128, 1], F32)
            ex = sb(G, es, "ex", [128, NE], F32)
            se = sb(G, es, "se", [128, 1], F32)
        for i, t in enumerate(tiles):
            x = xt[i % 2]
            src, sres = src_rows(G, l, which, t)
            P.dma(x[:, :], src, reads=[sres] if sres else [], writes=[x])
            P.op(P.act, lambda e: e.activation(out=junk[:, :], in_=x[:, :], func=AF.Square, accum_out=ss[:, 0:1]),
                 reads=[x], writes=[junk, ss])
            rstd_from_ss(G, ss, rs, 1, 1.0 / D)
            r = 1 if t < 2 else 0
            P.op(P.dve, lambda e: e.scalar_tensor_tensor(out=hf[:, :], in0=x[:, :], scalar=rs[:, 0:1], in1=Gm[r][:, :],
                                                         op0=ALU.mult, op1=ALU.mult), reads=[x, rs, Gm[r]], writes=[hf])
            if which == 1:
                h = hb[i % 2]
                P.op(P.pool, lambda e: e.tensor_tensor(out=h[:, :], in0=hf[:, :], in1=Sh[r][:, :], op=ALU.add),
                     reads=[hf, Sh[r]], writes=[h])
                for half in range(2):
                    pq = G.ps[(2 * i + half) % 4]
                    pv = pq[:, 0:512].bitcast(BF16)
                    P.mm([(lambda e, k=k: e.transpose(pv[:, (k % 8) * 128:(k % 8 + 1) * 128], h[:, k * 128:(k + 1) * 128], G.idb[:, :]))
                          for k in range(half * 8, half * 8 + 8)], reads=[h, G.idb], writes=[pq])
                    eng = P.act if half == 0 else P.dve
                    P.op(eng, lambda e: e.tensor_copy(hT[:, half * 8:half * 8 + 8, t * 128:(t + 1) * 128],
                                                      pv.rearrange("p (k n) -> p k n", k=8)) if eng is P.dve else
                         e.activation(out=hT[:, half * 8:half * 8 + 8, t * 128:(t + 1) * 128],
                                      in_=pv.rearrange("p (k n) -> p k n", k=8), func=AF.Copy),
                         reads=[pq], writes=[hT])
            else:
                P.op(P.dve, lambda e: e.tensor_tensor(out=hf[:, :], in0=hf[:, :], in1=Sh[r][:, :], op=ALU.add),
                     reads=[hf, Sh[r]], writes=[hf])
                P.op(P.act, lambda e: e.activation(out=h2[:, t, :], in_=hf[:, :], func=AF.Copy), reads=[hf], writes=[h2])
                for q4 in range(4):
                    pq = G.ps[q4]
                    P.mm([(lambda e, k=k: e.transpose(pq[:, (k % 4) * 128:(k % 4 + 1) * 128], hf[:, k * 128:(k + 1) * 128],
                                                      G.cst[:, C_ID:C_ID + 128])) for k in range(q4 * 4, q4 * 4 + 4)],
                         reads=[hf, G.cst], writes=[pq])
                    P.op(P.act if q4 % 2 else P.dve,
                         (lambda e: e.activation(out=hTf[:, q4 * 4:q4 * 4 + 4, :], in_=pq[:, :].rearrange("p (k n) -> p k n", k=4), func=AF.Copy))
                         if q4 % 2 else
                         (lambda e: e.tensor_copy(hTf[:, q4 * 4:q4 * 4 + 4, :], pq[:, :].rearrange("p (k n) -> p k n", k=4))),
                         reads=[pq], writes=[hTf])
                pl = G.ps[4]
                P.mm([(lambda e, k=k: e.matmul(pl[:, 0:NE], lhsT=hTf[:, k, :], rhs=rw[:, k, :], start=(k == 0), stop=(k == KC - 1)))
                      for k in range(KC)], reads=[hTf, rw], writes=[pl])
                P.op(P.dve, lambda e: e.tensor_tensor(out=lg[:, :], in0=pl[:, 0:NE], in1=rb[:, :], op=ALU.add),
                     reads=[pl, rb], writes=[lg])
                P.op(P.dve, lambda e: e.max(top[:, :], lg[:, :]), reads=[lg], writes=[top])
                mk, gt = route["mask"], route["gate"]
                P.op(P.dve, lambda e: e.tensor_scalar(out=mk[:, t, :], in0=lg[:, :], scalar1=top[:, 3:4], scalar2=None, op0=ALU.is_ge),
                     reads=[lg, top], writes=[mk])
                P.op(P.dve, lambda e: e.tensor_scalar(out=nm[:, :], in0=top[:, 0:1], scalar1=-1.0, scalar2=None, op0=ALU.mult),
                     reads=[top], writes=[nm])
                P.op(P.act, lambda e: e.activation(out=ex[:, :], in_=lg[:, :], func=AF.Exp, bias=nm[:, 0:1]),
                     reads=[lg, nm], writes=[ex])
                P.op(P.dve, lambda e: e.tensor_tensor(out=ex[:, :], in0=ex[:, :], in1=mk[:, t, :], op=ALU.mult),
                     reads=[ex, mk], writes=[ex])
                P.op(P.dve, lambda e: e.tensor_reduce(out=se[:, :], in_=ex[:, :], axis=AX.X, op=ALU.add), reads=[ex], writes=[se])
                P.op(P.dve, lambda e: e.reciprocal(se[:, :], se[:, :]), reads=[se], writes=[se])
                P.op(P.dve, lambda e: e.tensor_scalar(out=gt[:, t, :], in0=ex[:, :], scalar1=se[:, 0:1], scalar2=None, op0=ALU.mult),
                     reads=[ex, se], writes=[gt])
        if which == 2:
            pos = route["pos"]
            mk = route["mask"]
            for i, t in enumerate(tiles):
                pq = G.ps[5 + i % 2]
                fns = [(lambda e, tp=tp, j=j: e.matmul(pq[:, 0:NE], lhsT=G.cst[:, C_ONE:C_ONE + 128], rhs=mk[:, tp, :],
                                                      start=(j == 0), stop=False)) for j, tp in enumerate(tiles[:i])]
                fns.append(lambda e, i=i, t=t: e.matmul(pq[:, 0:NE], lhsT=G.cst[:, C_US:C_US + 128], rhs=mk[:, t, :],
                                                      start=(i == 0), stop=True))
                P.mm(fns, reads=[mk, G.cst], writes=[pq])
                P.op(P.act, lambda e: e.activation(out=pos[:, t, :], in_=pq[:, 0:NE], func=AF.Copy), reads=[pq], writes=[pos])


def linear_tm(G, es, W, groups, tiles, hT, post):
    P = G.P
    wst = [sb(G, es, "wst", [128, 8, 512], F32) for _ in range(2)]
    wb = [sb(G, es, "wb", [128, KC, 512], BF16) for _ in range(2)]
    cnt = 0
    for gi, (c0, n) in enumerate(groups):
        w = wb[gi % 2]
        for half in range(2):
            st = wst[half]
            P.dma(st[:, :, 0:n], W[half * 1024:(half + 1) * 1024, c0:c0 + n].rearrange("(k p) n -> p k n", p=128),
                  writes=[st], E=(P.sp if half == 0 else P.act))
            if half == 0:
                P.op(P.pool, lambda e: e.tensor_copy(w[:, 0:8, 0:n], st[:, :, 0:n]), reads=[st], writes=[w])
            else:
                P.op(P.act, lambda e: e.activation(out=w[:, 8:16, 0:n], in_=st[:, :, 0:n], func=AF.Copy), reads=[st], writes=[w])
        for t in tiles:
            pq = G.ps[cnt % 3]
            cnt += 1
            P.mm([(lambda e, k=k: e.matmul(pq[:, 0:n], lhsT=hT[:, k, t * 128:(t + 1) * 128], rhs=w[:, k, 0:n],
                                           start=(k == 0), stop=(k == KC - 1))) for k in range(KC)],
                 reads=[hT, w], writes=[pq])
            post(gi, t, pq, n)


def rope_heads(G, src, dst, cs, t, H, tmp):
    P = G.P
    a = src[:, 0:H, 0:64]
    b = src[:, 0:H, 64:128]
    cosb = cs[:, t - 2:t - 1, 0:64].to_broadcast([128, H, 64])
    sinb = cs[:, t - 2:t - 1, 64:128].to_broadcast([128, H, 64])
    t1, t2 = tmp[:, 0:H, 0:64], tmp[:, 0:H, 64:128]
    P.op(P.dve, lambda e: e.tensor_tensor(out=t1, in0=a, in1=cosb, op=ALU.mult), reads=[src, cs], writes=[tmp])
    P.op(P.dve, lambda e: e.tensor_tensor(out=t2, in0=b, in1=sinb, op=ALU.mult), reads=[src, cs], writes=[tmp])
    P.op(P.dve, lambda e: e.tensor_tensor(out=dst[:, 0:H, 0:64], in0=t1, in1=t2, op=ALU.subtract), reads=[tmp], writes=[dst])
    P.op(P.dve, lambda e: e.tensor_tensor(out=t1, in0=a, in1=sinb, op=ALU.mult), reads=[src, cs, dst], writes=[tmp])
    P.op(P.dve, lambda e: e.tensor_tensor(out=t2, in0=b, in1=cosb, op=ALU.mult), reads=[src, cs], writes=[tmp])
    P.op(P.dve, lambda e: e.tensor_tensor(out=dst[:, 0:H, 64:128], in0=t1, in1=t2, op=ALU.add), reads=[tmp], writes=[dst])


def phase_inproj0(G, hT):
    P, I, S = G.P, G.I, G.S
    groups = [(i * 512, 512) for i in range(11)]
    with ExitStack() as es:
        cs = sb(G, es, "cs", [128, 16, 128], F32)
        P.dma(cs[:, :, :], I["cossin"].rearrange("(t p) n -> p t n", p=128), writes=[cs])
        qg = bc_row(G, es, "qg", I["a_q_norm"][0, :], 128)
        kg = bc_row(G, es, "kg", I["a_k_norm"][0, :], 128)
        src = [sb(G, es, "src", [128, 4, 128], F32) for _ in range(2)]
        sq = sb(G, es, "sq", [128, 4, 128], F32)
        tmp = sb(G, es, "tmp", [128, 4, 128], F32)
        ob = [sb(G, es, "ob", [128, 4, 128], BF16) for _ in range(2)]
        oT = [sb(G, es, "oT", [128, 4, 128], BF16) for _ in range(2)]
        ss = sb(G, es, "ss", [128, 4], F32)
        rs = sb(G, es, "rs", [128, 4], F32)
        st = dict(i=0)

        def heads(pq, t, H, c_lo, gain, rope, scale, dT, h0, dTM, tm_c0):
            i = st["i"]
            st["i"] += 1
            s_, o_, oT_ = src[i % 2], ob[i % 2], oT[i % 2]
            pv = pq[:, c_lo:c_lo + H * 128].rearrange("p (h n) -> p h n", h=H)
            P.op(P.act, lambda e: e.activation(out=s_[:, 0:H, :], in_=pv, func=AF.Copy, scale=scale), reads=[pq], writes=[s_])
            if gain is not None:
                P.op(P.act, lambda e: e.activation(out=sq[:, 0:H, :], in_=s_[:, 0:H, :], func=AF.Square), reads=[s_], writes=[sq])
                P.op(P.dve, lambda e: e.tensor_reduce(out=ss[:, 0:H], in_=sq[:, 0:H, :], axis=AX.X, op=ALU.add), reads=[sq], writes=[ss])
                rstd_from_ss(G, ss, rs, H, 1.0 / 128)
                P.op(P.dve, lambda e: e.tensor_tensor(out=s_[:, 0:H, :], in0=s_[:, 0:H, :],
                                                      in1=rs[:, 0:H].unsqueeze(2).to_broadcast([128, H, 128]), op=ALU.mult),
                     reads=[s_, rs], writes=[s_])
                P.op(P.dve, lambda e: e.tensor_tensor(out=s_[:, 0:H, :], in0=s_[:, 0:H, :],
                                                      in1=gain[:, :].unsqueeze(1).to_broadcast([128, H, 128]), op=ALU.mult),
                     reads=[s_, gain], writes=[s_])
            if rope and t >= 2:
                rope_heads(G, s_, o_, cs, t, H, tmp)
            else:
                P.op(P.pool, lambda e: e.tensor_copy(o_[:, 0:H, :], s_[:, 0:H, :]), reads=[s_], writes=[o_])
            if dTM is not None:
                P.dma(dTM[0][t * 128:(t + 1) * 128, tm_c0:tm_c0 + H * 128], o_[:, 0:H, :].rearrange("p h n -> p (h n)"),
                      reads=[o_], writes=[dTM[1]], E=P.pool)
            if dT is not None:
                pt = G.ps[4 + i % 2]
                pvb = pt[:, 0:256].bitcast(BF16)
                P.mm([(lambda e, h=h: e.transpose(pvb[:, h * 128:(h + 1) * 128], o_[:, h, :], G.idb[:, :])) for h in range(H)],
                     reads=[o_, G.idb], writes=[pt])
                P.op(P.act, lambda e: e.activation(out=oT_[:, 0:H, :], in_=pvb[:, 0:H * 128].rearrange("p (h n) -> p h n", h=H), func=AF.Copy),
                     reads=[pt], writes=[oT_])
                P.dma(dT[0][h0:h0 + H, :, t * 128:(t + 1) * 128].rearrange("h d n -> d h n"), oT_[:, 0:H, :],
                      reads=[oT_], writes=[dT[1]], E=P.pool)

        R = G.res
        ksc = 128.0 ** -0.5

        def post(gi, t, pq, n):
            if gi == 0:
                heads(pq, t, 2, 0, kg, True, 1.0, (S["KTa"], R["KTa"]), 0, None, 0)
                heads(pq, t, 2, 256, None, False, 1.0, None, 0, (S["Va"], R["Va"]), 0)
            elif gi in (1, 2):
                heads(pq, t, 4, 0, None, True, ksc, (S["KTb"], R["KTb"]), (gi - 1) * 4, (S["Kb"], R["Kb"]), (gi - 1) * 512)
            elif gi in (3, 4):
                heads(pq, t, 4, 0, None, False, 1.0, None, 0, (S["Vb"], R["Vb"]), (gi - 3) * 512)
            elif gi in (5, 6):
                heads(pq, t, 4, 0, qg, True, 1.0, (S["QTa"], R["QTa"]), (gi - 5) * 4, None, 0)
            elif gi in (7, 8):
                heads(pq, t, 4, 0, None, True, 1.0, (S["QTb"], R["QTb"]), (gi - 7) * 4, None, 0)
            else:
                i = st["i"]
                st["i"] += 1
                o_ = ob[i % 2]
                P.op(P.act, lambda e: e.activation(out=o_[:, :, :], in_=pq[:, 0:512].rearrange("p (h n) -> p h n", h=4), func=AF.Silu),
                     reads=[pq], writes=[o_])
                P.dma(S["Gt"][t * 128:(t + 1) * 128, (gi - 9) * 512:(gi - 8) * 512], o_[:, :, :].rearrange("p h n -> p (h n)"),
                      reads=[o_], writes=[R["Gt"]], E=P.pool)

        linear_tm(G, es, I["ab_w_in"], groups, list(range(T)), hT, post)


def phase_attn(G):
    P, S, R = G.P, G.S, G.res
    sc = 128.0 ** -0.5
    with ExitStack() as es:
        KT = sb(G, es, "KT", [128, NT], BF16)
        V = sb(G, es, "V", [128, T, 128], BF16)
        QT = [sb(G, es, "QT", [128, 512], BF16) for _ in range(2)]
        PT = [sb(G, es, "PT", [128, 512], BF16) for _ in range(3)]
        rz = sb(G, es, "rz", [128, 512], F32)
        OT = [sb(G, es, "OT", [128, 512], BF16) for _ in range(2)]
        it = 0
        for hk in range(2):
            P.dma(KT[:, :], S["KTa"][hk, :, :], reads=[R["KTa"]], writes=[KT])
            P.dma(V[:, :, :], S["Va"][:, hk * 128:(hk + 1) * 128].rearrange("(t p) n -> p t n", p=128), reads=[R["Va"]], writes=[V])
            for h in range(hk * 4, hk * 4 + 4):
                chunks = [(0, 256, [0, 1])] + [(256 + 512 * i, 512, list(range(T))) for i in range(4)]
                for (q0, nq, keys) in chunks:
                    q = QT[it % 2]
                    o = OT[it % 2]
                    it += 1
                    P.dma(q[:, 0:nq], S["QTa"][h, :, q0:q0 + nq], reads=[R["QTa"]], writes=[q], E=P.act)
                    po, pz = G.ps[6], G.ps[7]
                    nk = len(keys)
                    for j, kt in enumerate(keys):
                        pS = G.ps[j % 3]
                        p_ = PT[j % 3]
                        P.mm([lambda e: e.matmul(pS[:, 0:nq], lhsT=KT[:, kt * 128:(kt + 1) * 128], rhs=q[:, 0:nq], start=True, stop=True)],
                             reads=[KT, q], writes=[pS])
                        P.op(P.act, lambda e: e.activation(out=p_[:, 0:nq], in_=pS[:, 0:nq], func=AF.Exp, scale=sc), reads=[pS], writes=[p_])
                        P.mm([lambda e: e.matmul(po[:, 0:nq], lhsT=V[:, kt, :], rhs=p_[:, 0:nq], start=(j == 0), stop=(j == nk - 1)),
                              lambda e: e.matmul(pz[:, 0:nq], lhsT=G.oneb[:, :], rhs=p_[:, 0:nq], start=(j == 0), stop=(j == nk - 1))],
                             reads=[V, p_, G.oneb], writes=[po, pz])
                    P.op(P.dve, lambda e: e.reciprocal(rz[:, 0:nq], pz[:, 0:nq]), reads=[pz], writes=[rz])
                    P.op(P.dve, lambda e: e.tensor_tensor(out=o[:, 0:nq], in0=po[:, 0:nq], in1=rz[:, 0:nq], op=ALU.mult),
                         reads=[po, rz], writes=[o])
                    P.dma(S["mixT"][h, :, q0:q0 + nq], o[:, 0:nq], reads=[o], writes=[R["mixT"]], E=P.pool)


def phase_ret(G):
    P, I, S, R = G.P, G.I, G.S, G.res
    fwd = list(range(T))
    bwd = [1, 0] + list(range(T - 1, 1, -1))
    with ExitStack() as es:
        dexp = bc_row(G, es, "dexp", I["b_decay_exp"][0, :], 16)
        lgm = sb(G, es, "lgm", [128, 16], F32)
        P.op(P.act, lambda e: e.activation(out=lgm[:, :], in_=dexp[:, :], func=AF.Exp, scale=-math.log(2.0)), reads=[dexp], writes=[lgm])
        P.op(P.act, lambda e: e.activation(out=lgm[:, :], in_=lgm[:, :], func=AF.Ln, scale=-1.0, bias=G.cst[:, C_COL + 3:C_COL + 4]),
             reads=[lgm, G.cst], writes=[lgm])
        gnb = bc_row(G, es, "gnb", I["b_gn"][0, :], 1024)
        QT = sb(G, es, "rQT", [128, NT], BF16)
        KT = sb(G, es, "rKT", [128, NT], BF16)
        Kt = sb(G, es, "rK", [128, T, 128], BF16)
        Vt = sb(G, es, "rV", [128, T, 128], BF16)
        Gt = sb(G, es, "rG", [128, T, 128], BF16)
        Dm = sb(G, es, "Dm", [128, 128], F32)
        Dt = sb(G, es, "Dt", [128, 128], F32)
        qdf = sb(G, es, "qdf", [128, 128], F32)
        qdb = sb(G, es, "qdb", [128, 128], F32)
        col = sb(G, es, "col", [128, 4], F32)
        Sb = sb(G, es, "Sb", [128, T, 128], BF16)
        Sf32 = sb(G, es, "Sf32", [128, 128], F32)
        Sfb = [sb(G, es, "Sfb", [128, 128], BF16) for _ in range(2)]
        Kd = [sb(G, es, "Kd", [128, 128], BF16) for _ in range(2)]
        AT = [sb(G, es, "AT", [128, 128], BF16) for _ in range(2)]
        Qf = [sb(G, es, "Qf", [128, 128], BF16) for _ in range(2)]
        Qb = [sb(G, es, "Qb", [128, 128], BF16) for _ in range(2)]
        O = sb(G, es, "O", [128, T, 128], F32)
        sq = sb(G, es, "osq", [128, T, 128], F32)
        st1 = sb(G, es, "st1", [128, T], F32)
        st2 = sb(G, es, "st2", [128, T], F32)
        ob = sb(G, es, "rob", [128, T, 128], BF16)
        oT = [sb(G, es, "roT", [128, 4, 128], BF16) for _ in range(2)]
        cst = G.cst
        for h in range(8):
            P.dma(QT[:, :], S["QTb"][h, :, :], reads=[R["QTb"]], writes=[QT])
            P.dma(KT[:, :], S["KTb"][h, :, :], reads=[R["KTb"]], writes=[KT], E=P.act)
            P.dma(Kt[:, :, :], S["Kb"][:, h * 128:(h + 1) * 128].rearrange("(t p) n -> p t n", p=128), reads=[R["Kb"]], writes=[Kt])
            P.dma(Vt[:, :, :], S["Vb"][:, h * 128:(h + 1) * 128].rearrange("(t p) n -> p t n", p=128), reads=[R["Vb"]], writes=[Vt], E=P.act)
            P.dma(Gt[:, :, :], S["Gt"][:, h * 128:(h + 1) * 128].rearrange("(t p) n -> p t n", p=128), reads=[R["Gt"]], writes=[Gt])
            lf, lb_ = lgm[:, h:h + 1], lgm[:, 8 + h:9 + h]
            P.op(P.act, lambda e: e.activation(out=Dm[:, :], in_=cst[:, C_RGE:C_RGE + 128], func=AF.Exp, scale=lf), reads=[cst, lgm], writes=[Dm])
            P.op(P.dve, lambda e: e.tensor_tensor(out=Dm[:, :], in0=Dm[:, :], in1=cst[:, C_MGE:C_MGE + 128], op=ALU.mult), reads=[Dm, cst], writes=[Dm])
            P.op(P.act, lambda e: e.activation(out=Dt[:, :], in_=cst[:, C_RLE:C_RLE + 128], func=AF.Exp, scale=lb_), reads=[cst, lgm], writes=[Dt])
            P.op(P.dve, lambda e: e.tensor_tensor(out=Dt[:, :], in0=Dt[:, :], in1=cst[:, C_MLE:C_MLE + 128], op=ALU.mult), reads=[Dt, cst], writes=[Dt])
            P.op(P.dve, lambda e: e.tensor_tensor(out=Dm[:, :], in0=Dm[:, :], in1=Dt[:, :], op=ALU.add), reads=[Dm, Dt], writes=[Dm])
            P.op(P.act, lambda e: e.activation(out=qdf[:, :], in_=cst[:, C_JP1:C_JP1 + 128], func=AF.Exp, scale=lf), reads=[cst, lgm], writes=[qdf])
            P.op(P.act, lambda e: e.activation(out=qdb[:, :], in_=cst[:, C_CMJ:C_CMJ + 128], func=AF.Exp, scale=lb_), reads=[cst, lgm], writes=[qdb])
            P.op(P.act, lambda e: e.activation(out=col[:, 0:1], in_=cst[:, C_COL:C_COL + 1], func=AF.Exp, scale=lf), reads=[cst, lgm], writes=[col])
            P.op(P.act, lambda e: e.activation(out=col[:, 1:2], in_=cst[:, C_COL + 1:C_COL + 2], func=AF.Exp, scale=lb_), reads=[cst, lgm], writes=[col])
            P.op(P.act, lambda e: e.activation(out=col[:, 2:3], in_=lf, func=AF.Exp, scale=128.0), reads=[lgm], writes=[col])
            P.op(P.act, lambda e: e.activation(out=col[:, 3:4], in_=lb_, func=AF.Exp, scale=128.0), reads=[lgm], writes=[col])

            def state_step(c, kcol, gcol, i):
                kd = Kd[i % 2]
                pu = G.ps[4 + i % 2]
                P.op(P.pool, lambda e: e.tensor_scalar(out=kd[:, :], in0=Kt[:, c, :], scalar1=col[:, kcol:kcol + 1], scalar2=None, op0=ALU.mult),
                     reads=[Kt, col], writes=[kd])
                P.mm([lambda e: e.matmul(pu[:, 0:128], lhsT=kd[:, :], rhs=Vt[:, c, :], start=True, stop=True)], reads=[kd, Vt], writes=[pu])
                P.op(P.dve, lambda e: e.scalar_tensor_tensor(out=Sf32[:, :], in0=Sf32[:, :], scalar=col[:, gcol:gcol + 1], in1=pu[:, 0:128],
                                                             op0=ALU.mult, op1=ALU.add), reads=[Sf32, col, pu], writes=[Sf32])

            P.op(P.dve, lambda e: e.memset(Sf32[:, :], 0.0), writes=[Sf32])
            for i, c in enumerate(bwd):
                P.op(P.act, lambda e: e.activation(out=Sb[:, c, :], in_=Sf32[:, :], func=AF.Copy), reads=[Sf32], writes=[Sb])
                if i < T - 1:
                    state_step(c, 1, 3, i)
            P.op(P.dve, lambda e: e.memset(Sf32[:, :], 0.0), writes=[Sf32])
            for i, c in enumerate(fwd):
                sfb, at, qf, qb = Sfb[i % 2], AT[i % 2], Qf[i % 2], Qb[i % 2]
                pS, pO = G.ps[i % 2], G.ps[2 + i % 2]
                P.op(P.act, lambda e: e.activation(out=sfb[:, :], in_=Sf32[:, :], func=AF.Copy), reads=[Sf32], writes=[sfb])
                P.mm([lambda e: e.matmul(pS[:, 0:128], lhsT=KT[:, c * 128:(c + 1) * 128], rhs=QT[:, c * 128:(c + 1) * 128], start=True, stop=True)],
                     reads=[KT, QT], writes=[pS])
                P.op(P.dve, lambda e: e.tensor_tensor(out=at[:, :], in0=pS[:, 0:128], in1=Dm[:, :], op=ALU.mult), reads=[pS, Dm], writes=[at])
                P.op(P.pool, lambda e: e.tensor_tensor(out=qf[:, :], in0=QT[:, c * 128:(c + 1) * 128], in1=qdf[:, :], op=ALU.mult), reads=[QT, qdf], writes=[qf])
                P.op(P.pool, lambda e: e.tensor_tensor(out=qb[:, :], in0=QT[:, c * 128:(c + 1) * 128], in1=qdb[:, :], op=ALU.mult), reads=[QT, qdb], writes=[qb])
                P.mm([lambda e: e.matmul(pO[:, 0:128], lhsT=at[:, :], rhs=Vt[:, c, :], start=True, stop=False),
                      lambda e: e.matmul(pO[:, 0:128], lhsT=qf[:, :], rhs=sfb[:, :], start=False, stop=False),
                      lambda e: e.matmul(pO[:, 0:128], lhsT=qb[:, :], rhs=Sb[:, c, :], start=False, stop=True)],
                     reads=[at, Vt, qf, sfb, qb, Sb], writes=[pO])
                P.op(P.act, lambda e: e.activation(out=O[:, c, :], in_=pO[:, 0:128], func=AF.Copy), reads=[pO], writes=[O])
                if i < T - 1:
                    state_step(c, 0, 2, i)
            P.op(P.dve, lambda e: e.tensor_reduce(out=st1[:, :], in_=O[:, :, :], axis=AX.X, op=ALU.add), reads=[O], writes=[st1])
            P.op(P.dve, lambda e: e.tensor_scalar(out=st1[:, :], in0=st1[:, :], scalar1=1.0 / 128, scalar2=None, op0=ALU.mult), reads=[st1], writes=[st1])
            P.op(P.dve, lambda e: e.tensor_tensor(out=O[:, :, :], in0=O[:, :, :], in1=st1[:, :].unsqueeze(2).to_broadcast([128, T, 128]), op=ALU.subtract),
                 reads=[O, st1], writes=[O])
            P.op(P.act, lambda e: e.activation(out=sq[:, :, :], in_=O[:, :, :], func=AF.Square), reads=[O], writes=[sq])
            P.op(P.dve, lambda e: e.tensor_reduce(out=st2[:, :], in_=sq[:, :, :], axis=AX.X, op=ALU.add), reads=[sq], writes=[st2])
            P.op(P.act, lambda e: e.activation(out=st2[:, :], in_=st2[:, :], func=AF.Sqrt, scale=1.0 / 128, bias=cst[:, C_COL + 2:C_COL + 3]),
                 reads=[st2, cst], writes=[st2])
            P.op(P.dve, lambda e: e.reciprocal(st2[:, :], st2[:, :]), reads=[st2], writes=[st2])
            P.op(P.dve, lambda e: e.tensor_tensor(out=O[:, :, :], in0=O[:, :, :], in1=st2[:, :].unsqueeze(2).to_broadcast([128, T, 128]), op=ALU.mult),
                 reads=[O, st2], writes=[O])
            P.op(P.dve, lambda e: e.tensor_tensor(out=O[:, :, :], in0=O[:, :, :], in1=gnb[:, h * 128:(h + 1) * 128].unsqueeze(1).to_broadcast([128, T, 128]), op=ALU.mult),
                 reads=[O, gnb], writes=[O])
            P.op(P.dve, lambda e: e.tensor_tensor(out=ob[:, :, :], in0=O[:, :, :], in1=Gt[:, :, :], op=ALU.mult), reads=[O, Gt], writes=[ob])
            transpose_out(G, ob, oT, S["mixT"], R["mixT"], 8 + h, list(range(T)))


def transpose_out(G, ob, oT, dst, dres, chunk, tiles):
    P = G.P
    for g0 in range(0, len(tiles), 4):
        ts = tiles[g0:g0 + 4]
        n = len(ts)
        pt = G.ps[6 + (g0 // 4) % 2]
        o_ = oT[(g0 // 4) % 2]
        pvb = pt[:, 0:256].bitcast(BF16)
        P.mm([(lambda e, j=j, t=t: e.transpose(pvb[:, j * 128:(j + 1) * 128], ob[:, t, :], G.idb[:, :])) for j, t in enumerate(ts)],
             reads=[ob, G.idb], writes=[pt])
        P.op(P.act, lambda e: e.activation(out=o_[:, 0:n, :], in_=pvb[:, 0:n * 128].rearrange("p (h n) -> p h n", h=n), func=AF.Copy),
             reads=[pt], writes=[o_])
        P.dma(dst[chunk, :, ts[0] * 128:(ts[0] + n) * 128], o_[:, 0:n, :].rearrange("p h n -> p (h n)"), reads=[o_], writes=[dres], E=P.pool)


def phase_outproj(G, l, hT, tiles):
    P, I, S, R = G.P, G.I, G.S, G.res
    W = I["ab_w_out"] if l == 0 else I["c_w_out"]
    with ExitStack() as es:
        P.dma(hT[:, :, :], S["mixT"].rearrange("k p n -> p k n"), reads=[R["mixT"]], writes=[hT])
        gate = [bc_row(G, es, "og", S["modD"][l, r, 2 * D:3 * D], D, E=P.act) for r in range(2)]
        xo = [sb(G, es, "xo", [128, 512], F32) for _ in range(2)]
        xn = [sb(G, es, "xn", [128, 512], F32) for _ in range(2)]
        st = dict(i=0)

        def post(gi, t, pq, n):
            i = st["i"]
            st["i"] += 1
            a, b = xo[i % 2], xn[i % 2]
            if l == 0:
                src = I["ctx"][t * 128:(t + 1) * 128, gi * 512:(gi + 1) * 512] if t < 2 else I["x"][(t - 2) * 128:(t - 1) * 128, gi * 512:(gi + 1) * 512]
                rd = []
            else:
                src = S["xres"][t * 128:(t + 1) * 128, gi * 512:(gi + 1) * 512]
                rd = [R["xres"]]
            P.dma(a[:, :], src, reads=rd, writes=[a], E=P.act)
            r = 1 if t < 2 else 0
            P.op(P.dve, lambda e: e.tensor_tensor(out=b[:, :], in0=pq[:, 0:512], in1=gate[r][:, gi * 512:(gi + 1) * 512], op=ALU.mult),
                 reads=[pq, gate[r]], writes=[b])
            P.op(P.pool, lambda e: e.tensor_tensor(out=b[:, :], in0=b[:, :], in1=a[:, :], op=ALU.add), reads=[b, a], writes=[b])
            P.dma(S["xres"][t * 128:(t + 1) * 128, gi * 512:(gi + 1) * 512], b[:, :], reads=[b], writes=[R["xres"]], E=P.pool)

        linear_tm(G, es, W, [(i * 512, 512) for i in range(4)], tiles, hT, post)


def phase_moe(G, l, tiles, h2, route, bigA):
    P, I, S, R, nc = G.P, G.I, G.S, G.res, G.nc
    nT = len(tiles)
    NB = -(-(4 * nT * 128 + NE * 255) // 256)
    BIG = 1.0e6
    mk, gt, pos = route["mask"], route["gate"], route["pos"]
    dselA, gselA = route["dsel"], route["gsel"]
    cst = G.cst
    t0, t1 = tiles[0], tiles[-1] + 1
    with ExitStack() as es:
        b1T = sb(G, es, "b1T", [128, 2 * KC, NE], F32)
        with ExitStack() as es1:
            b1s = sb(G, es1, "b1s", [NE, 2 * D], F32)
            P.dma(b1s[:, :], I["exp_b1"][l, :, :], writes=[b1s])
            for two in range(2):
                pq = G.ps[two]
                P.mm([(lambda e, c=c: e.transpose(pq[:, c * 32:(c + 1) * 32], b1s[:, c * 256 + two:(c + 1) * 256:2], cst[0:32, C_ID:C_ID + 32]))
                      for c in range(KC)], reads=[b1s, cst], writes=[pq])
                P.op(P.dve, lambda e: e.tensor_copy(b1T[:, two:2 * KC:2, :], pq[:, :].rearrange("p (c n) -> p c n", c=KC)), reads=[pq], writes=[b1T])
            P.barrier()
        cnt = sb(G, es, "cnt", [128, NE], F32)
        nblk = sb(G, es, "nblk", [128, NE], F32)
        bend = sb(G, es, "bend", [128, NE], F32)
        bst = sb(G, es, "bst", [128, NE], F32)
        tmpe = sb(G, es, "tmpe", [128, NE], F32)
        destm = sb(G, es, "destm", [128, T, NE], F32)
        oh = sb(G, es, "oh", [128, NB, NE], F32)
        oh2 = sb(G, es, "oh2", [128, NB, NE], F32)
        bef = sb(G, es, "bef", [128, NB], F32)
        bei = sb(G, es, "bei", [128, NB], mybir.dt.int32)
        tmp3 = sb(G, es, "tmp3", [128, T, NE], F32)
        b1tmp = sb(G, es, "b1tmp", [128, 2 * KC, NE], F32)
        b1sel = [sb(G, es, "b1sel", [128, 2 * KC], F32) for _ in range(2)]
        pq = G.ps[0]
        P.mm([(lambda e, j=j, t=t: e.matmul(pq[:, 0:NE], lhsT=cst[:, C_ONE:C_ONE + 128], rhs=mk[:, t, :], start=(j == 0), stop=(j == nT - 1)))
              for j, t in enumerate(tiles)], reads=[mk, cst], writes=[pq])
        P.op(P.dve, lambda e: e.tensor_copy(cnt[:, :], pq[:, 0:NE]), reads=[pq], writes=[cnt])
        P.op(P.dve, lambda e: e.tensor_scalar(out=nblk[:, :], in0=cnt[:, :], scalar1=0.0, scalar2=None, op0=ALU.is_gt), reads=[cnt], writes=[nblk])
        for j in range(1, 9):
            P.op(P.dve, lambda e: e.tensor_scalar(out=tmpe[:, :], in0=cnt[:, :], scalar1=256.0 * j, scalar2=None, op0=ALU.is_gt), reads=[cnt], writes=[tmpe])
            P.op(P.dve, lambda e: e.tensor_tensor(out=nblk[:, :], in0=nblk[:, :], in1=tmpe[:, :], op=ALU.add), reads=[nblk, tmpe], writes=[nblk])
        P.op(P.dve, lambda e: e.tensor_tensor_scan(out=bend[:, :], data0=cst[:, C_ONE:C_ONE + NE], data1=nblk[:, :], initial=0.0,
                                                   op0=ALU.mult, op1=ALU.add), reads=[cst, nblk], writes=[bend])
        P.op(P.dve, lambda e: e.tensor_tensor(out=bst[:, :], in0=bend[:, :], in1=nblk[:, :], op=ALU.subtract), reads=[bend, nblk], writes=[bst])
        P.op(P.dve, lambda e: e.tensor_scalar(out=tmpe[:, :], in0=bst[:, :], scalar1=256.0, scalar2=BIG, op0=ALU.mult, op1=ALU.add), reads=[bst], writes=[tmpe])
        P.op(P.dve, lambda e: e.tensor_tensor(out=destm[:, t0:t1, :], in0=pos[:, t0:t1, :], in1=tmpe[:, :].unsqueeze(1).to_broadcast([128, nT, NE]), op=ALU.add),
             reads=[pos, tmpe], writes=[destm])
        P.op(P.dve, lambda e: e.tensor_tensor(out=destm[:, t0:t1, :], in0=destm[:, t0:t1, :], in1=mk[:, t0:t1, :], op=ALU.mult), reads=[destm, mk], writes=[destm])
        P.op(P.dve, lambda e: e.tensor_scalar(out=destm[:, t0:t1, :], in0=destm[:, t0:t1, :], scalar1=-BIG, scalar2=None, op0=ALU.add), reads=[destm], writes=[destm])
        iob = cst[:, C_IOTA:C_IOTA + NB].unsqueeze(2).to_broadcast([128, NB, NE])
        P.op(P.dve, lambda e: e.tensor_tensor(out=oh[:, :, :], in0=iob, in1=bst[:, :].unsqueeze(1).to_broadcast([128, NB, NE]), op=ALU.is_ge), reads=[cst, bst], writes=[oh])
        P.op(P.dve, lambda e: e.tensor_tensor(out=oh2[:, :, :], in0=iob, in1=bend[:, :].unsqueeze(1).to_broadcast([128, NB, NE]), op=ALU.is_lt), reads=[cst, bend], writes=[oh2])
        P.op(P.dve, lambda e: e.tensor_tensor(out=oh[:, :, :], in0=oh[:, :, :], in1=oh2[:, :, :], op=ALU.mult), reads=[oh, oh2], writes=[oh])
        P.op(P.dve, lambda e: e.tensor_tensor(out=oh2[:, :, :], in0=oh[:, :, :], in1=cst[:, C_IOTA:C_IOTA + NE].unsqueeze(1).to_broadcast([128, NB, NE]), op=ALU.mult),
             reads=[oh, cst], writes=[oh2])
        P.op(P.dve, lambda e: e.tensor_reduce(out=bef[:, :], in_=oh2[:, :, :], axis=AX.X, op=ALU.add), reads=[oh2], writes=[bef])
        P.op(P.dve, lambda e: e.tensor_copy(bei[:, :], bef[:, :]), reads=[bef], writes=[bei])
        for b in range(NB):
            ohb = oh[:, b:b + 1, :].to_broadcast([128, nT, NE])
            P.op(P.dve, lambda e: e.tensor_tensor(out=tmp3[:, t0:t1, :], in0=destm[:, t0:t1, :], in1=ohb, op=ALU.mult), reads=[destm, oh], writes=[tmp3])
            P.op(P.dve, lambda e: e.tensor_reduce(out=dselA[:, b, t0:t1], in_=tmp3[:, t0:t1, :], axis=AX.X, op=ALU.add), reads=[tmp3], writes=[dselA])
            P.op(P.pool, lambda e: e.tensor_tensor(out=oh2[:, 0:nT, :], in0=gt[:, t0:t1, :], in1=ohb, op=ALU.mult), reads=[gt, oh], writes=[oh2])
            P.op(P.dve, lambda e: e.tensor_reduce(out=gselA[:, b, t0:t1], in_=oh2[:, 0:nT, :], axis=AX.X, op=ALU.add), reads=[oh2], writes=[gselA])
            P.op(P.dve, lambda e: e.tensor_scalar(out=dselA[:, b, t0:t1], in0=dselA[:, b, t0:t1], scalar1=-256.0 * b, scalar2=None, op0=ALU.add),
                 reads=[dselA], writes=[dselA])
        Sall = sb(G, es, "Sall", [128, nT, 256], BF16)
        hgT = sb(G, es, "hgT", [128, KC, 256], BF16)
        actT = sb(G, es, "actT", [128, KC, 256], BF16)
        wbb = [sb(G, es, "wbb", [128, KC * 256], BF16) for _ in range(4)]
        gg = [sb(G, es, "gg", [128, 256], F32) for _ in range(2)]
        sg = [sb(G, es, "sg", [128, 256], F32) for _ in range(2)]
        ll = [sb(G, es, "ll", [128, 256], F32) for _ in range(2)]
        oS = [sb(G, es, "oS", [128, 256], BF16) for _ in range(3)]
        be1 = sb(G, es, "be1", [128, NB], F32)
        be2 = sb(G, es, "be2", [128, NB], F32)
        idx = [sb(G, es, "idx", [128, 48], mybir.dt.int32) for _ in range(2)]
        P.op(P.dve, lambda e: e.tensor_scalar(out=be1[:, :], in0=bef[:, :], scalar1=4096.0, scalar2=None, op0=ALU.mult), reads=[bef], writes=[be1])
        P.op(P.dve, lambda e: e.tensor_scalar(out=be2[:, :], in0=bef[:, :], scalar1=2048.0, scalar2=None, op0=ALU.mult), reads=[bef], writes=[be2])
        wi = dict(i=0)

        class WV:
            def __init__(s_, tl):
                s_.ap = tl.t[:, :].rearrange("p (k n) -> p k n", k=KC)
                s_.r = tl.r

            def __getitem__(s_, k):
                return s_.ap[k]

        def load_block(ix, which, piece):
            i = wi["i"]
            wi["i"] += 1
            w = wbb[i % 4]
            W = I["w1t%d" % l] if which == 1 else I["w2t%d" % l]
            col0 = 0 if which == 1 else 32
            for h in range(2):
                j = col0 + 2 * piece + h
                P.dma(None, None, reads=[ix], writes=[w], E=P.pool,
                      fn=lambda e: e.indirect_dma_start(out=w[:, h * 2048:(h + 1) * 2048], out_offset=None, in_=W[:, :],
                                                        in_offset=bass.IndirectOffsetOnAxis(ap=ix[:, j:j + 1], axis=0)))
            return WV(w)

        oi = 0
        for b in range(NB):
            ix = idx[b % 2]
            P.op(P.dve, lambda e: e.tensor_scalar(out=ix[:, 0:32], in0=cst[:, C_OFF1:C_OFF1 + 32], scalar1=be1[:, b:b + 1], scalar2=None, op0=ALU.add),
                 reads=[cst, be1], writes=[ix])
            P.op(P.dve, lambda e: e.tensor_scalar(out=ix[:, 32:48], in0=cst[:, C_OFF2:C_OFF2 + 16], scalar1=be2[:, b:b + 1], scalar2=None, op0=ALU.add),
                 reads=[cst, be2], writes=[ix])
            bs = b1sel[b % 2]
            P.op(P.pool, lambda e: e.tensor_tensor(out=b1tmp[:, :, :], in0=b1T[:, :, :], in1=oh[:, b:b + 1, :].to_broadcast([128, 2 * KC, NE]), op=ALU.mult),
                 reads=[b1T, oh], writes=[b1tmp])
            P.op(P.dve, lambda e: e.tensor_reduce(out=bs[:, :], in_=b1tmp[:, :, :], axis=AX.X, op=ALU.add), reads=[b1tmp], writes=[bs])
            for j, t in enumerate(tiles):
                P.op(P.dve, lambda e: e.tensor_scalar(out=Sall[:, j, :], in0=cst[:, C_IOTA:C_IOTA + 256], scalar1=dselA[:, b, t:t + 1],
                                                      scalar2=None, op0=ALU.is_equal), reads=[cst, dselA], writes=[Sall])
            for k in range(KC):
                pq = G.ps[k % 2]
                P.mm([(lambda e, j=j, t=t: e.matmul(pq[:, 0:256], lhsT=h2[:, t, k * 128:(k + 1) * 128], rhs=Sall[:, j, :],
                                                   start=(j == 0), stop=(j == nT - 1))) for j, t in enumerate(tiles)],
                     reads=[h2, Sall], writes=[pq])
                if k % 2:
                    P.op(P.act, lambda e: e.activation(out=hgT[:, k, :], in_=pq[:, 0:256], func=AF.Copy), reads=[pq], writes=[hgT])
                else:
                    P.op(P.dve, lambda e: e.tensor_copy(hgT[:, k, :], pq[:, 0:256]), reads=[pq], writes=[hgT])
            for c in range(KC):
                w = load_block(ix, 1, c)
                pg, pl = G.ps[2 + c % 2], G.ps[4 + c % 2]
                g_, s_, l_ = gg[c % 2], sg[c % 2], ll[c % 2]
                P.mm([(lambda e, k=k: e.matmul(pg[:, 0:256], lhsT=w[:, k, 0:128], rhs=hgT[:, k, :], start=(k == 0), stop=(k == KC - 1)))
                      for k in range(KC)], reads=[w, hgT], writes=[pg])
                P.mm([(lambda e, k=k: e.matmul(pl[:, 0:256], lhsT=w[:, k, 128:256], rhs=hgT[:, k, :], start=(k == 0), stop=(k == KC - 1)))
                      for k in range(KC)], reads=[w, hgT], writes=[pl])
                P.op(P.dve, lambda e: e.tensor_scalar(out=g_[:, :], in0=pg[:, 0:256], scalar1=bs[:, 2 * c:2 * c + 1], scalar2=7.0,
                                                      op0=ALU.add, op1=ALU.min), reads=[pg, bs], writes=[g_])
                P.op(P.act, lambda e: e.activation(out=s_[:, :], in_=g_[:, :], func=AF.Sigmoid, scale=1.702), reads=[g_], writes=[s_])
                P.op(P.dve, lambda e: e.tensor_scalar(out=l_[:, :], in0=pl[:, 0:256], scalar1=bs[:, 2 * c + 1:2 * c + 2], scalar2=7.0,
                                                      op0=ALU.add, op1=ALU.min), reads=[pl, bs], writes=[l_])
                P.op(P.dve, lambda e: e.tensor_scalar(out=l_[:, :], in0=l_[:, :], scalar1=-7.0, scalar2=1.0, op0=ALU.max, op1=ALU.add),
                     reads=[l_], writes=[l_])
                P.op(P.pool, lambda e: e.tensor_tensor(out=g_[:, :], in0=g_[:, :], in1=s_[:, :], op=ALU.mult), reads=[g_, s_], writes=[g_])
                P.op(P.dve, lambda e: e.tensor_tensor(out=actT[:, c, :], in0=g_[:, :], in1=l_[:, :], op=ALU.mult), reads=[g_, l_], writes=[actT])
            for n in range(8):
                w = load_block(ix, 2, n)
                for s_i in range(2):
                    pq = G.ps[6 + oi % 2]
                    o_ = oS[oi % 3]
                    oi += 1
                    P.mm([(lambda e, k=k: e.matmul(pq[:, 0:256], lhsT=actT[:, k, s_i * 128:(s_i + 1) * 128], rhs=w[:, k, :],
                                                   start=(k == 0), stop=(k == KC - 1))) for k in range(KC)], reads=[actT, w], writes=[pq])
                    if oi % 2:
                        P.op(P.act, lambda e: e.activation(out=o_[:, :], in_=pq[:, 0:256], func=AF.Copy), reads=[pq], writes=[o_])
                    else:
                        P.op(P.dve, lambda e: e.tensor_copy(o_[:, :], pq[:, 0:256]), reads=[pq], writes=[o_])
                    P.dma(S["outE"][b, s_i * 128:(s_i + 1) * 128, n * 256:(n + 1) * 256], o_[:, :], reads=[o_], writes=[R["outE"]], E=P.pool)
    P.barrier()
    with ExitStack() as es:
        acc = bigA.t[:, :].bitcast(F32).rearrange("p (t n) -> p t n", n=D)
        accR = bigA.r
        oE = [sb(G, es, "oE", [128, 2, D], BF16) for _ in range(2)]
        Sg = [sb(G, es, "Sg", [128, 256], BF16) for _ in range(2)]
        SgT = [sb(G, es, "SgT", [128, 2, 128], BF16) for _ in range(2)]
        b2s = sb(G, es, "b2s", [NE, D], F32)
        gT = sb(G, es, "gT", [NE, 128], F32)
        xt = [sb(G, es, "mxt", [128, D], F32) for _ in range(2)]
        gate = [bc_row(G, es, "mg", S["modD"][l, r, 5 * D:6 * D], D, E=P.act) for r in range(2)]
        P.dma(b2s[:, :], I["exp_b2"][l, :, :], writes=[b2s])
        for g0 in range(0, nT, 9):
            grp = tiles[g0:g0 + 9]
            for j, t in enumerate(grp):
                pt = G.ps[4]
                P.mm([lambda e: e.transpose(pt[0:NE, 0:128], gt[:, t, :], cst[:, C_ID:C_ID + 128])], reads=[gt, cst], writes=[pt])
                P.op(P.act, lambda e: e.activation(out=gT[:, :], in_=pt[0:NE, 0:128], func=AF.Copy), reads=[pt], writes=[gT])
                for n in range(4):
                    pq = G.ps[n]
                    P.mm([lambda e: e.matmul(pq[:, :], lhsT=gT[:, :], rhs=b2s[:, n * 512:(n + 1) * 512], start=True, stop=True)],
                         reads=[gT, b2s], writes=[pq])
                    P.op(P.dve if n % 2 else P.act,
                         (lambda e: e.tensor_copy(acc[:, j, n * 512:(n + 1) * 512], pq[:, :])) if n % 2 else
                         (lambda e: e.activation(out=acc[:, j, n * 512:(n + 1) * 512], in_=pq[:, :], func=AF.Copy)),
                         reads=[pq], writes=[accR])
            it = 0
            for b in range(NB):
                o = oE[b % 2]
                P.dma(o[:, :, :], S["outE"][b, :, :].rearrange("(s p) n -> p s n", p=128), reads=[R["outE"]], writes=[o])
                for j, t in enumerate(grp):
                    sg_, sgT = Sg[it % 2], SgT[it % 2]
                    it += 1
                    P.op(P.pool, lambda e: e.tensor_scalar(out=sg_[:, :], in0=cst[:, C_IOTA:C_IOTA + 256], scalar1=dselA[:, b, t:t + 1],
                                                           scalar2=gselA[:, b, t:t + 1], op0=ALU.is_equal, op1=ALU.mult),
                         reads=[cst, dselA, gselA], writes=[sg_])
                    pt = G.ps[4 + it % 2]
                    pvb = pt[:, 0:128].bitcast(BF16)
                    P.mm([(lambda e, s_i=s_i: e.transpose(pvb[:, s_i * 128:(s_i + 1) * 128], sg_[:, s_i * 128:(s_i + 1) * 128], G.idb[:, :]))
                          for s_i in range(2)], reads=[sg_, G.idb], writes=[pt])
                    P.op(P.act, lambda e: e.activation(out=sgT[:, :, :], in_=pvb[:, 0:256].rearrange("p (s n) -> p s n", s=2), func=AF.Copy),
                         reads=[pt], writes=[sgT])
                    for n in range(4):
                        pq = G.ps[n]
                        P.mm([(lambda e, s_i=s_i: e.matmul(pq[:, :], lhsT=sgT[:, s_i, :], rhs=o[:, s_i, n * 512:(n + 1) * 512],
                                                          start=(s_i == 0), stop=(s_i == 1))) for s_i in range(2)], reads=[sgT, o], writes=[pq])
                        P.op(P.dve, lambda e: e.tensor_tensor(out=acc[:, j, n * 512:(n + 1) * 512], in0=acc[:, j, n * 512:(n + 1) * 512],
                                                              in1=pq[:, :], op=ALU.add), reads=[pq, accR], writes=[accR])
            for j, t in enumerate(grp):
                x = xt[j % 2]
                r = 1 if t < 2 else 0
                P.dma(x[:, :], S["xres"][t * 128:(t + 1) * 128, :], reads=[R["xres"]], writes=[x])
                P.op(P.pool, lambda e: e.tensor_tensor(out=acc[:, j, :], in0=acc[:, j, :], in1=gate[r][:, :], op=ALU.mult),
                     reads=[accR, gate[r]], writes=[accR])
                P.op(P.dve, lambda e: e.tensor_tensor(out=x[:, :], in0=x[:, :], in1=acc[:, j, :], op=ALU.add), reads=[x, accR], writes=[x])
                P.dma(S["xres"][t * 128:(t + 1) * 128, :], x[:, :], reads=[x], writes=[R["xres"]], E=P.pool)


def phase_hgrn(G, hT):
    P, I, S, R = G.P, G.I, G.S, G.res
    cst = G.cst
    W = I["c_w_in"]
    fwd = list(range(T))
    bwd = [1, 0] + list(range(T - 1, 1, -1))
    NCH = [(i * 512, min(512, NT - i * 512)) for i in range(5)]
    with ExitStack() as es:
        lbT = sb(G, es, "lbT", [128, 16], F32)
        omlT = sb(G, es, "omlT", [128, 16], F32)
        with ExitStack() as es1:
            c0 = sb(G, es1, "c0", [16, 128], F32)
            c1 = sb(G, es1, "c1", [16, 128], F32)
            P.dma(c0[:, :], I["c_lb"][0, :].rearrange("(h p) -> h p", p=128), writes=[c0])
            P.dma(c1[:, :], I["c_lb"][1, :].rearrange("(h p) -> h p", p=128), writes=[c1])
            P.op(P.dve, lambda e: e.tensor_tensor(out=c1[:, :], in0=c1[:, :], in1=c0[:, :], op=ALU.subtract), reads=[c0, c1], writes=[c1])
            P.op(P.act, lambda e: e.activation(out=c1[:, :], in_=c1[:, :], func=AF.Sigmoid), reads=[c1], writes=[c1])
            pq = G.ps[0]
            P.mm([lambda e: e.transpose(pq[:, 0:16], c1[:, :], cst[0:16, C_ID:C_ID + 16])], reads=[c1, cst], writes=[pq])
            P.op(P.dve, lambda e: e.tensor_copy(lbT[:, :], pq[:, 0:16]), reads=[pq], writes=[lbT])
            P.op(P.dve, lambda e: e.tensor_scalar(out=omlT[:, :], in0=lbT[:, :], scalar1=-1.0, scalar2=1.0, op0=ALU.mult, op1=ALU.add),
                 reads=[lbT], writes=[omlT])
            P.barrier()
        gnb = bc_row(G, es, "cgn", I["c_gn"][0, :], D)
        wst = sb(G, es, "hwst", [128, KC, 128], F32)
        W5 = sb(G, es, "W5", [128, KC, 5, 128], BF16)
        lf = sb(G, es, "lf", [128, NT], F32)
        kk = sb(G, es, "kk", [128, NT], F32)
        BX = sb(G, es, "BX", [128, NT], F32)
        qs = sb(G, es, "qs", [128, NT], F32)
        qe = sb(G, es, "qe", [128, NT], BF16)
        ke = sb(G, es, "ke", [128, NT], BF16)
        vt = sb(G, es, "vt", [128, T, 128], BF16)
        gtm = sb(G, es, "gtm", [128, T, 128], BF16)
        O = sb(G, es, "hO", [128, T, 128], F32)
        ones = sb(G, es, "hones", [128, NT], BF16)
        ob = sb(G, es, "hob", [128, T, 128], BF16)
        oT = [sb(G, es, "hoT", [128, 4, 128], BF16) for _ in range(2)]
        sc = sb(G, es, "hsc", [128, 4, T], F32)
        Sst = sb(G, es, "hS", [128, 128], F32)
        Ssc = [sb(G, es, "hSsc", [128, 128], BF16) for _ in range(2)]
        keT = [sb(G, es, "hkeT", [128, 128], BF16) for _ in range(2)]
        AT = [sb(G, es, "hAT", [128, 128], BF16) for _ in range(2)]
        st2 = sb(G, es, "hst2", [128, T], F32)
        P.op(P.pool, lambda e: e.memset(ones[:, :], 1.0), writes=[ones])
        BX3 = BX.t[:, :].rearrange("p (c n) -> p c n", n=128)
        lf3 = lf.t[:, :].rearrange("p (c n) -> p c n", n=128)
        for h in range(16):
            for j in range(5):
                P.dma(wst[:, :, :], W[:, j * D + h * 128:j * D + (h + 1) * 128].rearrange("(k p) n -> p k n", p=128), writes=[wst],
                      E=(P.sp if j % 2 == 0 else P.act))
                if j % 2 == 0:
                    P.op(P.pool, lambda e: e.tensor_copy(W5[:, :, j, :], wst[:, :, :]), reads=[wst], writes=[W5])
                else:
                    P.op(P.act, lambda e: e.activation(out=W5[:, :, j, :], in_=wst[:, :, :], func=AF.Copy), reads=[wst], writes=[W5])

            def proj_fm(j, post):
                for ci, (n0, nn) in enumerate(NCH):
                    pq = G.ps[ci % 2]
                    P.mm([(lambda e, k=k: e.matmul(pq[:, 0:nn], lhsT=W5[:, k, j, :], rhs=hT[:, k, n0:n0 + nn], start=(k == 0), stop=(k == KC - 1)))
                          for k in range(KC)], reads=[W5, hT], writes=[pq])
                    post(pq, n0, nn)

            def proj_tm(j, dst, tiles, func):
                for g0 in range(0, len(tiles), 4):
                    ts = tiles[g0:g0 + 4]
                    pq = G.ps[2 + (g0 // 4) % 2]
                    for jj, t in enumerate(ts):
                        P.mm([(lambda e, k=k: e.matmul(pq[:, jj * 128:(jj + 1) * 128], lhsT=hT[:, k, t * 128:(t + 1) * 128], rhs=W5[:, k, j, :],
                                                       start=(k == 0), stop=(k == KC - 1))) for k in range(KC)], reads=[W5, hT], writes=[pq])
                    n = len(ts)
                    P.op(P.act, lambda e: e.activation(out=dst[:, ts[0]:ts[0] + n, :], in_=pq[:, 0:n * 128].rearrange("p (t n) -> p t n", t=n), func=func),
                         reads=[pq], writes=[dst])

            proj_tm(2, vt, list(range(T)), AF.Copy)
            proj_tm(4, gtm, list(range(2, T)), AF.Silu)
            proj_fm(3, lambda pq, n0, nn: P.op(P.act, lambda e: e.activation(out=qs[:, n0:n0 + nn], in_=pq[:, 0:nn], func=AF.Silu), reads=[pq], writes=[qs]))
            for d in range(2):
                order = fwd if d == 0 else bwd
                proj_fm(d, lambda pq, n0, nn: P.op(P.act, lambda e: e.activation(out=lf[:, n0:n0 + nn], in_=pq[:, 0:nn], func=AF.Sigmoid), reads=[pq], writes=[lf]))
                P.op(P.dve, lambda e: e.tensor_scalar(out=lf[:, :], in0=lf[:, :], scalar1=omlT[:, h:h + 1], scalar2=lbT[:, h:h + 1], op0=ALU.mult, op1=ALU.add),
                     reads=[lf, omlT, lbT], writes=[lf])
                P.op(P.pool, lambda e: e.tensor_scalar(out=kk[:, :], in0=lf[:, :], scalar1=-1.0, scalar2=1.0, op0=ALU.mult, op1=ALU.add), reads=[lf], writes=[kk])
                P.op(P.act, lambda e: e.activation(out=lf[:, :], in_=lf[:, :], func=AF.Ln), reads=[lf], writes=[lf])
                P.op(P.dve, lambda e: e.tensor_tensor_scan(out=BX[:, :], data0=ones[:, :], data1=lf[:, :], initial=0.0, op0=ALU.mult, op1=ALU.add),
                     reads=[ones, lf], writes=[BX])
                if d == 0:
                    P.op(P.dve, lambda e: e.memset(sc[:, 0, 0:1], 0.0), writes=[sc])
                    P.op(P.dve, lambda e: e.tensor_scalar(out=sc[:, 0, 1:T], in0=BX3[:, 0:T - 1, 127], scalar1=-1.0, scalar2=None, op0=ALU.mult), reads=[BX], writes=[sc])
                    edge = 127
                else:
                    P.op(P.dve, lambda e: e.tensor_copy(sc[:, 0, :], BX3[:, :, 127]), reads=[BX], writes=[sc])
                    P.op(P.dve, lambda e: e.tensor_tensor(out=BX[:, :], in0=lf[:, :], in1=BX[:, :], op=ALU.subtract), reads=[lf, BX], writes=[BX])
                    edge = 0
                P.op(P.dve, lambda e: e.tensor_tensor(out=sc[:, 1, :], in0=sc[:, 0, :], in1=BX3[:, :, 64], op=ALU.add), reads=[sc, BX], writes=[sc])
                P.op(P.dve, lambda e: e.tensor_tensor(out=sc[:, 2, :], in0=sc[:, 0, :], in1=BX3[:, :, edge], op=ALU.add), reads=[sc, BX], writes=[sc])
                P.op(P.dve, lambda e: e.tensor_tensor(out=sc[:, 3, :], in0=BX3[:, :, edge], in1=BX3[:, :, 64], op=ALU.subtract), reads=[sc, BX], writes=[sc])
                P.op(P.act, lambda e: e.activation(out=sc[:, 1:4, :], in_=sc[:, 1:4, :], func=AF.Exp), reads=[sc], writes=[sc])
                P.op(P.dve, lambda e: e.tensor_tensor(out=lf3, in0=BX3, in1=BX3[:, :, 64:65].to_broadcast([128, T, 128]), op=ALU.subtract), reads=[BX], writes=[lf])
                P.op(P.act, lambda e: e.activation(out=BX[:, :], in_=lf[:, :], func=AF.Exp), reads=[lf], writes=[BX])
                P.op(P.dve, lambda e: e.tensor_tensor(out=qe[:, :], in0=qs[:, :], in1=BX[:, :], op=ALU.mult), reads=[qs, BX], writes=[qe])
                P.op(P.act, lambda e: e.activation(out=BX[:, :], in_=lf[:, :], func=AF.Exp, scale=-1.0), reads=[lf, qe], writes=[BX])
                P.op(P.dve, lambda e: e.tensor_tensor(out=ke[:, :], in0=kk[:, :], in1=BX[:, :], op=ALU.mult), reads=[kk, BX], writes=[ke])
                mcol = C_MGE if d == 0 else C_MLE
                P.op(P.dve, lambda e: e.memset(Sst[:, :], 0.0), writes=[Sst])
                for i, c in enumerate(order):
                    kt, at, ssc = keT[i % 2], AT[i % 2], Ssc[i % 2]
                    cs_ = slice(c * 128, (c + 1) * 128)
                    pt = G.ps[4 + i % 2]
                    ptb = pt[:, 0:64].bitcast(BF16)
                    P.mm([lambda e: e.transpose(ptb[:, 0:128], ke[:, cs_], G.idb[:, :])], reads=[ke, G.idb], writes=[pt])
                    P.op(P.act, lambda e: e.activation(out=kt[:, :], in_=ptb[:, 0:128], func=AF.Copy), reads=[pt], writes=[kt])
                    if c >= 2:
                        pS, pO = G.ps[6], G.ps[7]
                        P.mm([lambda e: e.matmul(pS[:, 0:128], lhsT=ke[:, cs_], rhs=qe[:, cs_], start=True, stop=True)], reads=[ke, qe], writes=[pS])
                        P.op(P.dve, lambda e: e.tensor_tensor(out=at[:, :], in0=pS[:, 0:128], in1=cst[:, mcol:mcol + 128], op=ALU.mult), reads=[pS, cst], writes=[at])
                        P.op(P.act, lambda e: e.activation(out=ssc[:, :], in_=Sst[:, :], func=AF.Copy, scale=sc[:, 1, c:c + 1]), reads=[Sst, sc], writes=[ssc])
                        P.mm([lambda e: e.matmul(pO[:, 0:128], lhsT=at[:, :], rhs=vt[:, c, :], start=True, stop=False),
                              lambda e: e.matmul(pO[:, 0:128], lhsT=qe[:, cs_], rhs=ssc[:, :], start=False, stop=True)],
                             reads=[at, vt, qe, ssc], writes=[pO])
                        if d == 0:
                            P.op(P.act, lambda e: e.activation(out=O[:, c, :], in_=pO[:, 0:128], func=AF.Copy), reads=[pO], writes=[O])
                        else:
                            P.op(P.dve, lambda e: e.tensor_tensor(out=O[:, c, :], in0=O[:, c, :], in1=pO[:, 0:128], op=ALU.add), reads=[pO, O], writes=[O])
                    if i < T - 1:
                        pU = G.ps[2 + i % 2]
                        P.mm([lambda e: e.matmul(pU[:, 0:128], lhsT=kt[:, :], rhs=vt[:, c, :], start=True, stop=True)], reads=[kt, vt], writes=[pU])
                        P.op(P.dve, lambda e: e.tensor_scalar(out=Sst[:, :], in0=Sst[:, :], scalar1=sc[:, 2, c:c + 1], scalar2=None, op0=ALU.mult),
                             reads=[Sst, sc], writes=[Sst])
                        P.op(P.dve, lambda e: e.scalar_tensor_tensor(out=Sst[:, :], in0=pU[:, 0:128], scalar=sc[:, 3, c:c + 1], in1=Sst[:, :],
                                                                     op0=ALU.mult, op1=ALU.add), reads=[pU, sc, Sst], writes=[Sst])
            Ox = O.t[:, 2:T, :]
            lfx = lf.t[:, :].rearrange("p (c n) -> p c n", n=128)[:, 2:T, :]
            P.op(P.act, lambda e: e.activation(out=lfx, in_=Ox, func=AF.Square), reads=[O], writes=[lf])
            P.op(P.dve, lambda e: e.tensor_reduce(out=st2[:, 2:T], in_=lfx, axis=AX.X, op=ALU.add), reads=[lf], writes=[st2])
            P.op(P.act, lambda e: e.activation(out=st2[:, 2:T], in_=st2[:, 2:T], func=AF.Sqrt, scale=1.0 / 128, bias=cst[:, C_COL + 2:C_COL + 3]),
                 reads=[st2, cst], writes=[st2])
            P.op(P.dve, lambda e: e.reciprocal(st2[:, 2:T], st2[:, 2:T]), reads=[st2], writes=[st2])
            P.op(P.dve, lambda e: e.tensor_tensor(out=Ox, in0=Ox, in1=st2[:, 2:T].unsqueeze(2).to_broadcast([128, T - 2, 128]), op=ALU.mult), reads=[O, st2], writes=[O])
            P.op(P.dve, lambda e: e.tensor_tensor(out=Ox, in0=Ox, in1=gnb[:, h * 128:(h + 1) * 128].unsqueeze(1).to_broadcast([128, T - 2, 128]), op=ALU.mult),
                 reads=[O, gnb], writes=[O])
            P.op(P.dve, lambda e: e.tensor_tensor(out=ob[:, 2:T, :], in0=Ox, in1=gtm[:, 2:T, :], op=ALU.mult), reads=[O, gtm], writes=[ob])
            transpose_out(G, ob, oT, S["mixT"], R["mixT"], h, list(range(2, T)))


def phase_final(G):
    P, I, S, R = G.P, G.I, G.S, G.res
    with ExitStack() as es:
        gb = bc_row(G, es, "fgb", I["norm_final"][0, :], D)
        xt = [sb(G, es, "fxt", [128, D], F32) for _ in range(2)]
        yo = [sb(G, es, "fyo", [128, D], F32) for _ in range(2)]
        junk = sb(G, es, "fjunk", [128, D], BF16)
        ss = sb(G, es, "fss", [128, 4], F32)
        rs = sb(G, es, "frs", [128, 4], F32)
        for i in range(16):
            x, y = xt[i % 2], yo[i % 2]
            P.dma(x[:, :], S["xres"][(i + 2) * 128:(i + 3) * 128, :], reads=[R["xres"]], writes=[x])
            P.op(P.act, lambda e: e.activation(out=junk[:, :], in_=x[:, :], func=AF.Square, accum_out=ss[:, 0:1]), reads=[x], writes=[junk, ss])
            rstd_from_ss(G, ss, rs, 1, 1.0 / D)
            P.op(P.dve, lambda e: e.scalar_tensor_tensor(out=y[:, :], in0=x[:, :], scalar=rs[:, 0:1], in1=gb[:, :], op0=ALU.mult, op1=ALU.mult),
                 reads=[x, rs, gb], writes=[y])
            P.dma(G.out[i * 128:(i + 1) * 128, :], y[:, :], reads=[y], writes=[G.out_res], E=P.act)


_PROG = {}


def _inputs_for_core(inp, b, consts, cossin):
    f = lambda a: np.ascontiguousarray(a, dtype=np.float32)
    return {
        "x": f(inp["x"][b]), "ctx": f(inp["ctx"][b]), "c": f(inp["c"][b:b + 1]), "c_ctx": f(inp["c_ctx"][None, :]),
        "ada_w": f(inp["ada_w"]), "ada_b": f(inp["ada_b"]), "norm_mix": f(inp["norm_mix"]), "norm_ffn": f(inp["norm_ffn"]),
        "ab_w_in": f(inp["ab_w_in"][0]), "ab_w_out": f(inp["ab_w_out"][0]), "a_q_norm": f(inp["a_q_norm"]),
        "a_k_norm": f(inp["a_k_norm"]), "b_decay_exp": f(inp["b_decay_exp"].reshape(1, 16)), "b_gn": f(inp["b_gn"]),
        "c_w_in": f(inp["c_w_in"][0]), "c_w_out": f(inp["c_w_out"][0]), "c_lb": f(inp["c_lb"]), "c_gn": f(inp["c_gn"]),
        "router_w": f(inp["router_w"]), "router_b": f(inp["router_b"]),
        "w1t0": inp["_w1"][0], "w1t1": inp["_w1"][1], "w2t0": inp["_w2"][0], "w2t1": inp["_w2"][1],
        "exp_b1": f(inp["exp_b1"]), "exp_b2": f(inp["exp_b2"]),
        "norm_final": f(inp["norm_final"][None, :]), "consts": consts, "cossin": cossin,
    }


def prep_weights(inp):
    w1, w2 = inp["exp_w1"], inp["exp_w2"]
    if w1 is None:
        z = np.zeros((1, 1), np.float32)
        inp["_w1"] = [z, z]
        inp["_w2"] = [z, z]
        return
    inp["_w1"], inp["_w2"] = [], []
    for l in range(2):
        a = np.asarray(w1[l], dtype=np.float32).reshape(NE, 2, 8, 128, 16, 128, 2)
        inp["_w1"].append(np.ascontiguousarray(a.transpose(0, 4, 3, 1, 2, 6, 5)).reshape(NE * 16 * 128 * 2, 2048))
        b_ = np.asarray(w2[l], dtype=np.float32).reshape(NE, 2, 8, 128, 8, 256)
        inp["_w2"].append(np.ascontiguousarray(b_.transpose(0, 4, 3, 1, 2, 5)).reshape(NE * 8 * 128 * 2, 2048))


def kernel(**inputs):
    inp = {k: np.asarray(v) for k, v in inputs.items()}
    prep_weights(inp)
    consts, cossin = make_consts(), make_cossin()
    if "nc" not in _PROG:
        _PROG["nc"] = build_program()
    nc = _PROG["nc"]
    in_maps = [_inputs_for_core(inp, b, consts, cossin) for b in range(N_CORES)]
    res = run_bass_kernel_spmd(nc, in_maps, core_ids=list(range(N_CORES)))
    return np.stack([np.asarray(res.results[b]["out"], dtype=np.float32) for b in range(N_CORES)], axis=0)
```

```python
import math
from contextlib import ExitStack

import numpy as np
import ml_dtypes
import concourse.bass as bass
import concourse.mybir as mybir
from concourse.bass_utils import run_bass_kernel_spmd

F32 = mybir.dt.float32
BF16 = mybir.dt.bfloat16
AF = mybir.ActivationFunctionType
ALU = mybir.AluOpType
AX = mybir.AxisListType

D = 2048
KC = 16
NCTX = 256
NX = 2048
NT = NCTX + NX
T = NT // 128
NE = 32
CAP = 384
EPS = 1e-6
N_CORES = 8

C_ID = 0
C_IOTA = 128
C_US = 512
C_RGE = 640
C_RLE = 768
C_MGE = 896
C_MLE = 1024
C_JP1 = 1152
C_CMJ = 1280
C_COL = 1408
C_ONE = 1412
C_OFF1 = 1540
C_OFF2 = 1572
CW = 1588


def make_consts():
    c = np.zeros((128, CW), np.float32)
    p = np.arange(128)[:, None].astype(np.float32)
    j = np.arange(128)[None, :].astype(np.float32)
    c[:, C_ID:C_ID + 128] = np.eye(128)
    c[:, C_IOTA:C_IOTA + 384] = np.arange(384)[None, :]
    c[:, C_US:C_US + 128] = (p < j)
    c[:, C_RGE:C_RGE + 128] = np.maximum(j - p, 0)
    c[:, C_RLE:C_RLE + 128] = np.maximum(p - j, 0)
    c[:, C_MGE:C_MGE + 128] = (j >= p)
    c[:, C_MLE:C_MLE + 128] = (j <= p)
    c[:, C_JP1:C_JP1 + 128] = j + 1
    c[:, C_CMJ:C_CMJ + 128] = 128 - j
    c[:, C_COL] = 127 - p[:, 0]
    c[:, C_COL + 1] = p[:, 0]
    c[:, C_COL + 2] = EPS
    c[:, C_COL + 3] = 1.0
    c[:, C_ONE:C_ONE + 128] = 1.0
    jj = np.arange(32)
    c[:, C_OFF1:C_OFF1 + 32] = 2 * p + (jj // 2) * 256 + jj % 2
    c[:, C_OFF2:C_OFF2 + 16] = 2 * p + (jj[:16] // 2) * 256 + jj[:16] % 2
    return c


def make_cossin():
    gw, hd = 64, 128
    n = NX
    row = np.repeat(np.arange(n // gw, dtype=np.float32), gw)
    col = np.tile(np.arange(gw, dtype=np.float32), n // gw)
    nf = hd // 4
    inv = (np.float32(10000.0) ** (-np.arange(nf, dtype=np.float32) / nf)).astype(np.float32)
    ang = np.concatenate([row[:, None] * inv, col[:, None] * inv], axis=-1).astype(np.float32)
    return np.concatenate([np.cos(ang), np.sin(ang)], axis=-1).astype(np.float32)


class Res:
    __slots__ = ("w", "r")

    def __init__(self):
        self.w = None
        self.r = []


class TL:
    def __init__(self, t):
        self.t = t
        self.r = Res()

    def __getitem__(self, k):
        return self.t[k]


class Eng:
    def __init__(self, e, name, sem):
        self.e = e
        self.name = name
        self.sem = sem
        self.n = 0
        self.seen = {}


def _res(x):
    return x if isinstance(x, Res) else x.r


class Prog:
    def __init__(self, nc, es):
        self.nc = nc

        def mk(e, name):
            return Eng(e, name, es.enter_context(nc.semaphore("s_" + name)))

        self.pe = mk(nc.tensor, "pe")
        self.dve = mk(nc.vector, "dve")
        self.act = mk(nc.scalar, "act")
        self.pool = mk(nc.gpsimd, "pool")
        self.sp = mk(nc.sync, "sp")
        self.engs = [self.pe, self.dve, self.act, self.pool, self.sp]
        self.NQ = 8
        self.dq = {}
        for E in (self.sp, self.act, self.pool):
            self.dq[E.name] = dict(
                sems=[es.enter_context(nc.semaphore("d_%s%d" % (E.name, i))) for i in range(self.NQ)], i=0)
        self.rr = 0

    def _wait(self, E, ev):
        sem, val = ev
        k = id(sem)
        if E.seen.get(k, 0) < val:
            E.e.wait_ge(sem, val)
            E.seen[k] = val

    def _deps(self, E, reads, writes):
        for b in reads:
            b = _res(b)
            if b.w is not None:
                self._wait(E, b.w)
        for b in writes:
            b = _res(b)
            if b.w is not None:
                self._wait(E, b.w)
            for ev in b.r:
                self._wait(E, ev)

    def _commit(self, ev, reads, writes):
        for b in reads:
            _res(b).r.append(ev)
        for b in writes:
            b = _res(b)
            b.w = ev
            b.r = []

    def op(self, E, fn, reads=(), writes=()):
        self._deps(E, reads, writes)
        ins = fn(E.e)
        E.n += 1
        ins.then_inc(E.sem, 1)
        ev = (E.sem, E.n)
        self._commit(ev, reads, writes)
        return ev

    def mm(self, fns, reads=(), writes=()):
        E = self.pe
        self._deps(E, reads, writes)
        ins = None
        for fn in fns:
            ins = fn(E.e)
        E.n += 1
        ins.then_inc(E.sem, 1)
        ev = (E.sem, E.n)
        self._commit(ev, reads, writes)
        return ev

    def dma(self, out, in_, reads=(), writes=(), E=None, fn=None, **kw):
        if E is None:
            E = self.sp
        q = self.dq[E.name]
        i = q["i"]
        q["i"] += 1
        sem = q["sems"][i % self.NQ]
        prev = 16 * (i // self.NQ)
        if prev:
            self._wait(E, (sem, prev))
        self._deps(E, reads, writes)
        if fn is not None:
            fn(E.e).then_inc(sem, 16)
        else:
            E.e.dma_start(out=out, in_=in_, **kw).then_inc(sem, 16)
        ev = (sem, prev + 16)
        self._commit(ev, reads, writes)
        return ev

    def barrier(self):
        evs = [(E.sem, E.n) for E in self.engs if E.n > 0]
        for q in self.dq.values():
            for k, sem in enumerate(q["sems"]):
                uses = (q["i"] - k + self.NQ - 1) // self.NQ
                if uses > 0:
                    evs.append((sem, 16 * uses))
        for E in self.engs:
            for ev in evs:
                if ev[0] is not E.sem:
                    self._wait(E, ev)


class Ctx:
    pass


def build_program(debug=None, stop_after=None, dummy=()):
    debug = debug or []
    nc = bass.Bass("TRN2", target_bir_lowering=False)
    G = Ctx()
    G.nc = nc
    G.debug = debug

    def din(name, shape, dt=F32):
        if name in dummy or ("exp_w1" in dummy and name[:3] in ("w1t", "w2t")):
            shape = [1] * len(shape)
        return nc.dram_tensor(name, list(shape), dt, kind="ExternalInput").ap()

    I = {}
    I["x"] = din("x", [NX, D])
    I["ctx"] = din("ctx", [NCTX, D])
    I["c"] = din("c", [1, D])
    I["c_ctx"] = din("c_ctx", [1, D])
    I["ada_w"] = din("ada_w", [2, D, 6 * D])
    I["ada_b"] = din("ada_b", [2, 6 * D])
    I["norm_mix"] = din("norm_mix", [2, D])
    I["norm_ffn"] = din("norm_ffn", [2, D])
    I["ab_w_in"] = din("ab_w_in", [D, 5632])
    I["ab_w_out"] = din("ab_w_out", [D, D])
    I["a_q_norm"] = din("a_q_norm", [1, 128])
    I["a_k_norm"] = din("a_k_norm", [1, 128])
    I["b_decay_exp"] = din("b_decay_exp", [1, 16])
    I["b_gn"] = din("b_gn", [1, 1024])
    I["c_w_in"] = din("c_w_in", [D, 10240])
    I["c_w_out"] = din("c_w_out", [D, D])
    I["c_lb"] = din("c_lb", [2, D])
    I["c_gn"] = din("c_gn", [1, D])
    I["router_w"] = din("router_w", [2, D, NE])
    I["router_b"] = din("router_b", [2, NE])
    for l_ in range(2):
        I["w1t%d" % l_] = din("w1t%d" % l_, [NE * 16 * 128 * 2, 2048])
        I["w2t%d" % l_] = din("w2t%d" % l_, [NE * 8 * 128 * 2, 2048])
    I["exp_b1"] = din("exp_b1", [2, NE, 2 * D])
    I["exp_b2"] = din("exp_b2", [2, NE, D])
    I["norm_final"] = din("norm_final", [1, D])
    I["consts"] = din("consts", [128, CW])
    I["cossin"] = din("cossin", [NX, 128])
    G.I = I
    G.out = nc.dram_tensor("out", [NX, D], F32, kind="ExternalOutput").ap()

    def dscr(name, shape, dt):
        kind = "ExternalOutput" if name in debug else "Internal"
        return nc.dram_tensor(name, list(shape), dt, kind=kind).ap()

    S = {}
    S["xres"] = dscr("xres", [NT, D], F32)
    S["modD"] = dscr("modD", [2, 2, 6 * D], F32)
    S["QTa"] = dscr("QTa", [8, 128, NT], BF16)
    S["KTa"] = dscr("KTa", [2, 128, NT], BF16)
    S["Va"] = dscr("Va", [NT, 256], BF16)
    S["QTb"] = dscr("QTb", [8, 128, NT], BF16)
    S["KTb"] = dscr("KTb", [8, 128, NT], BF16)
    S["Kb"] = dscr("Kb", [NT, 1024], BF16)
    S["Vb"] = dscr("Vb", [NT, 1024], BF16)
    S["Gt"] = dscr("Gt", [NT, 2048], BF16)
    S["mixT"] = dscr("mixT", [16, 128, NT], BF16)
    S["outE"] = dscr("outE", [68, 256, D], BF16)
    S["hTd"] = dscr("hTd", [KC, 128, NT], BF16)
    if "dbg_h2" in debug:
        S["dbg_h2"] = dscr("dbg_h2", [NT, D], BF16)
        S["dbg_route"] = dscr("dbg_route", [3, 128, T, NE], F32)
    G.S = S
    G.res = {k: Res() for k in S}
    G.out_res = Res()

    with ExitStack() as es:
        P = Prog(nc, es)
        G.P = P
        G.ps = [TL(es.enter_context(nc.psum_tensor("ps%d" % i, [128, 512], F32))) for i in range(8)]
        G.cst = TL(es.enter_context(nc.sbuf_tensor("cst", [128, CW], F32)))
        G.idb = TL(es.enter_context(nc.sbuf_tensor("idb", [128, 128], BF16)))
        G.oneb = TL(es.enter_context(nc.sbuf_tensor("oneb", [128, 128], BF16)))
        es.enter_context(nc.Block())
        P.dma(G.cst[:, :], I["consts"][:, :], writes=[G.cst])
        P.op(P.dve, lambda e: e.tensor_copy(G.idb[:, :], G.cst[:, C_ID:C_ID + 128]), reads=[G.cst], writes=[G.idb])
        P.op(P.dve, lambda e: e.tensor_copy(G.oneb[:, :], G.cst[:, C_ONE:C_ONE + 128]), reads=[G.cst], writes=[G.oneb])

        from_phases(G, stop_after)

        P.barrier()
    return nc


class V:
    def __init__(s, ap, r):
        s.ap = ap
        s.r = r

    def __getitem__(s, k):
        return s.ap[k]


class Stop(Exception):
    pass


def from_phases(G, stop_after):
    P = G.P
    G.stop = False
    with ExitStack() as es0:
        bigA = sb(G, es0, "bigA", [128, KC * NT], BF16)
        hT = V(bigA.t[:, :].rearrange("p (k n) -> p k n", k=KC), bigA.r)
        h2 = V(bigA.t[:, :].rearrange("p (t n) -> p t n", t=T), bigA.r)

        def ph(name, fn):
            if G.stop:
                return
            fn()
            P.barrier()
            if "hTd" in G.debug and name in ("norm0", "norm1"):
                P.dma(G.S["hTd"].rearrange("k p n -> p k n"), hT[:, :, :], reads=[hT], writes=[G.res["hTd"]])
                P.barrier()
            if stop_after == name:
                G.stop = True

        for l in range(2):
            ph("mods%d" % l, lambda: phase_mods(G, l))
        for l in range(2):
            tiles = list(range(T)) if l == 0 else list(range(2, T))
            ph("norm%d" % l, lambda: phase_norm(G, l, 1, list(range(T)), hT=hT))
            if l == 0:
                ph("inproj0", lambda: phase_inproj0(G, hT))
                ph("attn", lambda: phase_attn(G))
                ph("ret", lambda: phase_ret(G))
            else:
                ph("hgrn", lambda: phase_hgrn(G, hT))
            ph("outproj%d" % l, lambda: phase_outproj(G, l, hT, tiles))
            if G.stop:
                break
            with ExitStack() as esr:
                route = dict(mask=sb(G, esr, "rmask", [128, T, NE], F32), gate=sb(G, esr, "rgate", [128, T, NE], F32),
                             pos=sb(G, esr, "rpos", [128, T, NE], F32), dsel=sb(G, esr, "rdsel", [128, 68, T], F32),
                             gsel=sb(G, esr, "rgsel", [128, 68, T], F32))
                ph("norm%db" % l, lambda: phase_norm(G, l, 2, tiles, h2=h2, route=route))
                ph("moe%d" % l, lambda: phase_moe(G, l, tiles, h2, route, bigA))
        ph("final", lambda: phase_final(G))


_UID = [0]


def sb(G, es, name, shape, dt):
    _UID[0] += 1
    return TL(es.enter_context(G.nc.sbuf_tensor("%s_%d" % (name, _UID[0]), list(shape), dt)))


def phase_mods(G, l):
    P, I, S = G.P, G.I, G.S
    with ExitStack() as es:
        cin = sb(G, es, "cin", [32, 128], F32)
        cT = sb(G, es, "cT", [128, KC, 2], F32)
        wst = [sb(G, es, "mw%d" % i, [128, KC, 512], F32) for i in range(2)]
        bia = [sb(G, es, "mb%d" % i, [2, 512], F32) for i in range(2)]
        mro = [sb(G, es, "mr%d" % i, [2, 512], F32) for i in range(2)]
        P.dma(cin[0:16, :], I["c"].rearrange("o (k p) -> (o k) p", p=128), writes=[cin])
        P.dma(cin[16:32, :], I["c_ctx"].rearrange("o (k p) -> (o k) p", p=128), writes=[cin])
        P.op(P.act, lambda e: e.activation(out=cin[:, :], in_=cin[:, :], func=AF.Silu), reads=[cin], writes=[cin])
        ps = G.ps[0]
        P.mm([lambda e: e.transpose(ps[:, 0:32], cin[:, :], G.cst[0:32, C_ID:C_ID + 32])],
             reads=[cin, G.cst], writes=[ps])
        P.op(P.dve, lambda e: e.tensor_copy(cT[:, :, :].rearrange("p k o -> p o k"),
                                            ps[:, 0:32].rearrange("p (o k) -> p o k", o=2)),
             reads=[ps], writes=[cT])
        for g in range(24):
            w = wst[g % 2]
            b = bia[g % 2]
            m = mro[g % 2]
            pq = G.ps[1 + g % 2]
            P.dma(w[:, :, :], I["ada_w"][l, :, g * 512:(g + 1) * 512].rearrange("(k p) n -> p k n", p=128), writes=[w])
            P.dma(b[:, :], I["ada_b"][l, g * 512:(g + 1) * 512].partition_broadcast(2), writes=[b], E=P.act)
            P.mm([(lambda e, k=k: e.matmul(pq[0:2, :], lhsT=cT[:, k, :], rhs=w[:, k, :], start=(k == 0), stop=(k == KC - 1)))
                  for k in range(KC)], reads=[cT, w], writes=[pq])
            P.op(P.dve, lambda e: e.tensor_tensor(out=m[:, :], in0=pq[0:2, :], in1=b[:, :], op=ALU.add),
                 reads=[pq, b], writes=[m])
            P.dma(S["modD"][l, :, g * 512:(g + 1) * 512], m[:, :], reads=[m], writes=[G.res["modD"]], E=P.pool)


def bc_row(G, es, name, row_ap, n, E=None):
    t = sb(G, es, name, [128, n], F32)
    G.P.dma(t[:, :], row_ap.partition_broadcast(128), writes=[t], E=E)
    return t


def src_rows(G, l, which, t):
    I, S = G.I, G.S
    if l == 0 and which == 1:
        return (I["ctx"][t * 128:(t + 1) * 128, :] if t < 2 else I["x"][(t - 2) * 128:(t - 1) * 128, :]), None
    return S["xres"][t * 128:(t + 1) * 128, :], G.res["xres"]


def rstd_from_ss(G, ss, rs, n, inv_n):
    P = G.P
    P.op(P.act, lambda e: e.activation(out=rs[:, 0:n], in_=ss[:, 0:n], func=AF.Sqrt, scale=inv_n,
                                       bias=G.cst[:, C_COL + 2:C_COL + 3]), reads=[ss, G.cst], writes=[rs])
    P.op(P.dve, lambda e: e.reciprocal(rs[:, 0:n], rs[:, 0:n]), reads=[rs], writes=[rs])


def phase_norm(G, l, which, tiles, hT=None, h2=None, route=None):
    P, I, S = G.P, G.I, G.S
    gain = I["norm_mix"] if which == 1 else I["norm_ffn"]
    m0 = 0 if which == 1 else 3
    with ExitStack() as es:
        gb = bc_row(G, es, "gb", gain[l, :], D)
        Gm, Sh = [], []
        for r in range(2):
            sc = bc_row(G, es, "sc", S["modD"][l, r, (m0 + 1) * D:(m0 + 2) * D], D, E=P.act)
            P.op(P.dve, lambda e, sc=sc: e.scalar_tensor_tensor(out=sc[:, :], in0=sc[:, :], scalar=1.0, in1=gb[:, :],
                                                                op0=ALU.add, op1=ALU.mult), reads=[sc, gb], writes=[sc])
            Gm.append(sc)
            Sh.append(bc_row(G, es, "sh", S["modD"][l, r, m0 * D:(m0 + 1) * D], D, E=P.act))
        xt = [sb(G, es, "xt", [128, D], F32) for _ in range(2)]
        junk = sb(G, es, "junk", [128, D], BF16)
        hf = sb(G, es, "hf", [128, D], F32)
        hb = [sb(G, es, "hb", [128, D], BF16) for _ in range(2)]
        ss = sb(G, es, "ss", [128, 4], F32)
        rs = sb(G, es, "rs", [128, 4], F32)
        if which == 2:
            rw = sb(G, es, "rw", [128, KC, NE], F32)
            P.dma(rw[:, :, :], I["router_w"][l].rearrange("(k p) n -> p k n", p=128), writes=[rw])
            rb = bc_row(G, es, "rb", I["router_b"][l, :], NE)
            hTf = sb(G, es, "hTf", [128, KC, 128], F32)
            lg = sb(G, es, "lg", [128, NE], F32)
            top = sb(G, es, "top", [128, 8], F32)
            nm = sb(G, es, "nm", [128, 1], F32)
            ex = sb(G, es, "ex", [128, NE], F32)
            se = sb(G, es, "se", [128, 1], F32)
        for i, t in enumerate(tiles):
            x = xt[i % 2]
            src, sres = src_rows(G, l, which, t)
            P.dma(x[:, :], src, reads=[sres] if sres else [], writes=[x])
            P.op(P.act, lambda e: e.activation(out=junk[:, :], in_=x[:, :], func=AF.Square, accum_out=ss[:, 0:1]),
                 reads=[x], writes=[junk, ss])
            rstd_from_ss(G, ss, rs, 1, 1.0 / D)
            r = 1 if t < 2 else 0
            P.op(P.dve, lambda e: e.scalar_tensor_tensor(out=hf[:, :], in0=x[:, :], scalar=rs[:, 0:1], in1=Gm[r][:, :],
                                                         op0=ALU.mult, op1=ALU.mult), reads=[x, rs, Gm[r]], writes=[hf])
            if which == 1:
                h = hb[i % 2]
                P.op(P.pool, lambda e: e.tensor_tensor(out=h[:, :], in0=hf[:, :], in1=Sh[r][:, :], op=ALU.add),
                     reads=[hf, Sh[r]], writes=[h])
                for half in range(2):
                    pq = G.ps[(2 * i + half) % 4]
                    pv = pq[:, 0:512].bitcast(BF16)
                    P.mm([(lambda e, k=k: e.transpose(pv[:, (k % 8) * 128:(k % 8 + 1) * 128], h[:, k * 128:(k + 1) * 128], G.idb[:, :]))
                          for k in range(half * 8, half * 8 + 8)], reads=[h, G.idb], writes=[pq])
                    eng = P.act if half == 0 else P.dve
                    P.op(eng, lambda e: e.tensor_copy(hT[:, half * 8:half * 8 + 8, t * 128:(t + 1) * 128],
                                                      pv.rearrange("p (k n) -> p k n", k=8)) if eng is P.dve else
                         e.activation(out=hT[:, half * 8:half * 8 + 8, t * 128:(t + 1) * 128],
                                      in_=pv.rearrange("p (k n) -> p k n", k=8), func=AF.Copy),
                         reads=[pq], writes=[hT])
            else:
                P.op(P.dve, lambda e: e.tensor_tensor(out=hf[:, :], in0=hf[:, :], in1=Sh[r][:, :], op=ALU.add),
                     reads=[hf, Sh[r]], writes=[hf])
                P.op(P.act, lambda e: e.activation(out=h2[:, t, :], in_=hf[:, :], func=AF.Copy), reads=[hf], writes=[h2])
                for q4 in range(4):
                    pq = G.ps[q4]
                    P.mm([(lambda e, k=k: e.transpose(pq[:, (k % 4) * 128:(k % 4 + 1) * 128], hf[:, k * 128:(k + 1) * 128],
                                                      G.cst[:, C_ID:C_ID + 128])) for k in range(q4 * 4, q4 * 4 + 4)],
                         reads=[hf, G.cst], writes=[pq])
                    P.op(P.act if q4 % 2 else P.dve,
                         (lambda e: e.activation(out=hTf[:, q4 * 4:q4 * 4 + 4, :], in_=pq[:, :].rearrange("p (k n) -> p k n", k=4), func=AF.Copy))
                         if q4 % 2 else
                         (lambda e: e.tensor_copy(hTf[:, q4 * 4:q4 * 4 + 4, :], pq[:, :].rearrange("p (k n) -> p k n", k=4))),
                         reads=[pq], writes=[hTf])
                pl = G.ps[4]
                P.mm([(lambda e, k=k: e.matmul(pl[:, 0:NE], lhsT=hTf[:, k, :], rhs=rw[:, k, :], start=(k == 0), stop=(k == KC - 1)))
                      for k in range(KC)], reads=[hTf, rw], writes=[pl])
                P.op(P.dve, lambda e: e.tensor_tensor(out=lg[:, :], in0=pl[:, 0:NE], in1=rb[:, :], op=ALU.add),
                     reads=[pl, rb], writes=[lg])
                P.op(P.dve, lambda e: e.max(top[:, :], lg[:, :]), reads=[lg], writes=[top])
                mk, gt = route["mask"], route["gate"]
                P.op(P.dve, lambda e: e.tensor_scalar(out=mk[:, t, :], in0=lg[:, :], scalar1=top[:, 3:4], scalar2=None, op0=ALU.is_ge),
                     reads=[lg, top], writes=[mk])
                P.op(P.dve, lambda e: e.tensor_scalar(out=nm[:, :], in0=top[:, 0:1], scalar1=-1.0, scalar2=None, op0=ALU.mult),
                     reads=[top], writes=[nm])
                P.op(P.act, lambda e: e.activation(out=ex[:, :], in_=lg[:, :], func=AF.Exp, bias=nm[:, 0:1]),
                     reads=[lg, nm], writes=[ex])
                P.op(P.dve, lambda e: e.tensor_tensor(out=ex[:, :], in0=ex[:, :], in1=mk[:, t, :], op=ALU.mult),
                     reads=[ex, mk], writes=[ex])
                P.op(P.dve, lambda e: e.tensor_reduce(out=se[:, :], in_=ex[:, :], axis=AX.X, op=ALU.add), reads=[ex], writes=[se])
                P.op(P.dve, lambda e: e.reciprocal(se[:, :], se[:, :]), reads=[se], writes=[se])
                P.op(P.dve, lambda e: e.tensor_scalar(out=gt[:, t, :], in0=ex[:, :], scalar1=se[:, 0:1], scalar2=None, op0=ALU.mult),
                     reads=[ex, se], writes=[gt])
        if which == 2:
            pos = route["pos"]
            mk = route["mask"]
            for i, t in enumerate(tiles):
                pq = G.ps[5 + i % 2]
                fns = [(lambda e, tp=tp, j=j: e.matmul(pq[:, 0:NE], lhsT=G.cst[:, C_ONE:C_ONE + 128], rhs=mk[:, tp, :],
                                                      start=(j == 0), stop=False)) for j, tp in enumerate(tiles[:i])]
                fns.append(lambda e, i=i, t=t: e.matmul(pq[:, 0:NE], lhsT=G.cst[:, C_US:C_US + 128], rhs=mk[:, t, :],
                                                      start=(i == 0), stop=True))
                P.mm(fns, reads=[mk, G.cst], writes=[pq])
                P.op(P.act, lambda e: e.activation(out=pos[:, t, :], in_=pq[:, 0:NE], func=AF.Copy), reads=[pq], writes=[pos])


def linear_tm(G, es, W, groups, tiles, hT, post):
    P = G.P
    wst = [sb(G, es, "wst", [128, 8, 512], F32) for _ in range(2)]
    wb = [sb(G, es, "wb", [128, KC, 512], BF16) for _ in range(2)]
    cnt = 0
    for gi, (c0, n) in enumerate(groups):
        w = wb[gi % 2]
        for half in range(2):
            st = wst[half]
            P.dma(st[:, :, 0:n], W[half * 1024:(half + 1) * 1024, c0:c0 + n].rearrange("(k p) n -> p k n", p=128),
                  writes=[st], E=(P.sp if half == 0 else P.act))
            if half == 0:
                P.op(P.pool, lambda e: e.tensor_copy(w[:, 0:8, 0:n], st[:, :, 0:n]), reads=[st], writes=[w])
            else:
                P.op(P.act, lambda e: e.activation(out=w[:, 8:16, 0:n], in_=st[:, :, 0:n], func=AF.Copy), reads=[st], writes=[w])
        for t in tiles:
            pq = G.ps[cnt % 3]
            cnt += 1
            P.mm([(lambda e, k=k: e.matmul(pq[:, 0:n], lhsT=hT[:, k, t * 128:(t + 1) * 128], rhs=w[:, k, 0:n],
                                           start=(k == 0), stop=(k == KC - 1))) for k in range(KC)],
                 reads=[hT, w], writes=[pq])
            post(gi, t, pq, n)


def rope_heads(G, src, dst, cs, t, H, tmp):
    P = G.P
    a = src[:, 0:H, 0:64]
    b = src[:, 0:H, 64:128]
    cosb = cs[:, t - 2:t - 1, 0:64].to_broadcast([128, H, 64])
    sinb = cs[:, t - 2:t - 1, 64:128].to_broadcast([128, H, 64])
    t1, t2 = tmp[:, 0:H, 0:64], tmp[:, 0:H, 64:128]
    P.op(P.dve, lambda e: e.tensor_tensor(out=t1, in0=a, in1=cosb, op=ALU.mult), reads=[src, cs], writes=[tmp])
    P.op(P.dve, lambda e: e.tensor_tensor(out=t2, in0=b, in1=sinb, op=ALU.mult), reads=[src, cs], writes=[tmp])
    P.op(P.dve, lambda e: e.tensor_tensor(out=dst[:, 0:H, 0:64], in0=t1, in1=t2, op=ALU.subtract), reads=[tmp], writes=[dst])
    P.op(P.dve, lambda e: e.tensor_tensor(out=t1, in0=a, in1=sinb, op=ALU.mult), reads=[src, cs, dst], writes=[tmp])
    P.op(P.dve, lambda e: e.tensor_tensor(out=t2, in0=b, in1=cosb, op=ALU.mult), reads=[src, cs], writes=[tmp])
    P.op(P.dve, lambda e: e.tensor_tensor(out=dst[:, 0:H, 64:128], in0=t1, in1=t2, op=ALU.add), reads=[tmp], writes=[dst])


def phase_inproj0(G, hT):
    P, I, S = G.P, G.I, G.S
    groups = [(i * 512, 512) for i in range(11)]
    with ExitStack() as es:
        cs = sb(G, es, "cs", [128, 16, 128], F32)
        P.dma(cs[:, :, :], I["cossin"].rearrange("(t p) n -> p t n", p=128), writes=[cs])
        qg = bc_row(G, es, "qg", I["a_q_norm"][0, :], 128)
        kg = bc_row(G, es, "kg", I["a_k_norm"][0, :], 128)
        src = [sb(G, es, "src", [128, 4, 128], F32) for _ in range(2)]
        sq = sb(G, es, "sq", [128, 4, 128], F32)
        tmp = sb(G, es, "tmp", [128, 4, 128], F32)
        ob = [sb(G, es, "ob", [128, 4, 128], BF16) for _ in range(2)]
        oT = [sb(G, es, "oT", [128, 4, 128], BF16) for _ in range(2)]
        ss = sb(G, es, "ss", [128, 4], F32)
        rs = sb(G, es, "rs", [128, 4], F32)
        st = dict(i=0)

        def heads(pq, t, H, c_lo, gain, rope, scale, dT, h0, dTM, tm_c0):
            i = st["i"]
            st["i"] += 1
            s_, o_, oT_ = src[i % 2], ob[i % 2], oT[i % 2]
            pv = pq[:, c_lo:c_lo + H * 128].rearrange("p (h n) -> p h n", h=H)
            P.op(P.act, lambda e: e.activation(out=s_[:, 0:H, :], in_=pv, func=AF.Copy, scale=scale), reads=[pq], writes=[s_])
            if gain is not None:
                P.op(P.act, lambda e: e.activation(out=sq[:, 0:H, :], in_=s_[:, 0:H, :], func=AF.Square), reads=[s_], writes=[sq])
                P.op(P.dve, lambda e: e.tensor_reduce(out=ss[:, 0:H], in_=sq[:, 0:H, :], axis=AX.X, op=ALU.add), reads=[sq], writes=[ss])
                rstd_from_ss(G, ss, rs, H, 1.0 / 128)
                P.op(P.dve, lambda e: e.tensor_tensor(out=s_[:, 0:H, :], in0=s_[:, 0:H, :],
                                                      in1=rs[:, 0:H].unsqueeze(2).to_broadcast([128, H, 128]), op=ALU.mult),
                     reads=[s_, rs], writes=[s_])
                P.op(P.dve, lambda e: e.tensor_tensor(out=s_[:, 0:H, :], in0=s_[:, 0:H, :],
                                                      in1=gain[:, :].unsqueeze(1).to_broadcast([128, H, 128]), op=ALU.mult),
                     reads=[s_, gain], writes=[s_])
            if rope and t >= 2:
                rope_heads(G, s_, o_, cs, t, H, tmp)
            else:
                P.op(P.pool, lambda e: e.tensor_copy(o_[:, 0:H, :], s_[:, 0:H, :]), reads=[s_], writes=[o_])
            if dTM is not None:
                P.dma(dTM[0][t * 128:(t + 1) * 128, tm_c0:tm_c0 + H * 128], o_[:, 0:H, :].rearrange("p h n -> p (h n)"),
                      reads=[o_], writes=[dTM[1]], E=P.pool)
            if dT is not None:
                pt = G.ps[4 + i % 2]
                pvb = pt[:, 0:256].bitcast(BF16)
                P.mm([(lambda e, h=h: e.transpose(pvb[:, h * 128:(h + 1) * 128], o_[:, h, :], G.idb[:, :])) for h in range(H)],
                     reads=[o_, G.idb], writes=[pt])
                P.op(P.act, lambda e: e.activation(out=oT_[:, 0:H, :], in_=pvb[:, 0:H * 128].rearrange("p (h n) -> p h n", h=H), func=AF.Copy),
                     reads=[pt], writes=[oT_])
                P.dma(dT[0][h0:h0 + H, :, t * 128:(t + 1) * 128].rearrange("h d n -> d h n"), oT_[:, 0:H, :],
                      reads=[oT_], writes=[dT[1]], E=P.pool)

        R = G.res
        ksc = 128.0 ** -0.5

        def post(gi, t, pq, n):
            if gi == 0:
                heads(pq, t, 2, 0, kg, True, 1.0, (S["KTa"], R["KTa"]), 0, None, 0)
                heads(pq, t, 2, 256, None, False, 1.0, None, 0, (S["Va"], R["Va"]), 0)
            elif gi in (1, 2):
                heads(pq, t, 4, 0, None, True, ksc, (S["KTb"], R["KTb"]), (gi - 1) * 4, (S["Kb"], R["Kb"]), (gi - 1) * 512)
            elif gi in (3, 4):
                heads(pq, t, 4, 0, None, False, 1.0, None, 0, (S["Vb"], R["Vb"]), (gi - 3) * 512)
            elif gi in (5, 6):
                heads(pq, t, 4, 0, qg, True, 1.0, (S["QTa"], R["QTa"]), (gi - 5) * 4, None, 0)
            elif gi in (7, 8):
                heads(pq, t, 4, 0, None, True, 1.0, (S["QTb"], R["QTb"]), (gi - 7) * 4, None, 0)
            else:
                i = st["i"]
                st["i"] += 1
                o_ = ob[i % 2]
                P.op(P.act, lambda e: e.activation(out=o_[:, :, :], in_=pq[:, 0:512].rearrange("p (h n) -> p h n", h=4), func=AF.Silu),
                     reads=[pq], writes=[o_])
                P.dma(S["Gt"][t * 128:(t + 1) * 128, (gi - 9) * 512:(gi - 8) * 512], o_[:, :, :].rearrange("p h n -> p (h n)"),
                      reads=[o_], writes=[R["Gt"]], E=P.pool)

        linear_tm(G, es, I["ab_w_in"], groups, list(range(T)), hT, post)


def phase_attn(G):
    P, S, R = G.P, G.S, G.res
    sc = 128.0 ** -0.5
    with ExitStack() as es:
        KT = sb(G, es, "KT", [128, NT], BF16)
        V = sb(G, es, "V", [128, T, 128], BF16)
        QT = [sb(G, es, "QT", [128, 512], BF16) for _ in range(2)]
        PT = [sb(G, es, "PT", [128, 512], BF16) for _ in range(3)]
        rz = sb(G, es, "rz", [128, 512], F32)
        OT = [sb(G, es, "OT", [128, 512], BF16) for _ in range(2)]
        it = 0
        for hk in range(2):
            P.dma(KT[:, :], S["KTa"][hk, :, :], reads=[R["KTa"]], writes=[KT])
            P.dma(V[:, :, :], S["Va"][:, hk * 128:(hk + 1) * 128].rearrange("(t p) n -> p t n", p=128), reads=[R["Va"]], writes=[V])
            for h in range(hk * 4, hk * 4 + 4):
                chunks = [(0, 256, [0, 1])] + [(256 + 512 * i, 512, list(range(T))) for i in range(4)]
                for (q0, nq, keys) in chunks:
                    q = QT[it % 2]
                    o = OT[it % 2]
                    it += 1
                    P.dma(q[:, 0:nq], S["QTa"][h, :, q0:q0 + nq], reads=[R["QTa"]], writes=[q], E=P.act)
                    po, pz = G.ps[6], G.ps[7]
                    nk = len(keys)
                    for j, kt in enumerate(keys):
                        pS = G.ps[j % 3]
                        p_ = PT[j % 3]
                        P.mm([lambda e: e.matmul(pS[:, 0:nq], lhsT=KT[:, kt * 128:(kt + 1) * 128], rhs=q[:, 0:nq], start=True, stop=True)],
                             reads=[KT, q], writes=[pS])
                        P.op(P.act, lambda e: e.activation(out=p_[:, 0:nq], in_=pS[:, 0:nq], func=AF.Exp, scale=sc), reads=[pS], writes=[p_])
                        P.mm([lambda e: e.matmul(po[:, 0:nq], lhsT=V[:, kt, :], rhs=p_[:, 0:nq], start=(j == 0), stop=(j == nk - 1)),
                              lambda e: e.matmul(pz[:, 0:nq], lhsT=G.oneb[:, :], rhs=p_[:, 0:nq], start=(j == 0), stop=(j == nk - 1))],
                             reads=[V, p_, G.oneb], writes=[po, pz])
                    P.op(P.dve, lambda e: e.reciprocal(rz[:, 0:nq], pz[:, 0:nq]), reads=[pz], writes=[rz])
                    P.op(P.dve, lambda e: e.tensor_tensor(out=o[:, 0:nq], in0=po[:, 0:nq], in1=rz[:, 0:nq], op=ALU.mult),
                         reads=[po, rz], writes=[o])
                    P.dma(S["mixT"][h, :, q0:q0 + nq], o[:, 0:nq], reads=[o], writes=[R["mixT"]], E=P.pool)


def phase_ret(G):
    P, I, S, R = G.P, G.I, G.S, G.res
    fwd = list(range(T))
    bwd = [1, 0] + list(range(T - 1, 1, -1))
    with ExitStack() as es:
        dexp = bc_row(G, es, "dexp", I["b_decay_exp"][0, :], 16)
        lgm = sb(G, es, "lgm", [128, 16], F32)
        P.op(P.act, lambda e: e.activation(out=lgm[:, :], in_=dexp[:, :], func=AF.Exp, scale=-math.log(2.0)), reads=[dexp], writes=[lgm])
        P.op(P.act, lambda e: e.activation(out=lgm[:, :], in_=lgm[:, :], func=AF.Ln, scale=-1.0, bias=G.cst[:, C_COL + 3:C_COL + 4]),
             reads=[lgm, G.cst], writes=[lgm])
        gnb = bc_row(G, es, "gnb", I["b_gn"][0, :], 1024)
        QT = sb(G, es, "rQT", [128, NT], BF16)
        KT = sb(G, es, "rKT", [128, NT], BF16)
        Kt = sb(G, es, "rK", [128, T, 128], BF16)
        Vt = sb(G, es, "rV", [128, T, 128], BF16)
        Gt = sb(G, es, "rG", [128, T, 128], BF16)
        Dm = sb(G, es, "Dm", [128, 128], F32)
        Dt = sb(G, es, "Dt", [128, 128], F32)
        qdf = sb(G, es, "qdf", [128, 128], F32)
        qdb = sb(G, es, "qdb", [128, 128], F32)
        col = sb(G, es, "col", [128, 4], F32)
        Sb = sb(G, es, "Sb", [128, T, 128], BF16)
        Sf32 = sb(G, es, "Sf32", [128, 128], F32)
        Sfb = [sb(G, es, "Sfb", [128, 128], BF16) for _ in range(2)]
        Kd = [sb(G, es, "Kd", [128, 128], BF16) for _ in range(2)]
        AT = [sb(G, es, "AT", [128, 128], BF16) for _ in range(2)]
        Qf = [sb(G, es, "Qf", [128, 128], BF16) for _ in range(2)]
        Qb = [sb(G, es, "Qb", [128, 128], BF16) for _ in range(2)]
        O = sb(G, es, "O", [128, T, 128], F32)
        sq = sb(G, es, "osq", [128, T, 128], F32)
        st1 = sb(G, es, "st1", [128, T], F32)
        st2 = sb(G, es, "st2", [128, T], F32)
        ob = sb(G, es, "rob", [128, T, 128], BF16)
        oT = [sb(G, es, "roT", [128, 4, 128], BF16) for _ in range(2)]
        cst = G.cst
        for h in range(8):
            P.dma(QT[:, :], S["QTb"][h, :, :], reads=[R["QTb"]], writes=[QT])
            P.dma(KT[:, :], S["KTb"][h, :, :], reads=[R["KTb"]], writes=[KT], E=P.act)
            P.dma(Kt[:, :, :], S["Kb"][:, h * 128:(h + 1) * 128].rearrange("(t p) n -> p t n", p=128), reads=[R["Kb"]], writes=[Kt])
            P.dma(Vt[:, :, :], S["Vb"][:, h * 128:(h + 1) * 128].rearrange("(t p) n -> p t n", p=128), reads=[R["Vb"]], writes=[Vt], E=P.act)
            P.dma(Gt[:, :, :], S["Gt"][:, h * 128:(h + 1) * 128].rearrange("(t p) n -> p t n", p=128), reads=[R["Gt"]], writes=[Gt])
            lf, lb_ = lgm[:, h:h + 1], lgm[:, 8 + h:9 + h]
            P.op(P.act, lambda e: e.activation(out=Dm[:, :], in_=cst[:, C_RGE:C_RGE + 128], func=AF.Exp, scale=lf), reads=[cst, lgm], writes=[Dm])
            P.op(P.dve, lambda e: e.tensor_tensor(out=Dm[:, :], in0=Dm[:, :], in1=cst[:, C_MGE:C_MGE + 128], op=ALU.mult), reads=[Dm, cst], writes=[Dm])
            P.op(P.act, lambda e: e.activation(out=Dt[:, :], in_=cst[:, C_RLE:C_RLE + 128], func=AF.Exp, scale=lb_), reads=[cst, lgm], writes=[Dt])
            P.op(P.dve, lambda e: e.tensor_tensor(out=Dt[:, :], in0=Dt[:, :], in1=cst[:, C_MLE:C_MLE + 128], op=ALU.mult), reads=[Dt, cst], writes=[Dt])
            P.op(P.dve, lambda e: e.tensor_tensor(out=Dm[:, :], in0=Dm[:, :], in1=Dt[:, :], op=ALU.add), reads=[Dm, Dt], writes=[Dm])
            P.op(P.act, lambda e: e.activation(out=qdf[:, :], in_=cst[:, C_JP1:C_JP1 + 128], func=AF.Exp, scale=lf), reads=[cst, lgm], writes=[qdf])
            P.op(P.act, lambda e: e.activation(out=qdb[:, :], in_=cst[:, C_CMJ:C_CMJ + 128], func=AF.Exp, scale=lb_), reads=[cst, lgm], writes=[qdb])
            P.op(P.act, lambda e: e.activation(out=col[:, 0:1], in_=cst[:, C_COL:C_COL + 1], func=AF.Exp, scale=lf), reads=[cst, lgm], writes=[col])
            P.op(P.act, lambda e: e.activation(out=col[:, 1:2], in_=cst[:, C_COL + 1:C_COL + 2], func=AF.Exp, scale=lb_), reads=[cst, lgm], writes=[col])
            P.op(P.act, lambda e: e.activation(out=col[:, 2:3], in_=lf, func=AF.Exp, scale=128.0), reads=[lgm], writes=[col])
            P.op(P.act, lambda e: e.activation(out=col[:, 3:4], in_=lb_, func=AF.Exp, scale=128.0), reads=[lgm], writes=[col])

            def state_step(c, kcol, gcol, i):
                kd = Kd[i % 2]
                pu = G.ps[4 + i % 2]
                P.op(P.pool, lambda e: e.tensor_scalar(out=kd[:, :], in0=Kt[:, c, :], scalar1=col[:, kcol:kcol + 1], scalar2=None, op0=ALU.mult),
                     reads=[Kt, col], writes=[kd])
                P.mm([lambda e: e.matmul(pu[:, 0:128], lhsT=kd[:, :], rhs=Vt[:, c, :], start=True, stop=True)], reads=[kd, Vt], writes=[pu])
                P.op(P.dve, lambda e: e.scalar_tensor_tensor(out=Sf32[:, :], in0=Sf32[:, :], scalar=col[:, gcol:gcol + 1], in1=pu[:, 0:128],
                                                             op0=ALU.mult, op1=ALU.add), reads=[Sf32, col, pu], writes=[Sf32])

            P.op(P.dve, lambda e: e.memset(Sf32[:, :], 0.0), writes=[Sf32])
            for i, c in enumerate(bwd):
                P.op(P.act, lambda e: e.activation(out=Sb[:, c, :], in_=Sf32[:, :], func=AF.Copy), reads=[Sf32], writes=[Sb])
                if i < T - 1:
                    state_step(c, 1, 3, i)
            P.op(P.dve, lambda e: e.memset(Sf32[:, :], 0.0), writes=[Sf32])
            for i, c in enumerate(fwd):
                sfb, at, qf, qb = Sfb[i % 2], AT[i % 2], Qf[i % 2], Qb[i % 2]
                pS, pO = G.ps[i % 2], G.ps[2 + i % 2]
                P.op(P.act, lambda e: e.activation(out=sfb[:, :], in_=Sf32[:, :], func=AF.Copy), reads=[Sf32], writes=[sfb])
                P.mm([lambda e: e.matmul(pS[:, 0:128], lhsT=KT[:, c * 128:(c + 1) * 128], rhs=QT[:, c * 128:(c + 1) * 128], start=True, stop=True)],
                     reads=[KT, QT], writes=[pS])
                P.op(P.dve, lambda e: e.tensor_tensor(out=at[:, :], in0=pS[:, 0:128], in1=Dm[:, :], op=ALU.mult), reads=[pS, Dm], writes=[at])
                P.op(P.pool, lambda e: e.tensor_tensor(out=qf[:, :], in0=QT[:, c * 128:(c + 1) * 128], in1=qdf[:, :], op=ALU.mult), reads=[QT, qdf], writes=[qf])
                P.op(P.pool, lambda e: e.tensor_tensor(out=qb[:, :], in0=QT[:, c * 128:(c + 1) * 128], in1=qdb[:, :], op=ALU.mult), reads=[QT, qdb], writes=[qb])
                P.mm([lambda e: e.matmul(pO[:, 0:128], lhsT=at[:, :], rhs=Vt[:, c, :], start=True, stop=False),
                      lambda e: e.matmul(pO[:, 0:128], lhsT=qf[:, :], rhs=sfb[:, :], start=False, stop=False),
                      lambda e: e.matmul(pO[:, 0:128], lhsT=qb[:, :], rhs=Sb[:, c, :], start=False, stop=True)],
                     reads=[at, Vt, qf, sfb, qb, Sb], writes=[pO])
                P.op(P.act, lambda e: e.activation(out=O[:, c, :], in_=pO[:, 0:128], func=AF.Copy), reads=[pO], writes=[O])
                if i < T - 1:
                    state_step(c, 0, 2, i)
            P.op(P.dve, lambda e: e.tensor_reduce(out=st1[:, :], in_=O[:, :, :], axis=AX.X, op=ALU.add), reads=[O], writes=[st1])
            P.op(P.dve, lambda e: e.tensor_scalar(out=st1[:, :], in0=st1[:, :], scalar1=1.0 / 128, scalar2=None, op0=ALU.mult), reads=[st1], writes=[st1])
            P.op(P.dve, lambda e: e.tensor_tensor(out=O[:, :, :], in0=O[:, :, :], in1=st1[:, :].unsqueeze(2).to_broadcast([128, T, 128]), op=ALU.subtract),
                 reads=[O, st1], writes=[O])
            P.op(P.act, lambda e: e.activation(out=sq[:, :, :], in_=O[:, :, :], func=AF.Square), reads=[O], writes=[sq])
            P.op(P.dve, lambda e: e.tensor_reduce(out=st2[:, :], in_=sq[:, :, :], axis=AX.X, op=ALU.add), reads=[sq], writes=[st2])
            P.op(P.act, lambda e: e.activation(out=st2[:, :], in_=st2[:, :], func=AF.Sqrt, scale=1.0 / 128, bias=cst[:, C_COL + 2:C_COL + 3]),
                 reads=[st2, cst], writes=[st2])
            P.op(P.dve, lambda e: e.reciprocal(st2[:, :], st2[:, :]), reads=[st2], writes=[st2])
            P.op(P.dve, lambda e: e.tensor_tensor(out=O[:, :, :], in0=O[:, :, :], in1=st2[:, :].unsqueeze(2).to_broadcast([128, T, 128]), op=ALU.mult),
                 reads=[O, st2], writes=[O])
            P.op(P.dve, lambda e: e.tensor_tensor(out=O[:, :, :], in0=O[:, :, :], in1=gnb[:, h * 128:(h + 1) * 128].unsqueeze(1).to_broadcast([128, T, 128]), op=ALU.mult),
                 reads=[O, gnb], writes=[O])
            P.op(P.dve, lambda e: e.tensor_tensor(out=ob[:, :, :], in0=O[:, :, :], in1=Gt[:, :, :], op=ALU.mult), reads=[O, Gt], writes=[ob])
            transpose_out(G, ob, oT, S["mixT"], R["mixT"], 8 + h, list(range(T)))


def transpose_out(G, ob, oT, dst, dres, chunk, tiles):
    P = G.P
    for g0 in range(0, len(tiles), 4):
        ts = tiles[g0:g0 + 4]
        n = len(ts)
        pt = G.ps[6 + (g0 // 4) % 2]
        o_ = oT[(g0 // 4) % 2]
        pvb = pt[:, 0:256].bitcast(BF16)
        P.mm([(lambda e, j=j, t=t: e.transpose(pvb[:, j * 128:(j + 1) * 128], ob[:, t, :], G.idb[:, :])) for j, t in enumerate(ts)],
             reads=[ob, G.idb], writes=[pt])
        P.op(P.act, lambda e: e.activation(out=o_[:, 0:n, :], in_=pvb[:, 0:n * 128].rearrange("p (h n) -> p h n", h=n), func=AF.Copy),
             reads=[pt], writes=[o_])
        P.dma(dst[chunk, :, ts[0] * 128:(ts[0] + n) * 128], o_[:, 0:n, :].rearrange("p h n -> p (h n)"), reads=[o_], writes=[dres], E=P.pool)


def phase_outproj(G, l, hT, tiles):
    P, I, S, R = G.P, G.I, G.S, G.res
    W = I["ab_w_out"] if l == 0 else I["c_w_out"]
    with ExitStack() as es:
        P.dma(hT[:, :, :], S["mixT"].rearrange("k p n -> p k n"), reads=[R["mixT"]], writes=[hT])
        gate = [bc_row(G, es, "og", S["modD"][l, r, 2 * D:3 * D], D, E=P.act) for r in range(2)]
        xo = [sb(G, es, "xo", [128, 512], F32) for _ in range(2)]
        xn = [sb(G, es, "xn", [128, 512], F32) for _ in range(2)]
        st = dict(i=0)

        def post(gi, t, pq, n):
            i = st["i"]
            st["i"] += 1
            a, b = xo[i % 2], xn[i % 2]
            if l == 0:
                src = I["ctx"][t * 128:(t + 1) * 128, gi * 512:(gi + 1) * 512] if t < 2 else I["x"][(t - 2) * 128:(t - 1) * 128, gi * 512:(gi + 1) * 512]
                rd = []
            else:
                src = S["xres"][t * 128:(t + 1) * 128, gi * 512:(gi + 1) * 512]
                rd = [R["xres"]]
            P.dma(a[:, :], src, reads=rd, writes=[a], E=P.act)
            r = 1 if t < 2 else 0
            P.op(P.dve, lambda e: e.tensor_tensor(out=b[:, :], in0=pq[:, 0:512], in1=gate[r][:, gi * 512:(gi + 1) * 512], op=ALU.mult),
                 reads=[pq, gate[r]], writes=[b])
            P.op(P.pool, lambda e: e.tensor_tensor(out=b[:, :], in0=b[:, :], in1=a[:, :], op=ALU.add), reads=[b, a], writes=[b])
            P.dma(S["xres"][t * 128:(t + 1) * 128, gi * 512:(gi + 1) * 512], b[:, :], reads=[b], writes=[R["xres"]], E=P.pool)

        linear_tm(G, es, W, [(i * 512, 512) for i in range(4)], tiles, hT, post)


def phase_moe(G, l, tiles, h2, route, bigA):
    P, I, S, R, nc = G.P, G.I, G.S, G.res, G.nc
    nT = len(tiles)
    NB = -(-(4 * nT * 128 + NE * 255) // 256)
    BIG = 1.0e6
    mk, gt, pos = route["mask"], route["gate"], route["pos"]
    dselA, gselA = route["dsel"], route["gsel"]
    cst = G.cst
    t0, t1 = tiles[0], tiles[-1] + 1
    with ExitStack() as es:
        b1T = sb(G, es, "b1T", [128, 2 * KC, NE], F32)
        with ExitStack() as es1:
            b1s = sb(G, es1, "b1s", [NE, 2 * D], F32)
            P.dma(b1s[:, :], I["exp_b1"][l, :, :], writes=[b1s])
            for two in range(2):
                pq = G.ps[two]
                P.mm([(lambda e, c=c: e.transpose(pq[:, c * 32:(c + 1) * 32], b1s[:, c * 256 + two:(c + 1) * 256:2], cst[0:32, C_ID:C_ID + 32]))
                      for c in range(KC)], reads=[b1s, cst], writes=[pq])
                P.op(P.dve, lambda e: e.tensor_copy(b1T[:, two:2 * KC:2, :], pq[:, :].rearrange("p (c n) -> p c n", c=KC)), reads=[pq], writes=[b1T])
            P.barrier()
        cnt = sb(G, es, "cnt", [128, NE], F32)
        nblk = sb(G, es, "nblk", [128, NE], F32)
        bend = sb(G, es, "bend", [128, NE], F32)
        bst = sb(G, es, "bst", [128, NE], F32)
        tmpe = sb(G, es, "tmpe", [128, NE], F32)
        destm = sb(G, es, "destm", [128, T, NE], F32)
        oh = sb(G, es, "oh", [128, NB, NE], F32)
        oh2 = sb(G, es, "oh2", [128, NB, NE], F32)
        bef = sb(G, es, "bef", [128, NB], F32)
        bei = sb(G, es, "bei", [128, NB], mybir.dt.int32)
        tmp3 = sb(G, es, "tmp3", [128, T, NE], F32)
        b1tmp = sb(G, es, "b1tmp", [128, 2 * KC, NE], F32)
        b1sel = [sb(G, es, "b1sel", [128, 2 * KC], F32) for _ in range(2)]
        pq = G.ps[0]
        P.mm([(lambda e, j=j, t=t: e.matmul(pq[:, 0:NE], lhsT=cst[:, C_ONE:C_ONE + 128], rhs=mk[:, t, :], start=(j == 0), stop=(j == nT - 1)))
              for j, t in enumerate(tiles)], reads=[mk, cst], writes=[pq])
        P.op(P.dve, lambda e: e.tensor_copy(cnt[:, :], pq[:, 0:NE]), reads=[pq], writes=[cnt])
        P.op(P.dve, lambda e: e.tensor_scalar(out=nblk[:, :], in0=cnt[:, :], scalar1=0.0, scalar2=None, op0=ALU.is_gt), reads=[cnt], writes=[nblk])
        for j in range(1, 9):
            P.op(P.dve, lambda e: e.tensor_scalar(out=tmpe[:, :], in0=cnt[:, :], scalar1=256.0 * j, scalar2=None, op0=ALU.is_gt), reads=[cnt], writes=[tmpe])
            P.op(P.dve, lambda e: e.tensor_tensor(out=nblk[:, :], in0=nblk[:, :], in1=tmpe[:, :], op=ALU.add), reads=[nblk, tmpe], writes=[nblk])
        P.op(P.dve, lambda e: e.tensor_tensor_scan(out=bend[:, :], data0=cst[:, C_ONE:C_ONE + NE], data1=nblk[:, :], initial=0.0,
                                                   op0=ALU.mult, op1=ALU.add), reads=[cst, nblk], writes=[bend])
        P.op(P.dve, lambda e: e.tensor_tensor(out=bst[:, :], in0=bend[:, :], in1=nblk[:, :], op=ALU.subtract), reads=[bend, nblk], writes=[bst])
        P.op(P.dve, lambda e: e.tensor_scalar(out=tmpe[:, :], in0=bst[:, :], scalar1=256.0, scalar2=BIG, op0=ALU.mult, op1=ALU.add), reads=[bst], writes=[tmpe])
        P.op(P.dve, lambda e: e.tensor_tensor(out=destm[:, t0:t1, :], in0=pos[:, t0:t1, :], in1=tmpe[:, :].unsqueeze(1).to_broadcast([128, nT, NE]), op=ALU.add),
             reads=[pos, tmpe], writes=[destm])
        P.op(P.dve, lambda e: e.tensor_tensor(out=destm[:, t0:t1, :], in0=destm[:, t0:t1, :], in1=mk[:, t0:t1, :], op=ALU.mult), reads=[destm, mk], writes=[destm])
        P.op(P.dve, lambda e: e.tensor_scalar(out=destm[:, t0:t1, :], in0=destm[:, t0:t1, :], scalar1=-BIG, scalar2=None, op0=ALU.add), reads=[destm], writes=[destm])
        iob = cst[:, C_IOTA:C_IOTA + NB].unsqueeze(2).to_broadcast([128, NB, NE])
        P.op(P.dve, lambda e: e.tensor_tensor(out=oh[:, :, :], in0=iob, in1=bst[:, :].unsqueeze(1).to_broadcast([128, NB, NE]), op=ALU.is_ge), reads=[cst, bst], writes=[oh])
        P.op(P.dve, lambda e: e.tensor_tensor(out=oh2[:, :, :], in0=iob, in1=bend[:, :].unsqueeze(1).to_broadcast([128, NB, NE]), op=ALU.is_lt), reads=[cst, bend], writes=[oh2])
        P.op(P.dve, lambda e: e.tensor_tensor(out=oh[:, :, :], in0=oh[:, :, :], in1=oh2[:, :, :], op=ALU.mult), reads=[oh, oh2], writes=[oh])
        P.op(P.dve, lambda e: e.tensor_tensor(out=oh2[:, :, :], in0=oh[:, :, :], in1=cst[:, C_IOTA:C_IOTA + NE].unsqueeze(1).to_broadcast([128, NB, NE]), op=ALU.mult),
             reads=[oh, cst], writes=[oh2])
        P.op(P.dve, lambda e: e.tensor_reduce(out=bef[:, :], in_=oh2[:, :, :], axis=AX.X, op=ALU.add), reads=[oh2], writes=[bef])
        P.op(P.dve, lambda e: e.tensor_copy(bei[:, :], bef[:, :]), reads=[bef], writes=[bei])
        for b in range(NB):
            ohb = oh[:, b:b + 1, :].to_broadcast([128, nT, NE])
            P.op(P.dve, lambda e: e.tensor_tensor(out=tmp3[:, t0:t1, :], in0=destm[:, t0:t1, :], in1=ohb, op=ALU.mult), reads=[destm, oh], writes=[tmp3])
            P.op(P.dve, lambda e: e.tensor_reduce(out=dselA[:, b, t0:t1], in_=tmp3[:, t0:t1, :], axis=AX.X, op=ALU.add), reads=[tmp3], writes=[dselA])
            P.op(P.pool, lambda e: e.tensor_tensor(out=oh2[:, 0:nT, :], in0=gt[:, t0:t1, :], in1=ohb, op=ALU.mult), reads=[gt, oh], writes=[oh2])
            P.op(P.dve, lambda e: e.tensor_reduce(out=gselA[:, b, t0:t1], in_=oh2[:, 0:nT, :], axis=AX.X, op=ALU.add), reads=[oh2], writes=[gselA])
            P.op(P.dve, lambda e: e.tensor_scalar(out=dselA[:, b, t0:t1], in0=dselA[:, b, t0:t1], scalar1=-256.0 * b, scalar2=None, op0=ALU.add),
                 reads=[dselA], writes=[dselA])
        Sall = sb(G, es, "Sall", [128, nT, 256], BF16)
        hgT = sb(G, es, "hgT", [128, KC, 256], BF16)
        actT = sb(G, es, "actT", [128, KC, 256], BF16)
        wbb = [sb(G, es, "wbb", [128, KC * 256], BF16) for _ in range(4)]
        gg = [sb(G, es, "gg", [128, 256], F32) for _ in range(2)]
        sg = [sb(G, es, "sg", [128, 256], F32) for _ in range(2)]
        ll = [sb(G, es, "ll", [128, 256], F32) for _ in range(2)]
        oS = [sb(G, es, "oS", [128, 256], BF16) for _ in range(3)]
        be1 = sb(G, es, "be1", [128, NB], F32)
        be2 = sb(G, es, "be2", [128, NB], F32)
        idx = [sb(G, es, "idx", [128, 48], mybir.dt.int32) for _ in range(2)]
        P.op(P.dve, lambda e: e.tensor_scalar(out=be1[:, :], in0=bef[:, :], scalar1=4096.0, scalar2=None, op0=ALU.mult), reads=[bef], writes=[be1])
        P.op(P.dve, lambda e: e.tensor_scalar(out=be2[:, :], in0=bef[:, :], scalar1=2048.0, scalar2=None, op0=ALU.mult), reads=[bef], writes=[be2])
        wi = dict(i=0)

        class WV:
            def __init__(s_, tl):
                s_.ap = tl.t[:, :].rearrange("p (k n) -> p k n", k=KC)
                s_.r = tl.r

            def __getitem__(s_, k):
                return s_.ap[k]

        def load_block(ix, which, piece):
            i = wi["i"]
            wi["i"] += 1
            w = wbb[i % 4]
            W = I["w1t%d" % l] if which == 1 else I["w2t%d" % l]
            col0 = 0 if which == 1 else 32
            for h in range(2):
                j = col0 + 2 * piece + h
                P.dma(None, None, reads=[ix], writes=[w], E=P.pool,
                      fn=lambda e: e.indirect_dma_start(out=w[:, h * 2048:(h + 1) * 2048], out_offset=None, in_=W[:, :],
                                                        in_offset=bass.IndirectOffsetOnAxis(ap=ix[:, j:j + 1], axis=0)))
            return WV(w)

        oi = 0
        for b in range(NB):
            ix = idx[b % 2]
            P.op(P.dve, lambda e: e.tensor_scalar(out=ix[:, 0:32], in0=cst[:, C_OFF1:C_OFF1 + 32], scalar1=be1[:, b:b + 1], scalar2=None, op0=ALU.add),
                 reads=[cst, be1], writes=[ix])
            P.op(P.dve, lambda e: e.tensor_scalar(out=ix[:, 32:48], in0=cst[:, C_OFF2:C_OFF2 + 16], scalar1=be2[:, b:b + 1], scalar2=None, op0=ALU.add),
                 reads=[cst, be2], writes=[ix])
            bs = b1sel[b % 2]
            P.op(P.dve, lambda e: e.tensor_tensor(out=b1tmp[:, :, :], in0=b1T[:, :, :], in1=oh[:, b:b + 1, :].to_broadcast([128, 2 * KC, NE]), op=ALU.mult),
                 reads=[b1T, oh], writes=[b1tmp])
            P.op(P.dve, lambda e: e.tensor_reduce(out=bs[:, :], in_=b1tmp[:, :, :], axis=AX.X, op=ALU.add), reads=[b1tmp], writes=[bs])
            for j, t in enumerate(tiles):
                P.op(P.dve, lambda e: e.tensor_scalar(out=Sall[:, j, :], in0=cst[:, C_IOTA:C_IOTA + 256], scalar1=dselA[:, b, t:t + 1],
                                                      scalar2=None, op0=ALU.is_equal), reads=[cst, dselA], writes=[Sall])
            for k in range(KC):
                pq = G.ps[k % 2]
                P.mm([(lambda e, j=j, t=t: e.matmul(pq[:, 0:256], lhsT=h2[:, t, k * 128:(k + 1) * 128], rhs=Sall[:, j, :],
                                                   start=(j == 0), stop=(j == nT - 1))) for j, t in enumerate(tiles)],
                     reads=[h2, Sall], writes=[pq])
                if k % 2:
                    P.op(P.act, lambda e: e.activation(out=hgT[:, k, :], in_=pq[:, 0:256], func=AF.Copy), reads=[pq], writes=[hgT])
                else:
                    P.op(P.dve, lambda e: e.tensor_copy(hgT[:, k, :], pq[:, 0:256]), reads=[pq], writes=[hgT])
            for c in range(KC):
                w = load_block(ix, 1, c)
                pg, pl = G.ps[2 + c % 2], G.ps[4 + c % 2]
                g_, s_, l_ = gg[c % 2], sg[c % 2], ll[c % 2]
                P.mm([(lambda e, k=k: e.matmul(pg[:, 0:256], lhsT=w[:, k, 0:128], rhs=hgT[:, k, :], start=(k == 0), stop=(k == KC - 1)))
                      for k in range(KC)], reads=[w, hgT], writes=[pg])
                P.mm([(lambda e, k=k: e.matmul(pl[:, 0:256], lhsT=w[:, k, 128:256], rhs=hgT[:, k, :], start=(k == 0), stop=(k == KC - 1)))
                      for k in range(KC)], reads=[w, hgT], writes=[pl])
                P.op(P.dve, lambda e: e.tensor_scalar(out=g_[:, :], in0=pg[:, 0:256], scalar1=bs[:, 2 * c:2 * c + 1], scalar2=7.0,
                                                      op0=ALU.add, op1=ALU.min), reads=[pg, bs], writes=[g_])
                P.op(P.act, lambda e: e.activation(out=s_[:, :], in_=g_[:, :], func=AF.Sigmoid, scale=1.702), reads=[g_], writes=[s_])
                P.op(P.dve, lambda e: e.tensor_scalar(out=l_[:, :], in0=pl[:, 0:256], scalar1=bs[:, 2 * c + 1:2 * c + 2], scalar2=7.0,
                                                      op0=ALU.add, op1=ALU.min), reads=[pl, bs], writes=[l_])
                P.op(P.dve, lambda e: e.tensor_scalar(out=l_[:, :], in0=l_[:, :], scalar1=-7.0, scalar2=1.0, op0=ALU.max, op1=ALU.add),
                     reads=[l_], writes=[l_])
                P.op(P.dve, lambda e: e.tensor_tensor(out=g_[:, :], in0=g_[:, :], in1=s_[:, :], op=ALU.mult), reads=[g_, s_], writes=[g_])
                P.op(P.dve, lambda e: e.tensor_tensor(out=actT[:, c, :], in0=g_[:, :], in1=l_[:, :], op=ALU.mult), reads=[g_, l_], writes=[actT])
            for n in range(8):
                w = load_block(ix, 2, n)
                for s_i in range(2):
                    pq = G.ps[6 + oi % 2]
                    o_ = oS[oi % 3]
                    oi += 1
                    P.mm([(lambda e, k=k: e.matmul(pq[:, 0:256], lhsT=actT[:, k, s_i * 128:(s_i + 1) * 128], rhs=w[:, k, :],
                                                   start=(k == 0), stop=(k == KC - 1))) for k in range(KC)], reads=[actT, w], writes=[pq])
                    if oi % 2:
                        P.op(P.act, lambda e: e.activation(out=o_[:, :], in_=pq[:, 0:256], func=AF.Copy), reads=[pq], writes=[o_])
                    else:
                        P.op(P.dve, lambda e: e.tensor_copy(o_[:, :], pq[:, 0:256]), reads=[pq], writes=[o_])
                    P.dma(S["outE"][b, s_i * 128:(s_i + 1) * 128, n * 256:(n + 1) * 256], o_[:, :], reads=[o_], writes=[R["outE"]], E=P.sp)
    P.barrier()
    with ExitStack() as es:
        acc = bigA.t[:, :].bitcast(F32).rearrange("p (t n) -> p t n", n=D)
        accR = bigA.r
        oE = [sb(G, es, "oE", [128, 2, D], BF16) for _ in range(2)]
        Sg = [sb(G, es, "Sg", [128, 256], BF16) for _ in range(2)]
        SgT = [sb(G, es, "SgT", [128, 2, 128], BF16) for _ in range(2)]
        b2s = sb(G, es, "b2s", [NE, D], F32)
        gT = sb(G, es, "gT", [NE, 128], F32)
        xt = [sb(G, es, "mxt", [128, D], F32) for _ in range(2)]
        gate = [bc_row(G, es, "mg", S["modD"][l, r, 5 * D:6 * D], D, E=P.act) for r in range(2)]
        P.dma(b2s[:, :], I["exp_b2"][l, :, :], writes=[b2s])
        for g0 in range(0, nT, 9):
            grp = tiles[g0:g0 + 9]
            for j, t in enumerate(grp):
                pt = G.ps[4]
                P.mm([lambda e: e.transpose(pt[0:NE, 0:128], gt[:, t, :], cst[:, C_ID:C_ID + 128])], reads=[gt, cst], writes=[pt])
                P.op(P.act, lambda e: e.activation(out=gT[:, :], in_=pt[0:NE, 0:128], func=AF.Copy), reads=[pt], writes=[gT])
                for n in range(4):
                    pq = G.ps[n]
                    P.mm([lambda e: e.matmul(pq[:, :], lhsT=gT[:, :], rhs=b2s[:, n * 512:(n + 1) * 512], start=True, stop=True)],
                         reads=[gT, b2s], writes=[pq])
                    P.op(P.dve if n % 2 else P.act,
                         (lambda e: e.tensor_copy(acc[:, j, n * 512:(n + 1) * 512], pq[:, :])) if n % 2 else
                         (lambda e: e.activation(out=acc[:, j, n * 512:(n + 1) * 512], in_=pq[:, :], func=AF.Copy)),
                         reads=[pq], writes=[accR])
            it = 0
            for b in range(NB):
                o = oE[b % 2]
                P.dma(o[:, :, :], S["outE"][b, :, :].rearrange("(s p) n -> p s n", p=128), reads=[R["outE"]], writes=[o])
                for j, t in enumerate(grp):
                    sg_, sgT = Sg[it % 2], SgT[it % 2]
                    it += 1
                    P.op(P.dve, lambda e: e.tensor_scalar(out=sg_[:, :], in0=cst[:, C_IOTA:C_IOTA + 256], scalar1=dselA[:, b, t:t + 1],
                                                           scalar2=gselA[:, b, t:t + 1], op0=ALU.is_equal, op1=ALU.mult),
                         reads=[cst, dselA, gselA], writes=[sg_])
                    pt = G.ps[4 + it % 2]
                    pvb = pt[:, 0:128].bitcast(BF16)
                    P.mm([(lambda e, s_i=s_i: e.transpose(pvb[:, s_i * 128:(s_i + 1) * 128], sg_[:, s_i * 128:(s_i + 1) * 128], G.idb[:, :]))
                          for s_i in range(2)], reads=[sg_, G.idb], writes=[pt])
                    P.op(P.act, lambda e: e.activation(out=sgT[:, :, :], in_=pvb[:, 0:256].rearrange("p (s n) -> p s n", s=2), func=AF.Copy),
                         reads=[pt], writes=[sgT])
                    for n in range(4):
                        pq = G.ps[n]
                        P.mm([(lambda e, s_i=s_i: e.matmul(pq[:, :], lhsT=sgT[:, s_i, :], rhs=o[:, s_i, n * 512:(n + 1) * 512],
                                                          start=(s_i == 0), stop=(s_i == 1))) for s_i in range(2)], reads=[sgT, o], writes=[pq])
                        P.op(P.dve, lambda e: e.tensor_tensor(out=acc[:, j, n * 512:(n + 1) * 512], in0=acc[:, j, n * 512:(n + 1) * 512],
                                                              in1=pq[:, :], op=ALU.add), reads=[pq, accR], writes=[accR])
            for j, t in enumerate(grp):
                x = xt[j % 2]
                r = 1 if t < 2 else 0
                P.dma(x[:, :], S["xres"][t * 128:(t + 1) * 128, :], reads=[R["xres"]], writes=[x])
                P.op(P.pool, lambda e: e.tensor_tensor(out=acc[:, j, :], in0=acc[:, j, :], in1=gate[r][:, :], op=ALU.mult),
                     reads=[accR, gate[r]], writes=[accR])
                P.op(P.dve, lambda e: e.tensor_tensor(out=x[:, :], in0=x[:, :], in1=acc[:, j, :], op=ALU.add), reads=[x, accR], writes=[x])
                P.dma(S["xres"][t * 128:(t + 1) * 128, :], x[:, :], reads=[x], writes=[R["xres"]], E=P.pool)


def phase_hgrn(G, hT):
    P, I, S, R = G.P, G.I, G.S, G.res
    cst = G.cst
    W = I["c_w_in"]
    fwd = list(range(T))
    bwd = [1, 0] + list(range(T - 1, 1, -1))
    NCH = [(i * 512, min(512, NT - i * 512)) for i in range(5)]
    with ExitStack() as es:
        lbT = sb(G, es, "lbT", [128, 16], F32)
        omlT = sb(G, es, "omlT", [128, 16], F32)
        with ExitStack() as es1:
            c0 = sb(G, es1, "c0", [16, 128], F32)
            c1 = sb(G, es1, "c1", [16, 128], F32)
            P.dma(c0[:, :], I["c_lb"][0, :].rearrange("(h p) -> h p", p=128), writes=[c0])
            P.dma(c1[:, :], I["c_lb"][1, :].rearrange("(h p) -> h p", p=128), writes=[c1])
            P.op(P.dve, lambda e: e.tensor_tensor(out=c1[:, :], in0=c1[:, :], in1=c0[:, :], op=ALU.subtract), reads=[c0, c1], writes=[c1])
            P.op(P.act, lambda e: e.activation(out=c1[:, :], in_=c1[:, :], func=AF.Sigmoid), reads=[c1], writes=[c1])
            pq = G.ps[0]
            P.mm([lambda e: e.transpose(pq[:, 0:16], c1[:, :], cst[0:16, C_ID:C_ID + 16])], reads=[c1, cst], writes=[pq])
            P.op(P.dve, lambda e: e.tensor_copy(lbT[:, :], pq[:, 0:16]), reads=[pq], writes=[lbT])
            P.op(P.dve, lambda e: e.tensor_scalar(out=omlT[:, :], in0=lbT[:, :], scalar1=-1.0, scalar2=1.0, op0=ALU.mult, op1=ALU.add),
                 reads=[lbT], writes=[omlT])
            P.barrier()
        gnb = bc_row(G, es, "cgn", I["c_gn"][0, :], D)
        wst = sb(G, es, "hwst", [128, KC, 128], F32)
        W5 = sb(G, es, "W5", [128, KC, 5, 128], BF16)
        lf = sb(G, es, "lf", [128, NT], F32)
        kk = sb(G, es, "kk", [128, NT], F32)
        BX = sb(G, es, "BX", [128, NT], F32)
        qs = sb(G, es, "qs", [128, NT], F32)
        qe = sb(G, es, "qe", [128, NT], BF16)
        ke = sb(G, es, "ke", [128, NT], BF16)
        vt = sb(G, es, "vt", [128, T, 128], BF16)
        gtm = sb(G, es, "gtm", [128, T, 128], BF16)
        O = sb(G, es, "hO", [128, T, 128], F32)
        ones = sb(G, es, "hones", [128, NT], BF16)
        ob = sb(G, es, "hob", [128, T, 128], BF16)
        oT = [sb(G, es, "hoT", [128, 4, 128], BF16) for _ in range(2)]
        sc = sb(G, es, "hsc", [128, 4, T], F32)
        Sst = sb(G, es, "hS", [128, 128], F32)
        Ssc = [sb(G, es, "hSsc", [128, 128], BF16) for _ in range(2)]
        keT = [sb(G, es, "hkeT", [128, 128], BF16) for _ in range(2)]
        AT = [sb(G, es, "hAT", [128, 128], BF16) for _ in range(2)]
        st2 = sb(G, es, "hst2", [128, T], F32)
        P.op(P.pool, lambda e: e.memset(ones[:, :], 1.0), writes=[ones])
        BX3 = BX.t[:, :].rearrange("p (c n) -> p c n", n=128)
        lf3 = lf.t[:, :].rearrange("p (c n) -> p c n", n=128)
        for h in range(16):
            for j in range(5):
                P.dma(wst[:, :, :], W[:, j * D + h * 128:j * D + (h + 1) * 128].rearrange("(k p) n -> p k n", p=128), writes=[wst],
                      E=(P.sp if j % 2 == 0 else P.act))
                if j % 2 == 0:
                    P.op(P.pool, lambda e: e.tensor_copy(W5[:, :, j, :], wst[:, :, :]), reads=[wst], writes=[W5])
                else:
                    P.op(P.act, lambda e: e.activation(out=W5[:, :, j, :], in_=wst[:, :, :], func=AF.Copy), reads=[wst], writes=[W5])

            def proj_fm(j, post):
                for ci, (n0, nn) in enumerate(NCH):
                    pq = G.ps[ci % 2]
                    P.mm([(lambda e, k=k: e.matmul(pq[:, 0:nn], lhsT=W5[:, k, j, :], rhs=hT[:, k, n0:n0 + nn], start=(k == 0), stop=(k == KC - 1)))
                          for k in range(KC)], reads=[W5, hT], writes=[pq])
                    post(pq, n0, nn)

            def proj_tm(j, dst, tiles, func):
                for g0 in range(0, len(tiles), 4):
                    ts = tiles[g0:g0 + 4]
                    pq = G.ps[2 + (g0 // 4) % 2]
                    for jj, t in enumerate(ts):
                        P.mm([(lambda e, k=k: e.matmul(pq[:, jj * 128:(jj + 1) * 128], lhsT=hT[:, k, t * 128:(t + 1) * 128], rhs=W5[:, k, j, :],
                                                       start=(k == 0), stop=(k == KC - 1))) for k in range(KC)], reads=[W5, hT], writes=[pq])
                    n = len(ts)
                    P.op(P.act, lambda e: e.activation(out=dst[:, ts[0]:ts[0] + n, :], in_=pq[:, 0:n * 128].rearrange("p (t n) -> p t n", t=n), func=func),
                         reads=[pq], writes=[dst])

            proj_tm(2, vt, list(range(T)), AF.Copy)
            proj_tm(4, gtm, list(range(2, T)), AF.Silu)
            proj_fm(3, lambda pq, n0, nn: P.op(P.act, lambda e: e.activation(out=qs[:, n0:n0 + nn], in_=pq[:, 0:nn], func=AF.Silu), reads=[pq], writes=[qs]))
            for d in range(2):
                order = fwd if d == 0 else bwd
                proj_fm(d, lambda pq, n0, nn: P.op(P.act, lambda e: e.activation(out=lf[:, n0:n0 + nn], in_=pq[:, 0:nn], func=AF.Sigmoid), reads=[pq], writes=[lf]))
                P.op(P.dve, lambda e: e.tensor_scalar(out=lf[:, :], in0=lf[:, :], scalar1=omlT[:, h:h + 1], scalar2=lbT[:, h:h + 1], op0=ALU.mult, op1=ALU.add),
                     reads=[lf, omlT, lbT], writes=[lf])
                P.op(P.pool, lambda e: e.tensor_scalar(out=kk[:, :], in0=lf[:, :], scalar1=-1.0, scalar2=1.0, op0=ALU.mult, op1=ALU.add), reads=[lf], writes=[kk])
                P.op(P.act, lambda e: e.activation(out=lf[:, :], in_=lf[:, :], func=AF.Ln), reads=[lf], writes=[lf])
                P.op(P.dve, lambda e: e.tensor_tensor_scan(out=BX[:, :], data0=ones[:, :], data1=lf[:, :], initial=0.0, op0=ALU.mult, op1=ALU.add),
                     reads=[ones, lf], writes=[BX])
                if d == 0:
                    P.op(P.dve, lambda e: e.memset(sc[:, 0, 0:1], 0.0), writes=[sc])
                    P.op(P.dve, lambda e: e.tensor_scalar(out=sc[:, 0, 1:T], in0=BX3[:, 0:T - 1, 127], scalar1=-1.0, scalar2=None, op0=ALU.mult), reads=[BX], writes=[sc])
                    edge = 127
                else:
                    P.op(P.dve, lambda e: e.tensor_copy(sc[:, 0, :], BX3[:, :, 127]), reads=[BX], writes=[sc])
                    P.op(P.dve, lambda e: e.tensor_tensor(out=BX[:, :], in0=lf[:, :], in1=BX[:, :], op=ALU.subtract), reads=[lf, BX], writes=[BX])
                    edge = 0
                P.op(P.dve, lambda e: e.tensor_tensor(out=sc[:, 1, :], in0=sc[:, 0, :], in1=BX3[:, :, 64], op=ALU.add), reads=[sc, BX], writes=[sc])
                P.op(P.dve, lambda e: e.tensor_tensor(out=sc[:, 2, :], in0=sc[:, 0, :], in1=BX3[:, :, edge], op=ALU.add), reads=[sc, BX], writes=[sc])
                P.op(P.dve, lambda e: e.tensor_tensor(out=sc[:, 3, :], in0=BX3[:, :, edge], in1=BX3[:, :, 64], op=ALU.subtract), reads=[sc, BX], writes=[sc])
                P.op(P.act, lambda e: e.activation(out=sc[:, 1:4, :], in_=sc[:, 1:4, :], func=AF.Exp), reads=[sc], writes=[sc])
                P.op(P.dve, lambda e: e.tensor_tensor(out=lf3, in0=BX3, in1=BX3[:, :, 64:65].to_broadcast([128, T, 128]), op=ALU.subtract), reads=[BX], writes=[lf])
                P.op(P.act, lambda e: e.activation(out=BX[:, :], in_=lf[:, :], func=AF.Exp), reads=[lf], writes=[BX])
                P.op(P.dve, lambda e: e.tensor_tensor(out=qe[:, :], in0=qs[:, :], in1=BX[:, :], op=ALU.mult), reads=[qs, BX], writes=[qe])
                P.op(P.act, lambda e: e.activation(out=BX[:, :], in_=lf[:, :], func=AF.Exp, scale=-1.0), reads=[lf, qe], writes=[BX])
                P.op(P.dve, lambda e: e.tensor_tensor(out=ke[:, :], in0=kk[:, :], in1=BX[:, :], op=ALU.mult), reads=[kk, BX], writes=[ke])
                mcol = C_MGE if d == 0 else C_MLE
                P.op(P.dve, lambda e: e.memset(Sst[:, :], 0.0), writes=[Sst])
                for i, c in enumerate(order):
                    kt, at, ssc = keT[i % 2], AT[i % 2], Ssc[i % 2]
                    cs_ = slice(c * 128, (c + 1) * 128)
                    pt = G.ps[4 + i % 2]
                    ptb = pt[:, 0:64].bitcast(BF16)
                    P.mm([lambda e: e.transpose(ptb[:, 0:128], ke[:, cs_], G.idb[:, :])], reads=[ke, G.idb], writes=[pt])
                    P.op(P.act, lambda e: e.activation(out=kt[:, :], in_=ptb[:, 0:128], func=AF.Copy), reads=[pt], writes=[kt])
                    if c >= 2:
                        pS, pO = G.ps[6], G.ps[7]
                        P.mm([lambda e: e.matmul(pS[:, 0:128], lhsT=ke[:, cs_], rhs=qe[:, cs_], start=True, stop=True)], reads=[ke, qe], writes=[pS])
                        P.op(P.dve, lambda e: e.tensor_tensor(out=at[:, :], in0=pS[:, 0:128], in1=cst[:, mcol:mcol + 128], op=ALU.mult), reads=[pS, cst], writes=[at])
                        P.op(P.act, lambda e: e.activation(out=ssc[:, :], in_=Sst[:, :], func=AF.Copy, scale=sc[:, 1, c:c + 1]), reads=[Sst, sc], writes=[ssc])
                        P.mm([lambda e: e.matmul(pO[:, 0:128], lhsT=at[:, :], rhs=vt[:, c, :], start=True, stop=False),
                              lambda e: e.matmul(pO[:, 0:128], lhsT=qe[:, cs_], rhs=ssc[:, :], start=False, stop=True)],
                             reads=[at, vt, qe, ssc], writes=[pO])
                        if d == 0:
                            P.op(P.act, lambda e: e.activation(out=O[:, c, :], in_=pO[:, 0:128], func=AF.Copy), reads=[pO], writes=[O])
                        else:
                            P.op(P.dve, lambda e: e.tensor_tensor(out=O[:, c, :], in0=O[:, c, :], in1=pO[:, 0:128], op=ALU.add), reads=[pO, O], writes=[O])
                    if i < T - 1:
                        pU = G.ps[2 + i % 2]
                        P.mm([lambda e: e.matmul(pU[:, 0:128], lhsT=kt[:, :], rhs=vt[:, c, :], start=True, stop=True)], reads=[kt, vt], writes=[pU])
                        P.op(P.dve, lambda e: e.tensor_scalar(out=Sst[:, :], in0=Sst[:, :], scalar1=sc[:, 2, c:c + 1], scalar2=None, op0=ALU.mult),
                             reads=[Sst, sc], writes=[Sst])
                        P.op(P.dve, lambda e: e.scalar_tensor_tensor(out=Sst[:, :], in0=pU[:, 0:128], scalar=sc[:, 3, c:c + 1], in1=Sst[:, :],
                                                                     op0=ALU.mult, op1=ALU.add), reads=[pU, sc, Sst], writes=[Sst])
            Ox = O.t[:, 2:T, :]
            lfx = lf.t[:, :].rearrange("p (c n) -> p c n", n=128)[:, 2:T, :]
            P.op(P.act, lambda e: e.activation(out=lfx, in_=Ox, func=AF.Square), reads=[O], writes=[lf])
            P.op(P.dve, lambda e: e.tensor_reduce(out=st2[:, 2:T], in_=lfx, axis=AX.X, op=ALU.add), reads=[lf], writes=[st2])
            P.op(P.act, lambda e: e.activation(out=st2[:, 2:T], in_=st2[:, 2:T], func=AF.Sqrt, scale=1.0 / 128, bias=cst[:, C_COL + 2:C_COL + 3]),
                 reads=[st2, cst], writes=[st2])
            P.op(P.dve, lambda e: e.reciprocal(st2[:, 2:T], st2[:, 2:T]), reads=[st2], writes=[st2])
            P.op(P.dve, lambda e: e.tensor_tensor(out=Ox, in0=Ox, in1=st2[:, 2:T].unsqueeze(2).to_broadcast([128, T - 2, 128]), op=ALU.mult), reads=[O, st2], writes=[O])
            P.op(P.dve, lambda e: e.tensor_tensor(out=Ox, in0=Ox, in1=gnb[:, h * 128:(h + 1) * 128].unsqueeze(1).to_broadcast([128, T - 2, 128]), op=ALU.mult),
                 reads=[O, gnb], writes=[O])
            P.op(P.dve, lambda e: e.tensor_tensor(out=ob[:, 2:T, :], in0=Ox, in1=gtm[:, 2:T, :], op=ALU.mult), reads=[O, gtm], writes=[ob])
            transpose_out(G, ob, oT, S["mixT"], R["mixT"], h, list(range(2, T)))


def phase_final(G):
    P, I, S, R = G.P, G.I, G.S, G.res
    with ExitStack() as es:
        gb = bc_row(G, es, "fgb", I["norm_final"][0, :], D)
        xt = [sb(G, es, "fxt", [128, D], F32) for _ in range(2)]
        yo = [sb(G, es, "fyo", [128, D], F32) for _ in range(2)]
        junk = sb(G, es, "fjunk", [128, D], BF16)
        ss = sb(G, es, "fss", [128, 4], F32)
        rs = sb(G, es, "frs", [128, 4], F32)
        for i in range(16):
            x, y = xt[i % 2], yo[i % 2]
            P.dma(x[:, :], S["xres"][(i + 2) * 128:(i + 3) * 128, :], reads=[R["xres"]], writes=[x])
            P.op(P.act, lambda e: e.activation(out=junk[:, :], in_=x[:, :], func=AF.Square, accum_out=ss[:, 0:1]), reads=[x], writes=[junk, ss])
            rstd_from_ss(G, ss, rs, 1, 1.0 / D)
            P.op(P.dve, lambda e: e.scalar_tensor_tensor(out=y[:, :], in0=x[:, :], scalar=rs[:, 0:1], in1=gb[:, :], op0=ALU.mult, op1=ALU.mult),
                 reads=[x, rs, gb], writes=[y])
            P.dma(G.out[i * 128:(i + 1) * 128, :], y[:, :], reads=[y], writes=[G.out_res], E=P.act)


_PROG = {}


def _inputs_for_core(inp, b, consts, cossin):
    f = lambda a: np.ascontiguousarray(a, dtype=np.float32)
    return {
        "x": f(inp["x"][b]), "ctx": f(inp["ctx"][b]), "c": f(inp["c"][b:b + 1]), "c_ctx": f(inp["c_ctx"][None, :]),
        "ada_w": f(inp["ada_w"]), "ada_b": f(inp["ada_b"]), "norm_mix": f(inp["norm_mix"]), "norm_ffn": f(inp["norm_ffn"]),
        "ab_w_in": f(inp["ab_w_in"][0]), "ab_w_out": f(inp["ab_w_out"][0]), "a_q_norm": f(inp["a_q_norm"]),
        "a_k_norm": f(inp["a_k_norm"]), "b_decay_exp": f(inp["b_decay_exp"].reshape(1, 16)), "b_gn": f(inp["b_gn"]),
        "c_w_in": f(inp["c_w_in"][0]), "c_w_out": f(inp["c_w_out"][0]), "c_lb": f(inp["c_lb"]), "c_gn": f(inp["c_gn"]),
        "router_w": f(inp["router_w"]), "router_b": f(inp["router_b"]),
        "w1t0": inp["_w1"][0], "w1t1": inp["_w1"][1], "w2t0": inp["_w2"][0], "w2t1": inp["_w2"][1],
        "exp_b1": f(inp["exp_b1"]), "exp_b2": f(inp["exp_b2"]),
        "norm_final": f(inp["norm_final"][None, :]), "consts": consts, "cossin": cossin,
    }


def prep_weights(inp):
    w1, w2 = inp["exp_w1"], inp["exp_w2"]
    if w1 is None:
        z = np.zeros((1, 1), np.float32)
        inp["_w1"] = [z, z]
        inp["_w2"] = [z, z]
        return
    inp["_w1"], inp["_w2"] = [], []
    for l in range(2):
        a = np.asarray(w1[l], dtype=np.float32).reshape(NE, 2, 8, 128, 16, 128, 2)
        inp["_w1"].append(np.ascontiguousarray(a.transpose(0, 4, 3, 1, 2, 6, 5)).reshape(NE * 16 * 128 * 2, 2048))
        b_ = np.asarray(w2[l], dtype=np.float32).reshape(NE, 2, 8, 128, 8, 256)
        inp["_w2"].append(np.ascontiguousarray(b_.transpose(0, 4, 3, 1, 2, 5)).reshape(NE * 8 * 128 * 2, 2048))


def kernel(**inputs):
    inp = {k: np.asarray(v) for k, v in inputs.items()}
    prep_weights(inp)
    consts, cossin = make_consts(), make_cossin()
    if "nc" not in _PROG:
        _PROG["nc"] = build_program()
    nc = _PROG["nc"]
    in_maps = [_inputs_for_core(inp, b, consts, cossin) for b in range(N_CORES)]
    res = run_bass_kernel_spmd(nc, in_maps, core_ids=list(range(N_CORES)))
    return np.stack([np.asarray(res.results[b]["out"], dtype=np.float32) for b in range(N_CORES)], axis=0)
```

```python
import math
from contextlib import ExitStack

import numpy as np
import ml_dtypes
import concourse.bass as bass
import concourse.mybir as mybir
from concourse.bass_utils import run_bass_kernel_spmd

F32 = mybir.dt.float32
BF16 = mybir.dt.bfloat16
AF = mybir.ActivationFunctionType
ALU = mybir.AluOpType
AX = mybir.AxisListType

D = 2048
KC = 16
NCTX = 256
NX = 2048
NT = NCTX + NX
T = NT // 128
NE = 32
CAP = 384
EPS = 1e-6
N_CORES = 8

C_ID = 0
C_IOTA = 128
C_US = 512
C_RGE = 640
C_RLE = 768
C_MGE = 896
C_MLE = 1024
C_JP1 = 1152
C_CMJ = 1280
C_COL = 1408
C_ONE = 1412
C_OFF1 = 1540
C_OFF2 = 1572
CW = 1588


def make_consts():
    c = np.zeros((128, CW), np.float32)
    p = np.arange(128)[:, None].astype(np.float32)
    j = np.arange(128)[None, :].astype(np.float32)
    c[:, C_ID:C_ID + 128] = np.eye(128)
    c[:, C_IOTA:C_IOTA + 384] = np.arange(384)[None, :]
    c[:, C_US:C_US + 128] = (p < j)
    c[:, C_RGE:C_RGE + 128] = np.maximum(j - p, 0)
    c[:, C_RLE:C_RLE + 128] = np.maximum(p - j, 0)
    c[:, C_MGE:C_MGE + 128] = (j >= p)
    c[:, C_MLE:C_MLE + 128] = (j <= p)
    c[:, C_JP1:C_JP1 + 128] = j + 1
    c[:, C_CMJ:C_CMJ + 128] = 128 - j
    c[:, C_COL] = 127 - p[:, 0]
    c[:, C_COL + 1] = p[:, 0]
    c[:, C_COL + 2] = EPS
    c[:, C_COL + 3] = 1.0
    c[:, C_ONE:C_ONE + 128] = 1.0
    jj = np.arange(32)
    c[:, C_OFF1:C_OFF1 + 32] = p + jj * 128
    c[:, C_OFF2:C_OFF2 + 16] = p + jj[:16] * 128
    return c


def make_cossin():
    gw, hd = 64, 128
    n = NX
    row = np.repeat(np.arange(n // gw, dtype=np.float32), gw)
    col = np.tile(np.arange(gw, dtype=np.float32), n // gw)
    nf = hd // 4
    inv = (np.float32(10000.0) ** (-np.arange(nf, dtype=np.float32) / nf)).astype(np.float32)
    ang = np.concatenate([row[:, None] * inv, col[:, None] * inv], axis=-1).astype(np.float32)
    return np.concatenate([np.cos(ang), np.sin(ang)], axis=-1).astype(np.float32)


class Res:
    __slots__ = ("w", "r")

    def __init__(self):
        self.w = None
        self.r = []


class TL:
    def __init__(self, t):
        self.t = t
        self.r = Res()

    def __getitem__(self, k):
        return self.t[k]


class Eng:
    def __init__(self, e, name, sem):
        self.e = e
        self.name = name
        self.sem = sem
        self.n = 0
        self.seen = {}


def _res(x):
    return x if isinstance(x, Res) else x.r


class Prog:
    def __init__(self, nc, es):
        self.nc = nc

        def mk(e, name):
            return Eng(e, name, es.enter_context(nc.semaphore("s_" + name)))

        self.pe = mk(nc.tensor, "pe")
        self.dve = mk(nc.vector, "dve")
        self.act = mk(nc.scalar, "act")
        self.pool = mk(nc.gpsimd, "pool")
        self.sp = mk(nc.sync, "sp")
        self.engs = [self.pe, self.dve, self.act, self.pool, self.sp]
        self.NQ = 16
        self.dq = {}
        for E in (self.sp, self.act, self.pool):
            self.dq[E.name] = dict(
                sems=[es.enter_context(nc.semaphore("d_%s%d" % (E.name, i))) for i in range(self.NQ)], i=0)
        self.rr = 0

    def _wait(self, E, ev):
        sem, val = ev
        k = id(sem)
        if E.seen.get(k, 0) < val:
            E.e.wait_ge(sem, val)
            E.seen[k] = val

    def _deps(self, E, reads, writes):
        for b in reads:
            b = _res(b)
            if b.w is not None:
                self._wait(E, b.w)
        for b in writes:
            b = _res(b)
            if b.w is not None:
                self._wait(E, b.w)
            for ev in b.r:
                self._wait(E, ev)

    def _commit(self, ev, reads, writes):
        for b in reads:
            _res(b).r.append(ev)
        for b in writes:
            b = _res(b)
            b.w = ev
            b.r = []

    def op(self, E, fn, reads=(), writes=()):
        self._deps(E, reads, writes)
        ins = fn(E.e)
        E.n += 1
        ins.then_inc(E.sem, 1)
        ev = (E.sem, E.n)
        self._commit(ev, reads, writes)
        return ev

    def mm(self, fns, reads=(), writes=()):
        E = self.pe
        self._deps(E, reads, writes)
        ins = None
        for fn in fns:
            ins = fn(E.e)
        E.n += 1
        ins.then_inc(E.sem, 1)
        ev = (E.sem, E.n)
        self._commit(ev, reads, writes)
        return ev

    def dma(self, out, in_, reads=(), writes=(), E=None, fn=None, **kw):
        if E is None:
            E = self.sp
        q = self.dq[E.name]
        i = q["i"]
        q["i"] += 1
        sem = q["sems"][i % self.NQ]
        prev = 16 * (i // self.NQ)
        if prev:
            self._wait(E, (sem, prev))
        self._deps(E, reads, writes)
        if fn is not None:
            fn(E.e).then_inc(sem, 16)
        else:
            E.e.dma_start(out=out, in_=in_, **kw).then_inc(sem, 16)
        ev = (sem, prev + 16)
        self._commit(ev, reads, writes)
        return ev

    def barrier(self):
        evs = [(E.sem, E.n) for E in self.engs if E.n > 0]
        for q in self.dq.values():
            for k, sem in enumerate(q["sems"]):
                uses = (q["i"] - k + self.NQ - 1) // self.NQ
                if uses > 0:
                    evs.append((sem, 16 * uses))
        for E in self.engs:
            for ev in evs:
                if ev[0] is not E.sem:
                    self._wait(E, ev)


class Ctx:
    pass


def build_program(debug=None, stop_after=None, dummy=()):
    debug = debug or []
    nc = bass.Bass("TRN2", target_bir_lowering=False)
    G = Ctx()
    G.nc = nc
    G.debug = debug

    def din(name, shape, dt=F32):
        if name in dummy or ("exp_w1" in dummy and name[:3] in ("w1t", "w2t")):
            shape = [1] * len(shape)
        return nc.dram_tensor(name, list(shape), dt, kind="ExternalInput").ap()

    I = {}
    I["x"] = din("x", [NX, D])
    I["ctx"] = din("ctx", [NCTX, D])
    I["c"] = din("c", [1, D])
    I["c_ctx"] = din("c_ctx", [1, D])
    I["ada_w"] = din("ada_w", [2, D, 6 * D])
    I["ada_b"] = din("ada_b", [2, 6 * D])
    I["norm_mix"] = din("norm_mix", [2, D])
    I["norm_ffn"] = din("norm_ffn", [2, D])
    I["ab_w_in"] = din("ab_w_in", [D, 5632])
    I["ab_w_out"] = din("ab_w_out", [D, D])
    I["a_q_norm"] = din("a_q_norm", [1, 128])
    I["a_k_norm"] = din("a_k_norm", [1, 128])
    I["b_decay_exp"] = din("b_decay_exp", [1, 16])
    I["b_gn"] = din("b_gn", [1, 1024])
    I["c_w_in"] = din("c_w_in", [D, 10240])
    I["c_w_out"] = din("c_w_out", [D, D])
    I["c_lb"] = din("c_lb", [2, D])
    I["c_gn"] = din("c_gn", [1, D])
    I["router_w"] = din("router_w", [2, D, NE])
    I["router_b"] = din("router_b", [2, NE])
    for l_ in range(2):
        I["w1t%d" % l_] = din("w1t%d" % l_, [NE * 16 * 128, 4096])
        I["w2t%d" % l_] = din("w2t%d" % l_, [NE * 8 * 128, 4096])
    I["exp_b1"] = din("exp_b1", [2, NE, 2 * D])
    I["exp_b2"] = din("exp_b2", [2, NE, D])
    I["norm_final"] = din("norm_final", [1, D])
    I["consts"] = din("consts", [128, CW])
    I["cossin"] = din("cossin", [NX, 128])
    G.I = I
    G.out = nc.dram_tensor("out", [NX, D], F32, kind="ExternalOutput").ap()

    def dscr(name, shape, dt):
        kind = "ExternalOutput" if name in debug else "Internal"
        return nc.dram_tensor(name, list(shape), dt, kind=kind).ap()

    S = {}
    S["xres"] = dscr("xres", [NT, D], F32)
    S["modD"] = dscr("modD", [2, 2, 6 * D], F32)
    S["QTa"] = dscr("QTa", [8, 128, NT], BF16)
    S["KTa"] = dscr("KTa", [2, 128, NT], BF16)
    S["Va"] = dscr("Va", [NT, 256], BF16)
    S["QTb"] = dscr("QTb", [8, 128, NT], BF16)
    S["KTb"] = dscr("KTb", [8, 128, NT], BF16)
    S["Kb"] = dscr("Kb", [NT, 1024], BF16)
    S["Vb"] = dscr("Vb", [NT, 1024], BF16)
    S["Gt"] = dscr("Gt", [NT, 2048], BF16)
    S["mixT"] = dscr("mixT", [16, 128, NT], BF16)
    S["outE"] = dscr("outE", [68, 256, D], BF16)
    S["hTd"] = dscr("hTd", [KC, 128, NT], BF16)
    S["h2d"] = dscr("h2d", [NT, D], BF16)
    if "dbg_h2" in debug:
        S["dbg_h2"] = dscr("dbg_h2", [NT, D], BF16)
        S["dbg_route"] = dscr("dbg_route", [3, 128, T, NE], F32)
    G.S = S
    G.res = {k: Res() for k in S}
    G.out_res = Res()

    with ExitStack() as es:
        P = Prog(nc, es)
        G.P = P
        G.ps = [TL(es.enter_context(nc.psum_tensor("ps%d" % i, [128, 512], F32))) for i in range(8)]
        G.cst = TL(es.enter_context(nc.sbuf_tensor("cst", [128, CW], F32)))
        G.idb = TL(es.enter_context(nc.sbuf_tensor("idb", [128, 128], BF16)))
        G.oneb = TL(es.enter_context(nc.sbuf_tensor("oneb", [128, 128], BF16)))
        es.enter_context(nc.Block())
        P.dma(G.cst[:, :], I["consts"][:, :], writes=[G.cst])
        P.op(P.dve, lambda e: e.tensor_copy(G.idb[:, :], G.cst[:, C_ID:C_ID + 128]), reads=[G.cst], writes=[G.idb])
        P.op(P.dve, lambda e: e.tensor_copy(G.oneb[:, :], G.cst[:, C_ONE:C_ONE + 128]), reads=[G.cst], writes=[G.oneb])

        from_phases(G, stop_after)

        P.barrier()
    return nc


class V:
    def __init__(s, ap, r):
        s.ap = ap
        s.r = r

    def __getitem__(s, k):
        return s.ap[k]


class Stop(Exception):
    pass


def from_phases(G, stop_after):
    P = G.P
    G.stop = False
    with ExitStack() as es0:
        bigA = sb(G, es0, "bigA", [128, KC * NT], BF16)
        hT = V(bigA.t[:, :].rearrange("p (k n) -> p k n", k=KC), bigA.r)
        h2 = V(bigA.t[:, :].rearrange("p (t n) -> p t n", t=T), bigA.r)

        def ph(name, fn):
            if G.stop:
                return
            fn()
            P.barrier()
            if "hTd" in G.debug and name in ("norm0", "norm1"):
                P.dma(G.S["hTd"].rearrange("k p n -> p k n"), hT[:, :, :], reads=[hT], writes=[G.res["hTd"]])
                P.barrier()
            if stop_after == name:
                G.stop = True

        for l in range(2):
            ph("mods%d" % l, lambda: phase_mods(G, l))
        for l in range(2):
            tiles = list(range(T)) if l == 0 else list(range(2, T))
            ph("norm%d" % l, lambda: phase_norm(G, l, 1, list(range(T)), hT=hT))
            if l == 0:
                ph("inproj0", lambda: phase_inproj0(G, hT))
                ph("attn", lambda: phase_attn(G))
                ph("ret", lambda: phase_ret(G))
            else:
                ph("hgrn", lambda: phase_hgrn(G, hT))
            ph("outproj%d" % l, lambda: phase_outproj(G, l, hT, tiles))
            if G.stop:
                break
            with ExitStack() as esr:
                route = dict(mask=sb(G, esr, "rmask", [128, T, NE], F32), gate=sb(G, esr, "rgate", [128, T, NE], F32),
                             pos=sb(G, esr, "rpos", [128, T, NE], F32), dsel=sb(G, esr, "rdsel", [128, 68, T], F32),
                             gsel=sb(G, esr, "rgsel", [128, 68, T], F32))
                ph("norm%db" % l, lambda: phase_norm(G, l, 2, tiles, h2=h2, route=route))
                ph("moe%d" % l, lambda: phase_moe(G, l, tiles, h2, route, bigA))
        ph("final", lambda: phase_final(G))


_UID = [0]


def sb(G, es, name, shape, dt):
    _UID[0] += 1
    return TL(es.enter_context(G.nc.sbuf_tensor("%s_%d" % (name, _UID[0]), list(shape), dt)))


def phase_mods(G, l):
    P, I, S = G.P, G.I, G.S
    with ExitStack() as es:
        cin = sb(G, es, "cin", [32, 128], F32)
        cT = sb(G, es, "cT", [128, KC, 2], F32)
        wst = [sb(G, es, "mw%d" % i, [128, KC, 512], F32) for i in range(2)]
        bia = [sb(G, es, "mb%d" % i, [2, 512], F32) for i in range(2)]
        mro = [sb(G, es, "mr%d" % i, [2, 512], F32) for i in range(2)]
        P.dma(cin[0:16, :], I["c"].rearrange("o (k p) -> (o k) p", p=128), writes=[cin])
        P.dma(cin[16:32, :], I["c_ctx"].rearrange("o (k p) -> (o k) p", p=128), writes=[cin])
        P.op(P.act, lambda e: e.activation(out=cin[:, :], in_=cin[:, :], func=AF.Silu), reads=[cin], writes=[cin])
        ps = G.ps[0]
        P.mm([lambda e: e.transpose(ps[:, 0:32], cin[:, :], G.cst[0:32, C_ID:C_ID + 32])],
             reads=[cin, G.cst], writes=[ps])
        P.op(P.dve, lambda e: e.tensor_copy(cT[:, :, :].rearrange("p k o -> p o k"),
                                            ps[:, 0:32].rearrange("p (o k) -> p o k", o=2)),
             reads=[ps], writes=[cT])
        for g in range(24):
            w = wst[g % 2]
            b = bia[g % 2]
            m = mro[g % 2]
            pq = G.ps[1 + g % 2]
            P.dma(w[:, :, :], I["ada_w"][l, :, g * 512:(g + 1) * 512].rearrange("(k p) n -> p k n", p=128), writes=[w])
            P.dma(b[:, :], I["ada_b"][l, g * 512:(g + 1) * 512].partition_broadcast(2), writes=[b], E=P.act)
            P.mm([(lambda e, k=k: e.matmul(pq[0:2, :], lhsT=cT[:, k, :], rhs=w[:, k, :], start=(k == 0), stop=(k == KC - 1)))
                  for k in range(KC)], reads=[cT, w], writes=[pq])
            P.op(P.dve, lambda e: e.tensor_tensor(out=m[:, :], in0=pq[0:2, :], in1=b[:, :], op=ALU.add),
                 reads=[pq, b], writes=[m])
            P.dma(S["modD"][l, :, g * 512:(g + 1) * 512], m[:, :], reads=[m], writes=[G.res["modD"]], E=P.pool)


def bc_row(G, es, name, row_ap, n, E=None):
    t = sb(G, es, name, [128, n], F32)
    G.P.dma(t[:, :], row_ap.partition_broadcast(128), writes=[t], E=E)
    return t


def src_rows(G, l, which, t):
    I, S = G.I, G.S
    if l == 0 and which == 1:
        return (I["ctx"][t * 128:(t + 1) * 128, :] if t < 2 else I["x"][(t - 2) * 128:(t - 1) * 128, :]), None
    return S["xres"][t * 128:(t + 1) * 128, :], G.res["xres"]


def rstd_from_ss(G, ss, rs, n, inv_n):
    P = G.P
    P.op(P.act, lambda e: e.activation(out=rs[:, 0:n], in_=ss[:, 0:n], func=AF.Sqrt, scale=inv_n,
                                       bias=G.cst[:, C_COL + 2:C_COL + 3]), reads=[ss, G.cst], writes=[rs])
    P.op(P.dve, lambda e: e.reciprocal(rs[:, 0:n], rs[:, 0:n]), reads=[rs], writes=[rs])


def phase_norm(G, l, which, tiles, hT=None, h2=None, route=None):
    P, I, S = G.P, G.I, G.S
    gain = I["norm_mix"] if which == 1 else I["norm_ffn"]
    m0 = 0 if which == 1 else 3
    with ExitStack() as es:
        gb = bc_row(G, es, "gb", gain[l, :], D)
        Gm, Sh = [], []
        for r in range(2):
            sc = bc_row(G, es, "sc", S["modD"][l, r, (m0 + 1) * D:(m0 + 2) * D], D, E=P.act)
            P.op(P.dve, lambda e, sc=sc: e.scalar_tensor_tensor(out=sc[:, :], in0=sc[:, :], scalar=1.0, in1=gb[:, :],
                                                                op0=ALU.add, op1=ALU.mult), reads=[sc, gb], writes=[sc])
            Gm.append(sc)
            Sh.append(bc_row(G, es, "sh", S["modD"][l, r, m0 * D:(m0 + 1) * D], D, E=P.act))
        xt = [sb(G, es, "xt", [128, D], F32) for _ in range(2)]
        junk = sb(G, es, "junk", [128, D], BF16)
        hf = sb(G, es, "hf", [128, D], F32)
        hb = [sb(G, es, "hb", [128, D], BF16) for _ in range(2)]
        ss = sb(G, es, "ss", [128, 4], F32)
        rs = sb(G, es, "rs", [128, 4], F32)
        if which == 2:
            rw = sb(G, es, "rw", [128, KC, NE], F32)
            P.dma(rw[:, :, :], I["router_w"][l].rearrange("(k p) n -> p k n", p=128), writes=[rw])
            rb = bc_row(G, es, "rb", I["router_b"][l, :], NE)
            hTf = sb(G, es, "hTf", [128, KC, 128], F32)
            lg = sb(G, es, "lg", [128, NE], F32)
            top = sb(G, es, "top", [128, 8], F32)
            nm = sb(G, es, "nm", [128, 1], F32)
            ex = sb(G, es, "ex", [128, NE], F32)
            se = sb(G, es, "se", [128, 1], F32)
        for i, t in enumerate(tiles):
            x = xt[i % 2]
            src, sres = src_rows(G, l, which, t)
            P.dma(x[:, :], src, reads=[sres] if sres else [], writes=[x])
            P.op(P.act, lambda e: e.activation(out=junk[:, :], in_=x[:, :], func=AF.Square, accum_out=ss[:, 0:1]),
                 reads=[x], writes=[junk, ss])
            rstd_from_ss(G, ss, rs, 1, 1.0 / D)
            r = 1 if t < 2 else 0
            P.op(P.dve, lambda e: e.scalar_tensor_tensor(out=hf[:, :], in0=x[:, :], scalar=rs[:, 0:1], in1=Gm[r][:, :],
                                                         op0=ALU.mult, op1=ALU.mult), reads=[x, rs, Gm[r]], writes=[hf])
            if which == 1:
                h = hb[i % 2]
                P.op(P.pool, lambda e: e.tensor_tensor(out=h[:, :], in0=hf[:, :], in1=Sh[r][:, :], op=ALU.add),
                     reads=[hf, Sh[r]], writes=[h])
                for half in range(2):
                    pq = G.ps[(2 * i + half) % 4]
                    pv = pq[:, 0:512].bitcast(BF16)
                    P.mm([(lambda e, k=k: e.transpose(pv[:, (k % 8) * 128:(k % 8 + 1) * 128], h[:, k * 128:(k + 1) * 128], G.idb[:, :]))
                          for k in range(half * 8, half * 8 + 8)], reads=[h, G.idb], writes=[pq])
                    eng = P.act if half == 0 else P.dve
                    P.op(eng, lambda e: e.tensor_copy(hT[:, half * 8:half * 8 + 8, t * 128:(t + 1) * 128],
                                                      pv.rearrange("p (k n) -> p k n", k=8)) if eng is P.dve else
                         e.activation(out=hT[:, half * 8:half * 8 + 8, t * 128:(t + 1) * 128],
                                      in_=pv.rearrange("p (k n) -> p k n", k=8), func=AF.Copy),
                         reads=[pq], writes=[hT])
            else:
                P.op(P.dve, lambda e: e.tensor_tensor(out=hf[:, :], in0=hf[:, :], in1=Sh[r][:, :], op=ALU.add),
                     reads=[hf, Sh[r]], writes=[hf])
                hb_ = hb[i % 2]
                P.op(P.act, lambda e: e.activation(out=hb_[:, :], in_=hf[:, :], func=AF.Copy), reads=[hf], writes=[hb_])
                P.dma(S["h2d"][t * 128:(t + 1) * 128, :], hb_[:, :], reads=[hb_], writes=[G.res["h2d"]], E=P.sp)
                for q4 in range(4):
                    pq = G.ps[q4]
                    P.mm([(lambda e, k=k: e.transpose(pq[:, (k % 4) * 128:(k % 4 + 1) * 128], hf[:, k * 128:(k + 1) * 128],
                                                      G.cst[:, C_ID:C_ID + 128])) for k in range(q4 * 4, q4 * 4 + 4)],
                         reads=[hf, G.cst], writes=[pq])
                    P.op(P.act if q4 % 2 else P.dve,
                         (lambda e: e.activation(out=hTf[:, q4 * 4:q4 * 4 + 4, :], in_=pq[:, :].rearrange("p (k n) -> p k n", k=4), func=AF.Copy))
                         if q4 % 2 else
                         (lambda e: e.tensor_copy(hTf[:, q4 * 4:q4 * 4 + 4, :], pq[:, :].rearrange("p (k n) -> p k n", k=4))),
                         reads=[pq], writes=[hTf])
                pl = G.ps[4]
                P.mm([(lambda e, k=k: e.matmul(pl[:, 0:NE], lhsT=hTf[:, k, :], rhs=rw[:, k, :], start=(k == 0), stop=(k == KC - 1)))
                      for k in range(KC)], reads=[hTf, rw], writes=[pl])
                P.op(P.dve, lambda e: e.tensor_tensor(out=lg[:, :], in0=pl[:, 0:NE], in1=rb[:, :], op=ALU.add),
                     reads=[pl, rb], writes=[lg])
                P.op(P.dve, lambda e: e.max(top[:, :], lg[:, :]), reads=[lg], writes=[top])
                mk, gt = route["mask"], route["gate"]
                P.op(P.dve, lambda e: e.tensor_scalar(out=mk[:, t, :], in0=lg[:, :], scalar1=top[:, 3:4], scalar2=None, op0=ALU.is_ge),
                     reads=[lg, top], writes=[mk])
                P.op(P.dve, lambda e: e.tensor_scalar(out=nm[:, :], in0=top[:, 0:1], scalar1=-1.0, scalar2=None, op0=ALU.mult),
                     reads=[top], writes=[nm])
                P.op(P.act, lambda e: e.activation(out=ex[:, :], in_=lg[:, :], func=AF.Exp, bias=nm[:, 0:1]),
                     reads=[lg, nm], writes=[ex])
                P.op(P.dve, lambda e: e.tensor_tensor(out=ex[:, :], in0=ex[:, :], in1=mk[:, t, :], op=ALU.mult),
                     reads=[ex, mk], writes=[ex])
                P.op(P.dve, lambda e: e.tensor_reduce(out=se[:, :], in_=ex[:, :], axis=AX.X, op=ALU.add), reads=[ex], writes=[se])
                P.op(P.dve, lambda e: e.reciprocal(se[:, :], se[:, :]), reads=[se], writes=[se])
                P.op(P.dve, lambda e: e.tensor_scalar(out=gt[:, t, :], in0=ex[:, :], scalar1=se[:, 0:1], scalar2=None, op0=ALU.mult),
                     reads=[ex, se], writes=[gt])
        if which == 2:
            pos = route["pos"]
            mk = route["mask"]
            for i, t in enumerate(tiles):
                pq = G.ps[5 + i % 2]
                fns = [(lambda e, tp=tp, j=j: e.matmul(pq[:, 0:NE], lhsT=G.cst[:, C_ONE:C_ONE + 128], rhs=mk[:, tp, :],
                                                      start=(j == 0), stop=False)) for j, tp in enumerate(tiles[:i])]
                fns.append(lambda e, i=i, t=t: e.matmul(pq[:, 0:NE], lhsT=G.cst[:, C_US:C_US + 128], rhs=mk[:, t, :],
                                                      start=(i == 0), stop=True))
                P.mm(fns, reads=[mk, G.cst], writes=[pq])
                P.op(P.act, lambda e: e.activation(out=pos[:, t, :], in_=pq[:, 0:NE], func=AF.Copy), reads=[pq], writes=[pos])


def linear_tm(G, es, W, groups, tiles, hT, post):
    P = G.P
    wst = [sb(G, es, "wst", [128, 8, 512], F32) for _ in range(2)]
    wb = [sb(G, es, "wb", [128, KC, 512], BF16) for _ in range(2)]
    cnt = 0
    for gi, (c0, n) in enumerate(groups):
        w = wb[gi % 2]
        for half in range(2):
            st = wst[half]
            P.dma(st[:, :, 0:n], W[half * 1024:(half + 1) * 1024, c0:c0 + n].rearrange("(k p) n -> p k n", p=128),
                  writes=[st], E=(P.sp if half == 0 else P.act))
            if half == 0:
                P.op(P.pool, lambda e: e.tensor_copy(w[:, 0:8, 0:n], st[:, :, 0:n]), reads=[st], writes=[w])
            else:
                P.op(P.act, lambda e: e.activation(out=w[:, 8:16, 0:n], in_=st[:, :, 0:n], func=AF.Copy), reads=[st], writes=[w])
        for t in tiles:
            pq = G.ps[cnt % 3]
            cnt += 1
            P.mm([(lambda e, k=k: e.matmul(pq[:, 0:n], lhsT=hT[:, k, t * 128:(t + 1) * 128], rhs=w[:, k, 0:n],
                                           start=(k == 0), stop=(k == KC - 1))) for k in range(KC)],
                 reads=[hT, w], writes=[pq])
            post(gi, t, pq, n)


def rope_heads(G, src, dst, cs, t, H, tmp):
    P = G.P
    a = src[:, 0:H, 0:64]
    b = src[:, 0:H, 64:128]
    cosb = cs[:, t - 2:t - 1, 0:64].to_broadcast([128, H, 64])
    sinb = cs[:, t - 2:t - 1, 64:128].to_broadcast([128, H, 64])
    t1, t2 = tmp[:, 0:H, 0:64], tmp[:, 0:H, 64:128]
    P.op(P.dve, lambda e: e.tensor_tensor(out=t1, in0=a, in1=cosb, op=ALU.mult), reads=[src, cs], writes=[tmp])
    P.op(P.dve, lambda e: e.tensor_tensor(out=t2, in0=b, in1=sinb, op=ALU.mult), reads=[src, cs], writes=[tmp])
    P.op(P.dve, lambda e: e.tensor_tensor(out=dst[:, 0:H, 0:64], in0=t1, in1=t2, op=ALU.subtract), reads=[tmp], writes=[dst])
    P.op(P.dve, lambda e: e.tensor_tensor(out=t1, in0=a, in1=sinb, op=ALU.mult), reads=[src, cs, dst], writes=[tmp])
    P.op(P.dve, lambda e: e.tensor_tensor(out=t2, in0=b, in1=cosb, op=ALU.mult), reads=[src, cs], writes=[tmp])
    P.op(P.dve, lambda e: e.tensor_tensor(out=dst[:, 0:H, 64:128], in0=t1, in1=t2, op=ALU.add), reads=[tmp], writes=[dst])


def phase_inproj0(G, hT):
    P, I, S = G.P, G.I, G.S
    groups = [(i * 512, 512) for i in range(11)]
    with ExitStack() as es:
        cs = sb(G, es, "cs", [128, 16, 128], F32)
        P.dma(cs[:, :, :], I["cossin"].rearrange("(t p) n -> p t n", p=128), writes=[cs])
        qg = bc_row(G, es, "qg", I["a_q_norm"][0, :], 128)
        kg = bc_row(G, es, "kg", I["a_k_norm"][0, :], 128)
        src = [sb(G, es, "src", [128, 4, 128], F32) for _ in range(2)]
        sq = sb(G, es, "sq", [128, 4, 128], F32)
        tmp = sb(G, es, "tmp", [128, 4, 128], F32)
        ob = [sb(G, es, "ob", [128, 4, 128], BF16) for _ in range(2)]
        oT = [sb(G, es, "oT", [128, 4, 128], BF16) for _ in range(2)]
        ss = sb(G, es, "ss", [128, 4], F32)
        rs = sb(G, es, "rs", [128, 4], F32)
        st = dict(i=0)

        def heads(pq, t, H, c_lo, gain, rope, scale, dT, h0, dTM, tm_c0):
            i = st["i"]
            st["i"] += 1
            s_, o_, oT_ = src[i % 2], ob[i % 2], oT[i % 2]
            pv = pq[:, c_lo:c_lo + H * 128].rearrange("p (h n) -> p h n", h=H)
            P.op(P.act, lambda e: e.activation(out=s_[:, 0:H, :], in_=pv, func=AF.Copy, scale=scale), reads=[pq], writes=[s_])
            if gain is not None:
                P.op(P.act, lambda e: e.activation(out=sq[:, 0:H, :], in_=s_[:, 0:H, :], func=AF.Square), reads=[s_], writes=[sq])
                P.op(P.dve, lambda e: e.tensor_reduce(out=ss[:, 0:H], in_=sq[:, 0:H, :], axis=AX.X, op=ALU.add), reads=[sq], writes=[ss])
                rstd_from_ss(G, ss, rs, H, 1.0 / 128)
                P.op(P.dve, lambda e: e.tensor_tensor(out=s_[:, 0:H, :], in0=s_[:, 0:H, :],
                                                      in1=rs[:, 0:H].unsqueeze(2).to_broadcast([128, H, 128]), op=ALU.mult),
                     reads=[s_, rs], writes=[s_])
                P.op(P.dve, lambda e: e.tensor_tensor(out=s_[:, 0:H, :], in0=s_[:, 0:H, :],
                                                      in1=gain[:, :].unsqueeze(1).to_broadcast([128, H, 128]), op=ALU.mult),
                     reads=[s_, gain], writes=[s_])
            if rope and t >= 2:
                rope_heads(G, s_, o_, cs, t, H, tmp)
            else:
                P.op(P.pool, lambda e: e.tensor_copy(o_[:, 0:H, :], s_[:, 0:H, :]), reads=[s_], writes=[o_])
            if dTM is not None:
                P.dma(dTM[0][t * 128:(t + 1) * 128, tm_c0:tm_c0 + H * 128], o_[:, 0:H, :].rearrange("p h n -> p (h n)"),
                      reads=[o_], writes=[dTM[1]], E=P.pool)
            if dT is not None:
                pt = G.ps[4 + i % 2]
                pvb = pt[:, 0:256].bitcast(BF16)
                P.mm([(lambda e, h=h: e.transpose(pvb[:, h * 128:(h + 1) * 128], o_[:, h, :], G.idb[:, :])) for h in range(H)],
                     reads=[o_, G.idb], writes=[pt])
                P.op(P.act, lambda e: e.activation(out=oT_[:, 0:H, :], in_=pvb[:, 0:H * 128].rearrange("p (h n) -> p h n", h=H), func=AF.Copy),
                     reads=[pt], writes=[oT_])
                P.dma(dT[0][h0:h0 + H, :, t * 128:(t + 1) * 128].rearrange("h d n -> d h n"), oT_[:, 0:H, :],
                      reads=[oT_], writes=[dT[1]], E=P.pool)

        R = G.res
        ksc = 128.0 ** -0.5

        def post(gi, t, pq, n):
            if gi == 0:
                heads(pq, t, 2, 0, kg, True, 1.0, (S["KTa"], R["KTa"]), 0, None, 0)
                heads(pq, t, 2, 256, None, False, 1.0, None, 0, (S["Va"], R["Va"]), 0)
            elif gi in (1, 2):
                heads(pq, t, 4, 0, None, True, ksc, (S["KTb"], R["KTb"]), (gi - 1) * 4, (S["Kb"], R["Kb"]), (gi - 1) * 512)
            elif gi in (3, 4):
                heads(pq, t, 4, 0, None, False, 1.0, None, 0, (S["Vb"], R["Vb"]), (gi - 3) * 512)
            elif gi in (5, 6):
                heads(pq, t, 4, 0, qg, True, 1.0, (S["QTa"], R["QTa"]), (gi - 5) * 4, None, 0)
            elif gi in (7, 8):
                heads(pq, t, 4, 0, None, True, 1.0, (S["QTb"], R["QTb"]), (gi - 7) * 4, None, 0)
            else:
                i = st["i"]
                st["i"] += 1
                o_ = ob[i % 2]
                P.op(P.act, lambda e: e.activation(out=o_[:, :, :], in_=pq[:, 0:512].rearrange("p (h n) -> p h n", h=4), func=AF.Silu),
                     reads=[pq], writes=[o_])
                P.dma(S["Gt"][t * 128:(t + 1) * 128, (gi - 9) * 512:(gi - 8) * 512], o_[:, :, :].rearrange("p h n -> p (h n)"),
                      reads=[o_], writes=[R["Gt"]], E=P.pool)

        linear_tm(G, es, I["ab_w_in"], groups, list(range(T)), hT, post)


def phase_attn(G):
    P, S, R = G.P, G.S, G.res
    sc = 128.0 ** -0.5
    with ExitStack() as es:
        KT = sb(G, es, "KT", [128, NT], BF16)
        V = sb(G, es, "V", [128, T, 128], BF16)
        QT = [sb(G, es, "QT", [128, 512], BF16) for _ in range(2)]
        PT = [sb(G, es, "PT", [128, 512], BF16) for _ in range(3)]
        rz = sb(G, es, "rz", [128, 512], F32)
        OT = [sb(G, es, "OT", [128, 512], BF16) for _ in range(2)]
        it = 0
        for hk in range(2):
            P.dma(KT[:, :], S["KTa"][hk, :, :], reads=[R["KTa"]], writes=[KT])
            P.dma(V[:, :, :], S["Va"][:, hk * 128:(hk + 1) * 128].rearrange("(t p) n -> p t n", p=128), reads=[R["Va"]], writes=[V])
            for h in range(hk * 4, hk * 4 + 4):
                chunks = [(0, 256, [0, 1])] + [(256 + 512 * i, 512, list(range(T))) for i in range(4)]
                for (q0, nq, keys) in chunks:
                    q = QT[it % 2]
                    o = OT[it % 2]
                    it += 1
                    P.dma(q[:, 0:nq], S["QTa"][h, :, q0:q0 + nq], reads=[R["QTa"]], writes=[q], E=P.act)
                    po, pz = G.ps[6], G.ps[7]
                    nk = len(keys)
                    for j, kt in enumerate(keys):
                        pS = G.ps[j % 3]
                        p_ = PT[j % 3]
                        P.mm([lambda e: e.matmul(pS[:, 0:nq], lhsT=KT[:, kt * 128:(kt + 1) * 128], rhs=q[:, 0:nq], start=True, stop=True)],
                             reads=[KT, q], writes=[pS])
                        P.op(P.act, lambda e: e.activation(out=p_[:, 0:nq], in_=pS[:, 0:nq], func=AF.Exp, scale=sc), reads=[pS], writes=[p_])
                        P.mm([lambda e: e.matmul(po[:, 0:nq], lhsT=V[:, kt, :], rhs=p_[:, 0:nq], start=(j == 0), stop=(j == nk - 1)),
                              lambda e: e.matmul(pz[:, 0:nq], lhsT=G.oneb[:, :], rhs=p_[:, 0:nq], start=(j == 0), stop=(j == nk - 1))],
                             reads=[V, p_, G.oneb], writes=[po, pz])
                    P.op(P.dve, lambda e: e.reciprocal(rz[:, 0:nq], pz[:, 0:nq]), reads=[pz], writes=[rz])
                    P.op(P.dve, lambda e: e.tensor_tensor(out=o[:, 0:nq], in0=po[:, 0:nq], in1=rz[:, 0:nq], op=ALU.mult),
                         reads=[po, rz], writes=[o])
                    P.dma(S["mixT"][h, :, q0:q0 + nq], o[:, 0:nq], reads=[o], writes=[R["mixT"]], E=P.pool)


def phase_ret(G):
    P, I, S, R = G.P, G.I, G.S, G.res
    fwd = list(range(T))
    bwd = [1, 0] + list(range(T - 1, 1, -1))
    with ExitStack() as es:
        dexp = bc_row(G, es, "dexp", I["b_decay_exp"][0, :], 16)
        lgm = sb(G, es, "lgm", [128, 16], F32)
        P.op(P.act, lambda e: e.activation(out=lgm[:, :], in_=dexp[:, :], func=AF.Exp, scale=-math.log(2.0)), reads=[dexp], writes=[lgm])
        P.op(P.act, lambda e: e.activation(out=lgm[:, :], in_=lgm[:, :], func=AF.Ln, scale=-1.0, bias=G.cst[:, C_COL + 3:C_COL + 4]),
             reads=[lgm, G.cst], writes=[lgm])
        gnb = bc_row(G, es, "gnb", I["b_gn"][0, :], 1024)
        QT = sb(G, es, "rQT", [128, NT], BF16)
        KT = sb(G, es, "rKT", [128, NT], BF16)
        Kt = sb(G, es, "rK", [128, T, 128], BF16)
        Vt = sb(G, es, "rV", [128, T, 128], BF16)
        Gt = sb(G, es, "rG", [128, T, 128], BF16)
        Dm = sb(G, es, "Dm", [128, 128], F32)
        Dt = sb(G, es, "Dt", [128, 128], F32)
        qdf = sb(G, es, "qdf", [128, 128], F32)
        qdb = sb(G, es, "qdb", [128, 128], F32)
        col = sb(G, es, "col", [128, 4], F32)
        Sb = sb(G, es, "Sb", [128, T, 128], BF16)
        Sf32 = sb(G, es, "Sf32", [128, 128], F32)
        Sfb = [sb(G, es, "Sfb", [128, 128], BF16) for _ in range(2)]
        Kd = [sb(G, es, "Kd", [128, 128], BF16) for _ in range(2)]
        AT = [sb(G, es, "AT", [128, 128], BF16) for _ in range(2)]
        Qf = [sb(G, es, "Qf", [128, 128], BF16) for _ in range(2)]
        Qb = [sb(G, es, "Qb", [128, 128], BF16) for _ in range(2)]
        O = sb(G, es, "O", [128, T, 128], F32)
        sq = sb(G, es, "osq", [128, T, 128], F32)
        st1 = sb(G, es, "st1", [128, T], F32)
        st2 = sb(G, es, "st2", [128, T], F32)
        ob = sb(G, es, "rob", [128, T, 128], BF16)
        oT = [sb(G, es, "roT", [128, 4, 128], BF16) for _ in range(2)]
        cst = G.cst
        for h in range(8):
            P.dma(QT[:, :], S["QTb"][h, :, :], reads=[R["QTb"]], writes=[QT])
            P.dma(KT[:, :], S["KTb"][h, :, :], reads=[R["KTb"]], writes=[KT], E=P.act)
            P.dma(Kt[:, :, :], S["Kb"][:, h * 128:(h + 1) * 128].rearrange("(t p) n -> p t n", p=128), reads=[R["Kb"]], writes=[Kt])
            P.dma(Vt[:, :, :], S["Vb"][:, h * 128:(h + 1) * 128].rearrange("(t p) n -> p t n", p=128), reads=[R["Vb"]], writes=[Vt], E=P.act)
            P.dma(Gt[:, :, :], S["Gt"][:, h * 128:(h + 1) * 128].rearrange("(t p) n -> p t n", p=128), reads=[R["Gt"]], writes=[Gt])
            lf, lb_ = lgm[:, h:h + 1], lgm[:, 8 + h:9 + h]
            P.op(P.act, lambda e: e.activation(out=Dm[:, :], in_=cst[:, C_RGE:C_RGE + 128], func=AF.Exp, scale=lf), reads=[cst, lgm], writes=[Dm])
            P.op(P.dve, lambda e: e.tensor_tensor(out=Dm[:, :], in0=Dm[:, :], in1=cst[:, C_MGE:C_MGE + 128], op=ALU.mult), reads=[Dm, cst], writes=[Dm])
            P.op(P.act, lambda e: e.activation(out=Dt[:, :], in_=cst[:, C_RLE:C_RLE + 128], func=AF.Exp, scale=lb_), reads=[cst, lgm], writes=[Dt])
            P.op(P.dve, lambda e: e.tensor_tensor(out=Dt[:, :], in0=Dt[:, :], in1=cst[:, C_MLE:C_MLE + 128], op=ALU.mult), reads=[Dt, cst], writes=[Dt])
            P.op(P.dve, lambda e: e.tensor_tensor(out=Dm[:, :], in0=Dm[:, :], in1=Dt[:, :], op=ALU.add), reads=[Dm, Dt], writes=[Dm])
            P.op(P.act, lambda e: e.activation(out=qdf[:, :], in_=cst[:, C_JP1:C_JP1 + 128], func=AF.Exp, scale=lf), reads=[cst, lgm], writes=[qdf])
            P.op(P.act, lambda e: e.activation(out=qdb[:, :], in_=cst[:, C_CMJ:C_CMJ + 128], func=AF.Exp, scale=lb_), reads=[cst, lgm], writes=[qdb])
            P.op(P.act, lambda e: e.activation(out=col[:, 0:1], in_=cst[:, C_COL:C_COL + 1], func=AF.Exp, scale=lf), reads=[cst, lgm], writes=[col])
            P.op(P.act, lambda e: e.activation(out=col[:, 1:2], in_=cst[:, C_COL + 1:C_COL + 2], func=AF.Exp, scale=lb_), reads=[cst, lgm], writes=[col])
            P.op(P.act, lambda e: e.activation(out=col[:, 2:3], in_=lf, func=AF.Exp, scale=128.0), reads=[lgm], writes=[col])
            P.op(P.act, lambda e: e.activation(out=col[:, 3:4], in_=lb_, func=AF.Exp, scale=128.0), reads=[lgm], writes=[col])

            def state_step(c, kcol, gcol, i):
                kd = Kd[i % 2]
                pu = G.ps[4 + i % 2]
                P.op(P.pool, lambda e: e.tensor_scalar(out=kd[:, :], in0=Kt[:, c, :], scalar1=col[:, kcol:kcol + 1], scalar2=None, op0=ALU.mult),
                     reads=[Kt, col], writes=[kd])
                P.mm([lambda e: e.matmul(pu[:, 0:128], lhsT=kd[:, :], rhs=Vt[:, c, :], start=True, stop=True)], reads=[kd, Vt], writes=[pu])
                P.op(P.dve, lambda e: e.scalar_tensor_tensor(out=Sf32[:, :], in0=Sf32[:, :], scalar=col[:, gcol:gcol + 1], in1=pu[:, 0:128],
                                                             op0=ALU.mult, op1=ALU.add), reads=[Sf32, col, pu], writes=[Sf32])

            P.op(P.dve, lambda e: e.memset(Sf32[:, :], 0.0), writes=[Sf32])
            for i, c in enumerate(bwd):
                P.op(P.act, lambda e: e.activation(out=Sb[:, c, :], in_=Sf32[:, :], func=AF.Copy), reads=[Sf32], writes=[Sb])
                if i < T - 1:
                    state_step(c, 1, 3, i)
            P.op(P.dve, lambda e: e.memset(Sf32[:, :], 0.0), writes=[Sf32])
            for i, c in enumerate(fwd):
                sfb, at, qf, qb = Sfb[i % 2], AT[i % 2], Qf[i % 2], Qb[i % 2]
                pS, pO = G.ps[i % 2], G.ps[2 + i % 2]
                P.op(P.act, lambda e: e.activation(out=sfb[:, :], in_=Sf32[:, :], func=AF.Copy), reads=[Sf32], writes=[sfb])
                P.mm([lambda e: e.matmul(pS[:, 0:128], lhsT=KT[:, c * 128:(c + 1) * 128], rhs=QT[:, c * 128:(c + 1) * 128], start=True, stop=True)],
                     reads=[KT, QT], writes=[pS])
                P.op(P.dve, lambda e: e.tensor_tensor(out=at[:, :], in0=pS[:, 0:128], in1=Dm[:, :], op=ALU.mult), reads=[pS, Dm], writes=[at])
                P.op(P.pool, lambda e: e.tensor_tensor(out=qf[:, :], in0=QT[:, c * 128:(c + 1) * 128], in1=qdf[:, :], op=ALU.mult), reads=[QT, qdf], writes=[qf])
                P.op(P.pool, lambda e: e.tensor_tensor(out=qb[:, :], in0=QT[:, c * 128:(c + 1) * 128], in1=qdb[:, :], op=ALU.mult), reads=[QT, qdb], writes=[qb])
                P.mm([lambda e: e.matmul(pO[:, 0:128], lhsT=at[:, :], rhs=Vt[:, c, :], start=True, stop=False),
                      lambda e: e.matmul(pO[:, 0:128], lhsT=qf[:, :], rhs=sfb[:, :], start=False, stop=False),
                      lambda e: e.matmul(pO[:, 0:128], lhsT=qb[:, :], rhs=Sb[:, c, :], start=False, stop=True)],
                     reads=[at, Vt, qf, sfb, qb, Sb], writes=[pO])
                P.op(P.act, lambda e: e.activation(out=O[:, c, :], in_=pO[:, 0:128], func=AF.Copy), reads=[pO], writes=[O])
                if i < T - 1:
                    state_step(c, 0, 2, i)
            P.op(P.dve, lambda e: e.tensor_reduce(out=st1[:, :], in_=O[:, :, :], axis=AX.X, op=ALU.add), reads=[O], writes=[st1])
            P.op(P.dve, lambda e: e.tensor_scalar(out=st1[:, :], in0=st1[:, :], scalar1=1.0 / 128, scalar2=None, op0=ALU.mult), reads=[st1], writes=[st1])
            P.op(P.dve, lambda e: e.tensor_tensor(out=O[:, :, :], in0=O[:, :, :], in1=st1[:, :].unsqueeze(2).to_broadcast([128, T, 128]), op=ALU.subtract),
                 reads=[O, st1], writes=[O])
            P.op(P.act, lambda e: e.activation(out=sq[:, :, :], in_=O[:, :, :], func=AF.Square), reads=[O], writes=[sq])
            P.op(P.dve, lambda e: e.tensor_reduce(out=st2[:, :], in_=sq[:, :, :], axis=AX.X, op=ALU.add), reads=[sq], writes=[st2])
            P.op(P.act, lambda e: e.activation(out=st2[:, :], in_=st2[:, :], func=AF.Sqrt, scale=1.0 / 128, bias=cst[:, C_COL + 2:C_COL + 3]),
                 reads=[st2, cst], writes=[st2])
            P.op(P.dve, lambda e: e.reciprocal(st2[:, :], st2[:, :]), reads=[st2], writes=[st2])
            P.op(P.dve, lambda e: e.tensor_tensor(out=O[:, :, :], in0=O[:, :, :], in1=st2[:, :].unsqueeze(2).to_broadcast([128, T, 128]), op=ALU.mult),
                 reads=[O, st2], writes=[O])
            P.op(P.dve, lambda e: e.tensor_tensor(out=O[:, :, :], in0=O[:, :, :], in1=gnb[:, h * 128:(h + 1) * 128].unsqueeze(1).to_broadcast([128, T, 128]), op=ALU.mult),
                 reads=[O, gnb], writes=[O])
            P.op(P.dve, lambda e: e.tensor_tensor(out=ob[:, :, :], in0=O[:, :, :], in1=Gt[:, :, :], op=ALU.mult), reads=[O, Gt], writes=[ob])
            transpose_out(G, ob, oT, S["mixT"], R["mixT"], 8 + h, list(range(T)))


def transpose_out(G, ob, oT, dst, dres, chunk, tiles):
    P = G.P
    for g0 in range(0, len(tiles), 4):
        ts = tiles[g0:g0 + 4]
        n = len(ts)
        pt = G.ps[6 + (g0 // 4) % 2]
        o_ = oT[(g0 // 4) % 2]
        pvb = pt[:, 0:256].bitcast(BF16)
        P.mm([(lambda e, j=j, t=t: e.transpose(pvb[:, j * 128:(j + 1) * 128], ob[:, t, :], G.idb[:, :])) for j, t in enumerate(ts)],
             reads=[ob, G.idb], writes=[pt])
        P.op(P.act, lambda e: e.activation(out=o_[:, 0:n, :], in_=pvb[:, 0:n * 128].rearrange("p (h n) -> p h n", h=n), func=AF.Copy),
             reads=[pt], writes=[o_])
        P.dma(dst[chunk, :, ts[0] * 128:(ts[0] + n) * 128], o_[:, 0:n, :].rearrange("p h n -> p (h n)"), reads=[o_], writes=[dres], E=P.pool)


def phase_outproj(G, l, hT, tiles):
    P, I, S, R = G.P, G.I, G.S, G.res
    W = I["ab_w_out"] if l == 0 else I["c_w_out"]
    with ExitStack() as es:
        P.dma(hT[:, :, :], S["mixT"].rearrange("k p n -> p k n"), reads=[R["mixT"]], writes=[hT])
        gate = [bc_row(G, es, "og", S["modD"][l, r, 2 * D:3 * D], D, E=P.act) for r in range(2)]
        xo = [sb(G, es, "xo", [128, 512], F32) for _ in range(2)]
        xn = [sb(G, es, "xn", [128, 512], F32) for _ in range(2)]
        st = dict(i=0)

        def post(gi, t, pq, n):
            i = st["i"]
            st["i"] += 1
            a, b = xo[i % 2], xn[i % 2]
            if l == 0:
                src = I["ctx"][t * 128:(t + 1) * 128, gi * 512:(gi + 1) * 512] if t < 2 else I["x"][(t - 2) * 128:(t - 1) * 128, gi * 512:(gi + 1) * 512]
                rd = []
            else:
                src = S["xres"][t * 128:(t + 1) * 128, gi * 512:(gi + 1) * 512]
                rd = [R["xres"]]
            P.dma(a[:, :], src, reads=rd, writes=[a], E=P.act)
            r = 1 if t < 2 else 0
            P.op(P.dve, lambda e: e.tensor_tensor(out=b[:, :], in0=pq[:, 0:512], in1=gate[r][:, gi * 512:(gi + 1) * 512], op=ALU.mult),
                 reads=[pq, gate[r]], writes=[b])
            P.op(P.pool, lambda e: e.tensor_tensor(out=b[:, :], in0=b[:, :], in1=a[:, :], op=ALU.add), reads=[b, a], writes=[b])
            P.dma(S["xres"][t * 128:(t + 1) * 128, gi * 512:(gi + 1) * 512], b[:, :], reads=[b], writes=[R["xres"]], E=P.pool)

        linear_tm(G, es, W, [(i * 512, 512) for i in range(4)], tiles, hT, post)


def phase_moe(G, l, tiles, h2, route, bigA):
    P, I, S, R, nc = G.P, G.I, G.S, G.res, G.nc
    nT = len(tiles)
    NB = -(-(4 * nT * 128 + NE * 255) // 256)
    BIG = 1.0e6
    mk, gt, pos = route["mask"], route["gate"], route["pos"]
    dselA, gselA = route["dsel"], route["gsel"]
    cst = G.cst
    t0, t1 = tiles[0], tiles[-1] + 1
    with ExitStack() as es:
        b1T = sb(G, es, "b1T", [128, 2 * KC, NE], F32)
        with ExitStack() as es1:
            b1s = sb(G, es1, "b1s", [NE, 2 * D], F32)
            P.dma(b1s[:, :], I["exp_b1"][l, :, :], writes=[b1s])
            for two in range(2):
                pq = G.ps[two]
                P.mm([(lambda e, c=c: e.transpose(pq[:, c * 32:(c + 1) * 32], b1s[:, c * 256 + two:(c + 1) * 256:2], cst[0:32, C_ID:C_ID + 32]))
                      for c in range(KC)], reads=[b1s, cst], writes=[pq])
                P.op(P.dve, lambda e: e.tensor_copy(b1T[:, two:2 * KC:2, :], pq[:, :].rearrange("p (c n) -> p c n", c=KC)), reads=[pq], writes=[b1T])
            P.barrier()
        cnt = sb(G, es, "cnt", [128, NE], F32)
        nblk = sb(G, es, "nblk", [128, NE], F32)
        bend = sb(G, es, "bend", [128, NE], F32)
        bst = sb(G, es, "bst", [128, NE], F32)
        tmpe = sb(G, es, "tmpe", [128, NE], F32)
        destm = sb(G, es, "destm", [128, T, NE], F32)
        oh = sb(G, es, "oh", [128, NB, NE], F32)
        oh2 = sb(G, es, "oh2", [128, NB, NE], F32)
        bef = sb(G, es, "bef", [128, NB], F32)
        bei = sb(G, es, "bei", [128, NB], mybir.dt.int32)
        tmp3 = sb(G, es, "tmp3", [128, T, NE], F32)
        b1tmp = sb(G, es, "b1tmp", [128, 2 * KC, NE], F32)
        b1sel = [sb(G, es, "b1sel", [128, 2 * KC], F32) for _ in range(2)]
        pq = G.ps[0]
        P.mm([(lambda e, j=j, t=t: e.matmul(pq[:, 0:NE], lhsT=cst[:, C_ONE:C_ONE + 128], rhs=mk[:, t, :], start=(j == 0), stop=(j == nT - 1)))
              for j, t in enumerate(tiles)], reads=[mk, cst], writes=[pq])
        P.op(P.dve, lambda e: e.tensor_copy(cnt[:, :], pq[:, 0:NE]), reads=[pq], writes=[cnt])
        P.op(P.dve, lambda e: e.tensor_scalar(out=nblk[:, :], in0=cnt[:, :], scalar1=0.0, scalar2=None, op0=ALU.is_gt), reads=[cnt], writes=[nblk])
        for j in range(1, 9):
            P.op(P.dve, lambda e: e.tensor_scalar(out=tmpe[:, :], in0=cnt[:, :], scalar1=256.0 * j, scalar2=None, op0=ALU.is_gt), reads=[cnt], writes=[tmpe])
            P.op(P.dve, lambda e: e.tensor_tensor(out=nblk[:, :], in0=nblk[:, :], in1=tmpe[:, :], op=ALU.add), reads=[nblk, tmpe], writes=[nblk])
        P.op(P.dve, lambda e: e.tensor_tensor_scan(out=bend[:, :], data0=cst[:, C_ONE:C_ONE + NE], data1=nblk[:, :], initial=0.0,
                                                   op0=ALU.mult, op1=ALU.add), reads=[cst, nblk], writes=[bend])
        P.op(P.dve, lambda e: e.tensor_tensor(out=bst[:, :], in0=bend[:, :], in1=nblk[:, :], op=ALU.subtract), reads=[bend, nblk], writes=[bst])
        P.op(P.dve, lambda e: e.tensor_scalar(out=tmpe[:, :], in0=bst[:, :], scalar1=256.0, scalar2=BIG, op0=ALU.mult, op1=ALU.add), reads=[bst], writes=[tmpe])
        P.op(P.dve, lambda e: e.tensor_tensor(out=destm[:, t0:t1, :], in0=pos[:, t0:t1, :], in1=tmpe[:, :].unsqueeze(1).to_broadcast([128, nT, NE]), op=ALU.add),
             reads=[pos, tmpe], writes=[destm])
        P.op(P.dve, lambda e: e.tensor_tensor(out=destm[:, t0:t1, :], in0=destm[:, t0:t1, :], in1=mk[:, t0:t1, :], op=ALU.mult), reads=[destm, mk], writes=[destm])
        P.op(P.dve, lambda e: e.tensor_scalar(out=destm[:, t0:t1, :], in0=destm[:, t0:t1, :], scalar1=-BIG, scalar2=None, op0=ALU.add), reads=[destm], writes=[destm])
        iob = cst[:, C_IOTA:C_IOTA + NB].unsqueeze(2).to_broadcast([128, NB, NE])
        P.op(P.dve, lambda e: e.tensor_tensor(out=oh[:, :, :], in0=iob, in1=bst[:, :].unsqueeze(1).to_broadcast([128, NB, NE]), op=ALU.is_ge), reads=[cst, bst], writes=[oh])
        P.op(P.dve, lambda e: e.tensor_tensor(out=oh2[:, :, :], in0=iob, in1=bend[:, :].unsqueeze(1).to_broadcast([128, NB, NE]), op=ALU.is_lt), reads=[cst, bend], writes=[oh2])
        P.op(P.dve, lambda e: e.tensor_tensor(out=oh[:, :, :], in0=oh[:, :, :], in1=oh2[:, :, :], op=ALU.mult), reads=[oh, oh2], writes=[oh])
        P.op(P.dve, lambda e: e.tensor_tensor(out=oh2[:, :, :], in0=oh[:, :, :], in1=cst[:, C_IOTA:C_IOTA + NE].unsqueeze(1).to_broadcast([128, NB, NE]), op=ALU.mult),
             reads=[oh, cst], writes=[oh2])
        P.op(P.dve, lambda e: e.tensor_reduce(out=bef[:, :], in_=oh2[:, :, :], axis=AX.X, op=ALU.add), reads=[oh2], writes=[bef])
        P.op(P.dve, lambda e: e.tensor_copy(bei[:, :], bef[:, :]), reads=[bef], writes=[bei])
        for b in range(NB):
            ohb = oh[:, b:b + 1, :].to_broadcast([128, nT, NE])
            P.op(P.dve, lambda e: e.tensor_tensor(out=tmp3[:, t0:t1, :], in0=destm[:, t0:t1, :], in1=ohb, op=ALU.mult), reads=[destm, oh], writes=[tmp3])
            P.op(P.dve, lambda e: e.tensor_reduce(out=dselA[:, b, t0:t1], in_=tmp3[:, t0:t1, :], axis=AX.X, op=ALU.add), reads=[tmp3], writes=[dselA])
            P.op(P.pool, lambda e: e.tensor_tensor(out=oh2[:, 0:nT, :], in0=gt[:, t0:t1, :], in1=ohb, op=ALU.mult), reads=[gt, oh], writes=[oh2])
            P.op(P.dve, lambda e: e.tensor_reduce(out=gselA[:, b, t0:t1], in_=oh2[:, 0:nT, :], axis=AX.X, op=ALU.add), reads=[oh2], writes=[gselA])
            P.op(P.dve, lambda e: e.tensor_scalar(out=dselA[:, b, t0:t1], in0=dselA[:, b, t0:t1], scalar1=-256.0 * b, scalar2=None, op0=ALU.add),
                 reads=[dselA], writes=[dselA])
        Sall = sb(G, es, "Sall", [128, nT, 256], BF16)
        hgT = sb(G, es, "hgT", [128, KC, 256], BF16)
        actT = sb(G, es, "actT", [128, KC, 256], BF16)
        NWB = 3
        stgv = [V(bigA.t[:, i * 8192:(i + 1) * 8192].bitcast(F32), Res()) for i in range(3)]
        wbb = [V(bigA.t[:, 24576 + i * 4096:24576 + (i + 1) * 4096], Res()) for i in range(NWB)]
        gg = [sb(G, es, "gg", [128, 256], F32) for _ in range(2)]
        sg = [sb(G, es, "sg", [128, 256], F32) for _ in range(2)]
        ll = [sb(G, es, "ll", [128, 256], F32) for _ in range(2)]
        oS = [sb(G, es, "oS", [128, 256], BF16) for _ in range(3)]
        be1 = sb(G, es, "be1", [128, NB], F32)
        be2 = sb(G, es, "be2", [128, NB], F32)
        idx = [sb(G, es, "idx", [128, 48], mybir.dt.int32) for _ in range(2)]
        P.op(P.dve, lambda e: e.tensor_scalar(out=be1[:, :], in0=bef[:, :], scalar1=2048.0, scalar2=None, op0=ALU.mult), reads=[bef], writes=[be1])
        P.op(P.dve, lambda e: e.tensor_scalar(out=be2[:, :], in0=bef[:, :], scalar1=1024.0, scalar2=None, op0=ALU.mult), reads=[bef], writes=[be2])
        wi = dict(i=0)

        class WV:
            def __init__(s_, tl):
                s_.ap = tl.ap.rearrange("p (k n) -> p k n", k=KC)
                s_.r = tl.r

            def __getitem__(s_, k):
                return s_.ap[k]

        def load_block(ix, which, piece):
            i = wi["i"]
            wi["i"] += 1
            w = wbb[i % NWB]
            st = stgv[i % 3]
            W = I["w1t%d" % l] if which == 1 else I["w2t%d" % l]
            j = piece if which == 1 else 32 + piece
            P.dma(None, None, reads=[ix], writes=[st], E=P.pool,
                  fn=lambda e: e.indirect_dma_start(out=st[:, :], out_offset=None, in_=W[:, :],
                                                    in_offset=bass.IndirectOffsetOnAxis(ap=ix[:, j:j + 1], axis=0)))
            if i % 2 == 0:
                P.op(P.act, lambda e: e.activation(out=w[:, :], in_=st[:, :], func=AF.Copy), reads=[st], writes=[w])
            else:
                P.op(P.dve, lambda e: e.tensor_copy(w[:, :], st[:, :]), reads=[st], writes=[w])
            return WV(w)

        hg = [sb(G, es, "hg", [128, 2, D], BF16) for _ in range(2)]
        tokpt = sb(G, es, "tokpt", [128, nT, 2], BF16)
        gidx = [sb(G, es, "gidx", [128, 2], mybir.dt.int32) for _ in range(2)]
        gidf = [sb(G, es, "gidf", [128, 4], F32) for _ in range(2)]
        for j, t in enumerate(tiles):
            P.op(P.dve, lambda e: e.tensor_copy(tokpt[:, j, 0:1], cst[:, C_COL + 1:C_COL + 2]), reads=[cst], writes=[tokpt])
            P.op(P.dve, lambda e: e.memset(tokpt[:, j, 1:2], float(t)), writes=[tokpt])

        def prep(b):
            ix = idx[b % 2]
            P.op(P.dve, lambda e: e.tensor_scalar(out=ix[:, 0:32], in0=cst[:, C_OFF1:C_OFF1 + 32], scalar1=be1[:, b:b + 1], scalar2=None, op0=ALU.add),
                 reads=[cst, be1], writes=[ix])
            P.op(P.dve, lambda e: e.tensor_scalar(out=ix[:, 32:48], in0=cst[:, C_OFF2:C_OFF2 + 16], scalar1=be2[:, b:b + 1], scalar2=None, op0=ALU.add),
                 reads=[cst, be2], writes=[ix])
            bs = b1sel[b % 2]
            P.op(P.dve, lambda e: e.tensor_tensor(out=b1tmp[:, :, :], in0=b1T[:, :, :], in1=oh[:, b:b + 1, :].to_broadcast([128, 2 * KC, NE]), op=ALU.mult),
                 reads=[b1T, oh], writes=[b1tmp])
            P.op(P.dve, lambda e: e.tensor_reduce(out=bs[:, :], in_=b1tmp[:, :, :], axis=AX.X, op=ALU.add), reads=[b1tmp], writes=[bs])
            for j, t in enumerate(tiles):
                P.op(P.dve, lambda e: e.tensor_scalar(out=Sall[:, j, :], in0=cst[:, C_IOTA:C_IOTA + 256], scalar1=dselA[:, b, t:t + 1],
                                                      scalar2=None, op0=ALU.is_equal), reads=[cst, dselA], writes=[Sall])
            pq = G.ps[b % 2]
            P.mm([(lambda e, j=j, s_i=s_i: e.matmul(pq[:, 2 * s_i:2 * s_i + 2], lhsT=Sall[:, j, s_i * 128:(s_i + 1) * 128], rhs=tokpt[:, j, :],
                                                   start=(j == 0), stop=(j == nT - 1))) for s_i in range(2) for j in range(nT)],
                 reads=[Sall, tokpt], writes=[pq])
            gi = gidx[b % 2]
            gf = gidf[b % 2]
            P.op(P.dve, lambda e: e.tensor_copy(gf[:, :], pq[:, 0:4]), reads=[pq], writes=[gf])
            P.op(P.dve, lambda e: e.scalar_tensor_tensor(out=gi[:, :], in0=gf[:, 1:4:2], scalar=128.0, in1=gf[:, 0:4:2], op0=ALU.mult, op1=ALU.add),
                 reads=[gf], writes=[gi])
            g_ = hg[b % 2]
            for s_i in range(2):
                P.dma(None, None, reads=[gi, R["h2d"]], writes=[g_], E=P.pool,
                      fn=lambda e: e.indirect_dma_start(out=g_[:, s_i, :], out_offset=None, in_=S["h2d"][:, :],
                                                        in_offset=bass.IndirectOffsetOnAxis(ap=gi[:, s_i:s_i + 1], axis=0)))

        oi = 0
        prep(0)
        for b in range(NB):
            if b + 1 < NB:
                prep(b + 1)
            ix = idx[b % 2]
            bs = b1sel[b % 2]
            g_ = hg[b % 2]
            for s_i in range(2):
                for half in range(2):
                    pq = G.ps[(2 * s_i + half) % 2]
                    pvb = pq[:, 0:512].bitcast(BF16)
                    P.mm([(lambda e, k=k: e.transpose(pvb[:, (k % 8) * 128:(k % 8 + 1) * 128], g_[:, s_i, k * 128:(k + 1) * 128], G.idb[:, :]))
                          for k in range(half * 8, half * 8 + 8)], reads=[g_, G.idb], writes=[pq])
                    dst = hgT[:, half * 8:half * 8 + 8, s_i * 128:(s_i + 1) * 128]
                    if half:
                        P.op(P.act, lambda e: e.activation(out=dst, in_=pvb.rearrange("p (k n) -> p k n", k=8), func=AF.Copy), reads=[pq], writes=[hgT])
                    else:
                        P.op(P.dve, lambda e: e.tensor_copy(dst, pvb.rearrange("p (k n) -> p k n", k=8)), reads=[pq], writes=[hgT])
            for c in range(KC):
                w = load_block(ix, 1, c)
                pg, pl = G.ps[2 + c % 2], G.ps[4 + c % 2]
                g_, s_, l_ = gg[c % 2], sg[c % 2], ll[c % 2]
                P.mm([(lambda e, k=k: e.matmul(pg[:, 0:256], lhsT=w[:, k, 0:128], rhs=hgT[:, k, :], start=(k == 0), stop=(k == KC - 1)))
                      for k in range(KC)], reads=[w, hgT], writes=[pg])
                P.mm([(lambda e, k=k: e.matmul(pl[:, 0:256], lhsT=w[:, k, 128:256], rhs=hgT[:, k, :], start=(k == 0), stop=(k == KC - 1)))
                      for k in range(KC)], reads=[w, hgT], writes=[pl])
                P.op(P.dve, lambda e: e.tensor_scalar(out=g_[:, :], in0=pg[:, 0:256], scalar1=bs[:, 2 * c:2 * c + 1], scalar2=7.0,
                                                      op0=ALU.add, op1=ALU.min), reads=[pg, bs], writes=[g_])
                P.op(P.act, lambda e: e.activation(out=s_[:, :], in_=g_[:, :], func=AF.Sigmoid, scale=1.702), reads=[g_], writes=[s_])
                P.op(P.dve, lambda e: e.tensor_scalar(out=l_[:, :], in0=pl[:, 0:256], scalar1=bs[:, 2 * c + 1:2 * c + 2], scalar2=7.0,
                                                      op0=ALU.add, op1=ALU.min), reads=[pl, bs], writes=[l_])
                P.op(P.dve, lambda e: e.tensor_scalar(out=l_[:, :], in0=l_[:, :], scalar1=-7.0, scalar2=1.0, op0=ALU.max, op1=ALU.add),
                     reads=[l_], writes=[l_])
                P.op(P.dve, lambda e: e.tensor_tensor(out=g_[:, :], in0=g_[:, :], in1=s_[:, :], op=ALU.mult), reads=[g_, s_], writes=[g_])
                P.op(P.dve, lambda e: e.tensor_tensor(out=actT[:, c, :], in0=g_[:, :], in1=l_[:, :], op=ALU.mult), reads=[g_, l_], writes=[actT])
            for n in range(8):
                w = load_block(ix, 2, n)
                for s_i in range(2):
                    pq = G.ps[6 + oi % 2]
                    o_ = oS[oi % 3]
                    oi += 1
                    P.mm([(lambda e, k=k: e.matmul(pq[:, 0:256], lhsT=actT[:, k, s_i * 128:(s_i + 1) * 128], rhs=w[:, k, :],
                                                   start=(k == 0), stop=(k == KC - 1))) for k in range(KC)], reads=[actT, w], writes=[pq])
                    if oi % 2:
                        P.op(P.act, lambda e: e.activation(out=o_[:, :], in_=pq[:, 0:256], func=AF.Copy), reads=[pq], writes=[o_])
                    else:
                        P.op(P.dve, lambda e: e.tensor_copy(o_[:, :], pq[:, 0:256]), reads=[pq], writes=[o_])
                    P.dma(S["outE"][b, s_i * 128:(s_i + 1) * 128, n * 256:(n + 1) * 256], o_[:, :], reads=[o_], writes=[R["outE"]], E=P.sp)
    P.barrier()
    with ExitStack() as es:
        acc = bigA.t[:, :].bitcast(F32).rearrange("p (t n) -> p t n", n=D)
        accR = bigA.r
        oE = [sb(G, es, "oE", [128, 2, D], BF16) for _ in range(2)]
        Sg = [sb(G, es, "Sg", [128, 256], BF16) for _ in range(2)]
        SgT = [sb(G, es, "SgT", [128, 2, 128], BF16) for _ in range(2)]
        b2s = sb(G, es, "b2s", [NE, D], F32)
        gT = sb(G, es, "gT", [NE, 128], F32)
        xt = [sb(G, es, "mxt", [128, D], F32) for _ in range(2)]
        gate = [bc_row(G, es, "mg", S["modD"][l, r, 5 * D:6 * D], D, E=P.act) for r in range(2)]
        P.dma(b2s[:, :], I["exp_b2"][l, :, :], writes=[b2s])
        for g0 in range(0, nT, 9):
            grp = tiles[g0:g0 + 9]
            for j, t in enumerate(grp):
                pt = G.ps[4]
                P.mm([lambda e: e.transpose(pt[0:NE, 0:128], gt[:, t, :], cst[:, C_ID:C_ID + 128])], reads=[gt, cst], writes=[pt])
                P.op(P.act, lambda e: e.activation(out=gT[:, :], in_=pt[0:NE, 0:128], func=AF.Copy), reads=[pt], writes=[gT])
                for n in range(4):
                    pq = G.ps[n]
                    P.mm([lambda e: e.matmul(pq[:, :], lhsT=gT[:, :], rhs=b2s[:, n * 512:(n + 1) * 512], start=True, stop=True)],
                         reads=[gT, b2s], writes=[pq])
                    P.op(P.dve if n % 2 else P.act,
                         (lambda e: e.tensor_copy(acc[:, j, n * 512:(n + 1) * 512], pq[:, :])) if n % 2 else
                         (lambda e: e.activation(out=acc[:, j, n * 512:(n + 1) * 512], in_=pq[:, :], func=AF.Copy)),
                         reads=[pq], writes=[accR])
            it = 0
            for b in range(NB):
                o = oE[b % 2]
                P.dma(o[:, :, :], S["outE"][b, :, :].rearrange("(s p) n -> p s n", p=128), reads=[R["outE"]], writes=[o])
                for j, t in enumerate(grp):
                    sg_, sgT = Sg[it % 2], SgT[it % 2]
                    it += 1
                    P.op(P.dve, lambda e: e.tensor_scalar(out=sg_[:, :], in0=cst[:, C_IOTA:C_IOTA + 256], scalar1=dselA[:, b, t:t + 1],
                                                           scalar2=gselA[:, b, t:t + 1], op0=ALU.is_equal, op1=ALU.mult),
                         reads=[cst, dselA, gselA], writes=[sg_])
                    pt = G.ps[4 + it % 2]
                    pvb = pt[:, 0:128].bitcast(BF16)
                    P.mm([(lambda e, s_i=s_i: e.transpose(pvb[:, s_i * 128:(s_i + 1) * 128], sg_[:, s_i * 128:(s_i + 1) * 128], G.idb[:, :]))
                          for s_i in range(2)], reads=[sg_, G.idb], writes=[pt])
                    P.op(P.act, lambda e: e.activation(out=sgT[:, :, :], in_=pvb[:, 0:256].rearrange("p (s n) -> p s n", s=2), func=AF.Copy),
                         reads=[pt], writes=[sgT])
                    for n in range(4):
                        pq = G.ps[n]
                        P.mm([(lambda e, s_i=s_i: e.matmul(pq[:, :], lhsT=sgT[:, s_i, :], rhs=o[:, s_i, n * 512:(n + 1) * 512],
                                                          start=(s_i == 0), stop=(s_i == 1))) for s_i in range(2)], reads=[sgT, o], writes=[pq])
                        P.op(P.dve, lambda e: e.tensor_tensor(out=acc[:, j, n * 512:(n + 1) * 512], in0=acc[:, j, n * 512:(n + 1) * 512],
                                                              in1=pq[:, :], op=ALU.add), reads=[pq, accR], writes=[accR])
            for j, t in enumerate(grp):
                x = xt[j % 2]
                r = 1 if t < 2 else 0
                P.dma(x[:, :], S["xres"][t * 128:(t + 1) * 128, :], reads=[R["xres"]], writes=[x])
                P.op(P.pool, lambda e: e.tensor_tensor(out=acc[:, j, :], in0=acc[:, j, :], in1=gate[r][:, :], op=ALU.mult),
                     reads=[accR, gate[r]], writes=[accR])
                P.op(P.dve, lambda e: e.tensor_tensor(out=x[:, :], in0=x[:, :], in1=acc[:, j, :], op=ALU.add), reads=[x, accR], writes=[x])
                P.dma(S["xres"][t * 128:(t + 1) * 128, :], x[:, :], reads=[x], writes=[R["xres"]], E=P.pool)


def phase_hgrn(G, hT):
    P, I, S, R = G.P, G.I, G.S, G.res
    cst = G.cst
    W = I["c_w_in"]
    fwd = list(range(T))
    bwd = [1, 0] + list(range(T - 1, 1, -1))
    NCH = [(i * 512, min(512, NT - i * 512)) for i in range(5)]
    with ExitStack() as es:
        lbT = sb(G, es, "lbT", [128, 16], F32)
        omlT = sb(G, es, "omlT", [128, 16], F32)
        with ExitStack() as es1:
            c0 = sb(G, es1, "c0", [16, 128], F32)
            c1 = sb(G, es1, "c1", [16, 128], F32)
            P.dma(c0[:, :], I["c_lb"][0, :].rearrange("(h p) -> h p", p=128), writes=[c0])
            P.dma(c1[:, :], I["c_lb"][1, :].rearrange("(h p) -> h p", p=128), writes=[c1])
            P.op(P.dve, lambda e: e.tensor_tensor(out=c1[:, :], in0=c1[:, :], in1=c0[:, :], op=ALU.subtract), reads=[c0, c1], writes=[c1])
            P.op(P.act, lambda e: e.activation(out=c1[:, :], in_=c1[:, :], func=AF.Sigmoid), reads=[c1], writes=[c1])
            pq = G.ps[0]
            P.mm([lambda e: e.transpose(pq[:, 0:16], c1[:, :], cst[0:16, C_ID:C_ID + 16])], reads=[c1, cst], writes=[pq])
            P.op(P.dve, lambda e: e.tensor_copy(lbT[:, :], pq[:, 0:16]), reads=[pq], writes=[lbT])
            P.op(P.dve, lambda e: e.tensor_scalar(out=omlT[:, :], in0=lbT[:, :], scalar1=-1.0, scalar2=1.0, op0=ALU.mult, op1=ALU.add),
                 reads=[lbT], writes=[omlT])
            P.barrier()
        gnb = bc_row(G, es, "cgn", I["c_gn"][0, :], D)
        wst = sb(G, es, "hwst", [128, KC, 128], F32)
        W5 = sb(G, es, "W5", [128, KC, 5, 128], BF16)
        lf = sb(G, es, "lf", [128, NT], F32)
        kk = sb(G, es, "kk", [128, NT], F32)
        BX = sb(G, es, "BX", [128, NT], F32)
        qs = sb(G, es, "qs", [128, NT], F32)
        qe = sb(G, es, "qe", [128, NT], BF16)
        ke = sb(G, es, "ke", [128, NT], BF16)
        vt = sb(G, es, "vt", [128, T, 128], BF16)
        gtm = sb(G, es, "gtm", [128, T, 128], BF16)
        O = sb(G, es, "hO", [128, T, 128], F32)
        ones = sb(G, es, "hones", [128, NT], BF16)
        ob = sb(G, es, "hob", [128, T, 128], BF16)
        oT = [sb(G, es, "hoT", [128, 4, 128], BF16) for _ in range(2)]
        sc = sb(G, es, "hsc", [128, 4, T], F32)
        Sst = sb(G, es, "hS", [128, 128], F32)
        Ssc = [sb(G, es, "hSsc", [128, 128], BF16) for _ in range(2)]
        keT = [sb(G, es, "hkeT", [128, 128], BF16) for _ in range(2)]
        AT = [sb(G, es, "hAT", [128, 128], BF16) for _ in range(2)]
        st2 = sb(G, es, "hst2", [128, T], F32)
        P.op(P.pool, lambda e: e.memset(ones[:, :], 1.0), writes=[ones])
        BX3 = BX.t[:, :].rearrange("p (c n) -> p c n", n=128)
        lf3 = lf.t[:, :].rearrange("p (c n) -> p c n", n=128)
        for h in range(16):
            for j in range(5):
                P.dma(wst[:, :, :], W[:, j * D + h * 128:j * D + (h + 1) * 128].rearrange("(k p) n -> p k n", p=128), writes=[wst],
                      E=(P.sp if j % 2 == 0 else P.act))
                if j % 2 == 0:
                    P.op(P.pool, lambda e: e.tensor_copy(W5[:, :, j, :], wst[:, :, :]), reads=[wst], writes=[W5])
                else:
                    P.op(P.act, lambda e: e.activation(out=W5[:, :, j, :], in_=wst[:, :, :], func=AF.Copy), reads=[wst], writes=[W5])

            def proj_fm(j, post):
                for ci, (n0, nn) in enumerate(NCH):
                    pq = G.ps[ci % 2]
                    P.mm([(lambda e, k=k: e.matmul(pq[:, 0:nn], lhsT=W5[:, k, j, :], rhs=hT[:, k, n0:n0 + nn], start=(k == 0), stop=(k == KC - 1)))
                          for k in range(KC)], reads=[W5, hT], writes=[pq])
                    post(pq, n0, nn)

            def proj_tm(j, dst, tiles, func):
                for g0 in range(0, len(tiles), 4):
                    ts = tiles[g0:g0 + 4]
                    pq = G.ps[2 + (g0 // 4) % 2]
                    for jj, t in enumerate(ts):
                        P.mm([(lambda e, k=k: e.matmul(pq[:, jj * 128:(jj + 1) * 128], lhsT=hT[:, k, t * 128:(t + 1) * 128], rhs=W5[:, k, j, :],
                                                       start=(k == 0), stop=(k == KC - 1))) for k in range(KC)], reads=[W5, hT], writes=[pq])
                    n = len(ts)
                    P.op(P.act, lambda e: e.activation(out=dst[:, ts[0]:ts[0] + n, :], in_=pq[:, 0:n * 128].rearrange("p (t n) -> p t n", t=n), func=func),
                         reads=[pq], writes=[dst])

            proj_tm(2, vt, list(range(T)), AF.Copy)
            proj_tm(4, gtm, list(range(2, T)), AF.Silu)
            proj_fm(3, lambda pq, n0, nn: P.op(P.act, lambda e: e.activation(out=qs[:, n0:n0 + nn], in_=pq[:, 0:nn], func=AF.Silu), reads=[pq], writes=[qs]))
            for d in range(2):
                order = fwd if d == 0 else bwd
                proj_fm(d, lambda pq, n0, nn: P.op(P.act, lambda e: e.activation(out=lf[:, n0:n0 + nn], in_=pq[:, 0:nn], func=AF.Sigmoid), reads=[pq], writes=[lf]))
                P.op(P.dve, lambda e: e.tensor_scalar(out=lf[:, :], in0=lf[:, :], scalar1=omlT[:, h:h + 1], scalar2=lbT[:, h:h + 1], op0=ALU.mult, op1=ALU.add),
                     reads=[lf, omlT, lbT], writes=[lf])
                P.op(P.pool, lambda e: e.tensor_scalar(out=kk[:, :], in0=lf[:, :], scalar1=-1.0, scalar2=1.0, op0=ALU.mult, op1=ALU.add), reads=[lf], writes=[kk])
                P.op(P.act, lambda e: e.activation(out=lf[:, :], in_=lf[:, :], func=AF.Ln), reads=[lf], writes=[lf])
                P.op(P.dve, lambda e: e.tensor_tensor_scan(out=BX[:, :], data0=ones[:, :], data1=lf[:, :], initial=0.0, op0=ALU.mult, op1=ALU.add),
                     reads=[ones, lf], writes=[BX])
                if d == 0:
                    P.op(P.dve, lambda e: e.memset(sc[:, 0, 0:1], 0.0), writes=[sc])
                    P.op(P.dve, lambda e: e.tensor_scalar(out=sc[:, 0, 1:T], in0=BX3[:, 0:T - 1, 127], scalar1=-1.0, scalar2=None, op0=ALU.mult), reads=[BX], writes=[sc])
                    edge = 127
                else:
                    P.op(P.dve, lambda e: e.tensor_copy(sc[:, 0, :], BX3[:, :, 127]), reads=[BX], writes=[sc])
                    P.op(P.dve, lambda e: e.tensor_tensor(out=BX[:, :], in0=lf[:, :], in1=BX[:, :], op=ALU.subtract), reads=[lf, BX], writes=[BX])
                    edge = 0
                P.op(P.dve, lambda e: e.tensor_tensor(out=sc[:, 1, :], in0=sc[:, 0, :], in1=BX3[:, :, 64], op=ALU.add), reads=[sc, BX], writes=[sc])
                P.op(P.dve, lambda e: e.tensor_tensor(out=sc[:, 2, :], in0=sc[:, 0, :], in1=BX3[:, :, edge], op=ALU.add), reads=[sc, BX], writes=[sc])
                P.op(P.dve, lambda e: e.tensor_tensor(out=sc[:, 3, :], in0=BX3[:, :, edge], in1=BX3[:, :, 64], op=ALU.subtract), reads=[sc, BX], writes=[sc])
                P.op(P.act, lambda e: e.activation(out=sc[:, 1:4, :], in_=sc[:, 1:4, :], func=AF.Exp), reads=[sc], writes=[sc])
                P.op(P.dve, lambda e: e.tensor_tensor(out=lf3, in0=BX3, in1=BX3[:, :, 64:65].to_broadcast([128, T, 128]), op=ALU.subtract), reads=[BX], writes=[lf])
                P.op(P.act, lambda e: e.activation(out=BX[:, :], in_=lf[:, :], func=AF.Exp), reads=[lf], writes=[BX])
                P.op(P.dve, lambda e: e.tensor_tensor(out=qe[:, :], in0=qs[:, :], in1=BX[:, :], op=ALU.mult), reads=[qs, BX], writes=[qe])
                P.op(P.act, lambda e: e.activation(out=BX[:, :], in_=lf[:, :], func=AF.Exp, scale=-1.0), reads=[lf, qe], writes=[BX])
                P.op(P.dve, lambda e: e.tensor_tensor(out=ke[:, :], in0=kk[:, :], in1=BX[:, :], op=ALU.mult), reads=[kk, BX], writes=[ke])
                mcol = C_MGE if d == 0 else C_MLE
                P.op(P.dve, lambda e: e.memset(Sst[:, :], 0.0), writes=[Sst])
                for i, c in enumerate(order):
                    kt, at, ssc = keT[i % 2], AT[i % 2], Ssc[i % 2]
                    cs_ = slice(c * 128, (c + 1) * 128)
                    pt = G.ps[4 + i % 2]
                    ptb = pt[:, 0:64].bitcast(BF16)
                    P.mm([lambda e: e.transpose(ptb[:, 0:128], ke[:, cs_], G.idb[:, :])], reads=[ke, G.idb], writes=[pt])
                    P.op(P.act, lambda e: e.activation(out=kt[:, :], in_=ptb[:, 0:128], func=AF.Copy), reads=[pt], writes=[kt])
                    if c >= 2:
                        pS, pO = G.ps[6], G.ps[7]
                        P.mm([lambda e: e.matmul(pS[:, 0:128], lhsT=ke[:, cs_], rhs=qe[:, cs_], start=True, stop=True)], reads=[ke, qe], writes=[pS])
                        P.op(P.dve, lambda e: e.tensor_tensor(out=at[:, :], in0=pS[:, 0:128], in1=cst[:, mcol:mcol + 128], op=ALU.mult), reads=[pS, cst], writes=[at])
                        P.op(P.act, lambda e: e.activation(out=ssc[:, :], in_=Sst[:, :], func=AF.Copy, scale=sc[:, 1, c:c + 1]), reads=[Sst, sc], writes=[ssc])
                        P.mm([lambda e: e.matmul(pO[:, 0:128], lhsT=at[:, :], rhs=vt[:, c, :], start=True, stop=False),
                              lambda e: e.matmul(pO[:, 0:128], lhsT=qe[:, cs_], rhs=ssc[:, :], start=False, stop=True)],
                             reads=[at, vt, qe, ssc], writes=[pO])
                        if d == 0:
                            P.op(P.act, lambda e: e.activation(out=O[:, c, :], in_=pO[:, 0:128], func=AF.Copy), reads=[pO], writes=[O])
                        else:
                            P.op(P.dve, lambda e: e.tensor_tensor(out=O[:, c, :], in0=O[:, c, :], in1=pO[:, 0:128], op=ALU.add), reads=[pO, O], writes=[O])
                    if i < T - 1:
                        pU = G.ps[2 + i % 2]
                        P.mm([lambda e: e.matmul(pU[:, 0:128], lhsT=kt[:, :], rhs=vt[:, c, :], start=True, stop=True)], reads=[kt, vt], writes=[pU])
                        P.op(P.dve, lambda e: e.tensor_scalar(out=Sst[:, :], in0=Sst[:, :], scalar1=sc[:, 2, c:c + 1], scalar2=None, op0=ALU.mult),
                             reads=[Sst, sc], writes=[Sst])
                        P.op(P.dve, lambda e: e.scalar_tensor_tensor(out=Sst[:, :], in0=pU[:, 0:128], scalar=sc[:, 3, c:c + 1], in1=Sst[:, :],
                                                                     op0=ALU.mult, op1=ALU.add), reads=[pU, sc, Sst], writes=[Sst])
            Ox = O.t[:, 2:T, :]
            lfx = lf.t[:, :].rearrange("p (c n) -> p c n", n=128)[:, 2:T, :]
            P.op(P.act, lambda e: e.activation(out=lfx, in_=Ox, func=AF.Square), reads=[O], writes=[lf])
            P.op(P.dve, lambda e: e.tensor_reduce(out=st2[:, 2:T], in_=lfx, axis=AX.X, op=ALU.add), reads=[lf], writes=[st2])
            P.op(P.act, lambda e: e.activation(out=st2[:, 2:T], in_=st2[:, 2:T], func=AF.Sqrt, scale=1.0 / 128, bias=cst[:, C_COL + 2:C_COL + 3]),
                 reads=[st2, cst], writes=[st2])
            P.op(P.dve, lambda e: e.reciprocal(st2[:, 2:T], st2[:, 2:T]), reads=[st2], writes=[st2])
            P.op(P.dve, lambda e: e.tensor_tensor(out=Ox, in0=Ox, in1=st2[:, 2:T].unsqueeze(2).to_broadcast([128, T - 2, 128]), op=ALU.mult), reads=[O, st2], writes=[O])
            P.op(P.dve, lambda e: e.tensor_tensor(out=Ox, in0=Ox, in1=gnb[:, h * 128:(h + 1) * 128].unsqueeze(1).to_broadcast([128, T - 2, 128]), op=ALU.mult),
                 reads=[O, gnb], writes=[O])
            P.op(P.dve, lambda e: e.tensor_tensor(out=ob[:, 2:T, :], in0=Ox, in1=gtm[:, 2:T, :], op=ALU.mult), reads=[O, gtm], writes=[ob])
            transpose_out(G, ob, oT, S["mixT"], R["mixT"], h, list(range(2, T)))


def phase_final(G):
    P, I, S, R = G.P, G.I, G.S, G.res
    with ExitStack() as es:
        gb = bc_row(G, es, "fgb", I["norm_final"][0, :], D)
        xt = [sb(G, es, "fxt", [128, D], F32) for _ in range(2)]
        yo = [sb(G, es, "fyo", [128, D], F32) for _ in range(2)]
        junk = sb(G, es, "fjunk", [128, D], BF16)
        ss = sb(G, es, "fss", [128, 4], F32)
        rs = sb(G, es, "frs", [128, 4], F32)
        for i in range(16):
            x, y = xt[i % 2], yo[i % 2]
            P.dma(x[:, :], S["xres"][(i + 2) * 128:(i + 3) * 128, :], reads=[R["xres"]], writes=[x])
            P.op(P.act, lambda e: e.activation(out=junk[:, :], in_=x[:, :], func=AF.Square, accum_out=ss[:, 0:1]), reads=[x], writes=[junk, ss])
            rstd_from_ss(G, ss, rs, 1, 1.0 / D)
            P.op(P.dve, lambda e: e.scalar_tensor_tensor(out=y[:, :], in0=x[:, :], scalar=rs[:, 0:1], in1=gb[:, :], op0=ALU.mult, op1=ALU.mult),
                 reads=[x, rs, gb], writes=[y])
            P.dma(G.out[i * 128:(i + 1) * 128, :], y[:, :], reads=[y], writes=[G.out_res], E=P.act)


_PROG = {}


def _inputs_for_core(inp, b, consts, cossin):
    f = lambda a: np.ascontiguousarray(a, dtype=np.float32)
    return {
        "x": f(inp["x"][b]), "ctx": f(inp["ctx"][b]), "c": f(inp["c"][b:b + 1]), "c_ctx": f(inp["c_ctx"][None, :]),
        "ada_w": f(inp["ada_w"]), "ada_b": f(inp["ada_b"]), "norm_mix": f(inp["norm_mix"]), "norm_ffn": f(inp["norm_ffn"]),
        "ab_w_in": f(inp["ab_w_in"][0]), "ab_w_out": f(inp["ab_w_out"][0]), "a_q_norm": f(inp["a_q_norm"]),
        "a_k_norm": f(inp["a_k_norm"]), "b_decay_exp": f(inp["b_decay_exp"].reshape(1, 16)), "b_gn": f(inp["b_gn"]),
        "c_w_in": f(inp["c_w_in"][0]), "c_w_out": f(inp["c_w_out"][0]), "c_lb": f(inp["c_lb"]), "c_gn": f(inp["c_gn"]),
        "router_w": f(inp["router_w"]), "router_b": f(inp["router_b"]),
        "w1t0": inp["_w1"][0], "w1t1": inp["_w1"][1], "w2t0": inp["_w2"][0], "w2t1": inp["_w2"][1],
        "exp_b1": f(inp["exp_b1"]), "exp_b2": f(inp["exp_b2"]),
        "norm_final": f(inp["norm_final"][None, :]), "consts": consts, "cossin": cossin,
    }


def prep_weights(inp):
    w1, w2 = inp["exp_w1"], inp["exp_w2"]
    if w1 is None:
        z = np.zeros((1, 1), np.float32)
        inp["_w1"] = [z, z]
        inp["_w2"] = [z, z]
        return
    inp["_w1"], inp["_w2"] = [], []
    for l in range(2):
        a = np.asarray(w1[l], dtype=np.float32).reshape(NE, 2, 8, 128, 16, 128, 2)
        inp["_w1"].append(np.ascontiguousarray(a.transpose(0, 4, 3, 1, 2, 6, 5)).reshape(NE * 16 * 128, 4096))
        b_ = np.asarray(w2[l], dtype=np.float32).reshape(NE, 2, 8, 128, 8, 256)
        inp["_w2"].append(np.ascontiguousarray(b_.transpose(0, 4, 3, 1, 2, 5)).reshape(NE * 8 * 128, 4096))


def kernel(**inputs):
    inp = {k: np.asarray(v) for k, v in inputs.items()}
    prep_weights(inp)
    consts, cossin = make_consts(), make_cossin()
    if "nc" not in _PROG:
        _PROG["nc"] = build_program()
    nc = _PROG["nc"]
    in_maps = [_inputs_for_core(inp, b, consts, cossin) for b in range(N_CORES)]
    res = run_bass_kernel_spmd(nc, in_maps, core_ids=list(range(N_CORES)))
    return np.stack([np.asarray(res.results[b]["out"], dtype=np.float32) for b in range(N_CORES)], axis=0)
```

```python
import math
from contextlib import ExitStack

import numpy as np
import ml_dtypes
import concourse.bass as bass
import concourse.mybir as mybir
from concourse.bass_utils import run_bass_kernel_spmd

F32 = mybir.dt.float32
BF16 = mybir.dt.bfloat16
AF = mybir.ActivationFunctionType
ALU = mybir.AluOpType
AX = mybir.AxisListType

D = 2048
KC = 16
NCTX = 256
NX = 2048
NT = NCTX + NX
T = NT // 128
NE = 32
CAP = 384
EPS = 1e-6
N_CORES = 8

C_ID = 0
C_IOTA = 128
C_US = 512
C_RGE = 640
C_RLE = 768
C_MGE = 896
C_MLE = 1024
C_JP1 = 1152
C_CMJ = 1280
C_COL = 1408
C_ONE = 1412
C_OFF1 = 1540
C_OFF2 = 1572
CW = 1588


def make_consts():
    c = np.zeros((128, CW), np.float32)
    p = np.arange(128)[:, None].astype(np.float32)
    j = np.arange(128)[None, :].astype(np.float32)
    c[:, C_ID:C_ID + 128] = np.eye(128)
    c[:, C_IOTA:C_IOTA + 384] = np.arange(384)[None, :]
    c[:, C_US:C_US + 128] = (p < j)
    c[:, C_RGE:C_RGE + 128] = np.maximum(j - p, 0)
    c[:, C_RLE:C_RLE + 128] = np.maximum(p - j, 0)
    c[:, C_MGE:C_MGE + 128] = (j >= p)
    c[:, C_MLE:C_MLE + 128] = (j <= p)
    c[:, C_JP1:C_JP1 + 128] = j + 1
    c[:, C_CMJ:C_CMJ + 128] = 128 - j
    c[:, C_COL] = 127 - p[:, 0]
    c[:, C_COL + 1] = p[:, 0]
    c[:, C_COL + 2] = EPS
    c[:, C_COL + 3] = 1.0
    c[:, C_ONE:C_ONE + 128] = 1.0
    jj = np.arange(32)
    c[:, C_OFF1:C_OFF1 + 32] = p + jj * 128
    c[:, C_OFF2:C_OFF2 + 16] = p + jj[:16] * 128
    return c


def make_cossin():
    gw, hd = 64, 128
    n = NX
    row = np.repeat(np.arange(n // gw, dtype=np.float32), gw)
    col = np.tile(np.arange(gw, dtype=np.float32), n // gw)
    nf = hd // 4
    inv = (np.float32(10000.0) ** (-np.arange(nf, dtype=np.float32) / nf)).astype(np.float32)
    ang = np.concatenate([row[:, None] * inv, col[:, None] * inv], axis=-1).astype(np.float32)
    return np.concatenate([np.cos(ang), np.sin(ang)], axis=-1).astype(np.float32)


class Res:
    __slots__ = ("w", "r")

    def __init__(self):
        self.w = None
        self.r = []


class TL:
    def __init__(self, t):
        self.t = t
        self.r = Res()

    def __getitem__(self, k):
        return self.t[k]


class Eng:
    def __init__(self, e, name, sem):
        self.e = e
        self.name = name
        self.sem = sem
        self.n = 0
        self.seen = {}


def _res(x):
    return x if isinstance(x, Res) else x.r


class Prog:
    def __init__(self, nc, es):
        self.nc = nc

        def mk(e, name):
            return Eng(e, name, es.enter_context(nc.semaphore("s_" + name)))

        self.pe = mk(nc.tensor, "pe")
        self.dve = mk(nc.vector, "dve")
        self.act = mk(nc.scalar, "act")
        self.pool = mk(nc.gpsimd, "pool")
        self.sp = mk(nc.sync, "sp")
        self.engs = [self.pe, self.dve, self.act, self.pool, self.sp]
        self.NQ = 16
        self.dq = {}
        for E in (self.sp, self.act, self.pool):
            self.dq[E.name] = dict(
                sems=[es.enter_context(nc.semaphore("d_%s%d" % (E.name, i))) for i in range(self.NQ)], i=0)
        self.rr = 0

    def _wait(self, E, ev):
        sem, val = ev
        k = id(sem)
        if E.seen.get(k, 0) < val:
            E.e.wait_ge(sem, val)
            E.seen[k] = val

    def _deps(self, E, reads, writes):
        for b in reads:
            b = _res(b)
            if b.w is not None:
                self._wait(E, b.w)
        for b in writes:
            b = _res(b)
            if b.w is not None:
                self._wait(E, b.w)
            for ev in b.r:
                self._wait(E, ev)

    def _commit(self, ev, reads, writes):
        for b in reads:
            _res(b).r.append(ev)
        for b in writes:
            b = _res(b)
            b.w = ev
            b.r = []

    def op(self, E, fn, reads=(), writes=()):
        self._deps(E, reads, writes)
        ins = fn(E.e)
        E.n += 1
        ins.then_inc(E.sem, 1)
        ev = (E.sem, E.n)
        self._commit(ev, reads, writes)
        return ev

    def mm(self, fns, reads=(), writes=()):
        E = self.pe
        self._deps(E, reads, writes)
        ins = None
        for fn in fns:
            ins = fn(E.e)
        E.n += 1
        ins.then_inc(E.sem, 1)
        ev = (E.sem, E.n)
        self._commit(ev, reads, writes)
        return ev

    def dma(self, out, in_, reads=(), writes=(), E=None, fn=None, **kw):
        if E is None:
            E = self.sp
        q = self.dq[E.name]
        i = q["i"]
        q["i"] += 1
        sem = q["sems"][i % self.NQ]
        prev = 16 * (i // self.NQ)
        if prev:
            self._wait(E, (sem, prev))
        self._deps(E, reads, writes)
        if fn is not None:
            fn(E.e).then_inc(sem, 16)
        else:
            E.e.dma_start(out=out, in_=in_, **kw).then_inc(sem, 16)
        ev = (sem, prev + 16)
        self._commit(ev, reads, writes)
        return ev

    def barrier(self):
        evs = [(E.sem, E.n) for E in self.engs if E.n > 0]
        for q in self.dq.values():
            for k, sem in enumerate(q["sems"]):
                uses = (q["i"] - k + self.NQ - 1) // self.NQ
                if uses > 0:
                    evs.append((sem, 16 * uses))
        for E in self.engs:
            for ev in evs:
                if ev[0] is not E.sem:
                    self._wait(E, ev)


class Ctx:
    pass


def build_program(debug=None, stop_after=None, dummy=()):
    debug = debug or []
    nc = bass.Bass("TRN2", target_bir_lowering=False)
    G = Ctx()
    G.nc = nc
    G.debug = debug

    def din(name, shape, dt=F32):
        if name in dummy or ("exp_w1" in dummy and name[:3] in ("w1t", "w2t")):
            shape = [1] * len(shape)
        return nc.dram_tensor(name, list(shape), dt, kind="ExternalInput").ap()

    I = {}
    I["x"] = din("x", [NX, D])
    I["ctx"] = din("ctx", [NCTX, D])
    I["c"] = din("c", [1, D])
    I["c_ctx"] = din("c_ctx", [1, D])
    I["ada_w"] = din("ada_w", [2, D, 6 * D])
    I["ada_b"] = din("ada_b", [2, 6 * D])
    I["norm_mix"] = din("norm_mix", [2, D])
    I["norm_ffn"] = din("norm_ffn", [2, D])
    I["ab_w_in"] = din("ab_w_in", [D, 5632])
    I["ab_w_out"] = din("ab_w_out", [D, D])
    I["a_q_norm"] = din("a_q_norm", [1, 128])
    I["a_k_norm"] = din("a_k_norm", [1, 128])
    I["b_decay_exp"] = din("b_decay_exp", [1, 16])
    I["b_gn"] = din("b_gn", [1, 1024])
    I["c_w_in"] = din("c_w_in", [D, 10240])
    I["c_w_out"] = din("c_w_out", [D, D])
    I["c_lb"] = din("c_lb", [2, D])
    I["c_gn"] = din("c_gn", [1, D])
    I["router_w"] = din("router_w", [2, D, NE])
    I["router_b"] = din("router_b", [2, NE])
    for l_ in range(2):
        I["w1t%d" % l_] = din("w1t%d" % l_, [NE * 16 * 128, 4096])
        I["w2t%d" % l_] = din("w2t%d" % l_, [NE * 8 * 128, 4096])
    I["exp_b1"] = din("exp_b1", [2, NE, 2 * D])
    I["exp_b2"] = din("exp_b2", [2, NE, D])
    I["norm_final"] = din("norm_final", [1, D])
    I["consts"] = din("consts", [128, CW])
    I["cossin"] = din("cossin", [NX, 128])
    G.I = I
    G.out = nc.dram_tensor("out", [NX, D], F32, kind="ExternalOutput").ap()

    def dscr(name, shape, dt):
        kind = "ExternalOutput" if name in debug else "Internal"
        return nc.dram_tensor(name, list(shape), dt, kind=kind).ap()

    S = {}
    S["xres"] = dscr("xres", [NT, D], F32)
    S["modD"] = dscr("modD", [2, 2, 6 * D], F32)
    S["QTa"] = dscr("QTa", [8, 128, NT], BF16)
    S["KTa"] = dscr("KTa", [2, 128, NT], BF16)
    S["Va"] = dscr("Va", [NT, 256], BF16)
    S["QTb"] = dscr("QTb", [8, 128, NT], BF16)
    S["KTb"] = dscr("KTb", [8, 128, NT], BF16)
    S["Kb"] = dscr("Kb", [NT, 1024], BF16)
    S["Vb"] = dscr("Vb", [NT, 1024], BF16)
    S["Gt"] = dscr("Gt", [NT, 2048], BF16)
    S["mixT"] = dscr("mixT", [16, 128, NT], BF16)
    S["outE"] = dscr("outE", [68, 256, D], BF16)
    S["hTd"] = dscr("hTd", [KC, 128, NT], BF16)
    S["h2d"] = dscr("h2d", [NT, D], BF16)
    if "dbg_h2" in debug:
        S["dbg_h2"] = dscr("dbg_h2", [NT, D], BF16)
        S["dbg_route"] = dscr("dbg_route", [3, 128, T, NE], F32)
    G.S = S
    G.res = {k: Res() for k in S}
    G.out_res = Res()

    with ExitStack() as es:
        P = Prog(nc, es)
        G.P = P
        G.ps = [TL(es.enter_context(nc.psum_tensor("ps%d" % i, [128, 512], F32))) for i in range(8)]
        G.cst = TL(es.enter_context(nc.sbuf_tensor("cst", [128, CW], F32)))
        G.idb = TL(es.enter_context(nc.sbuf_tensor("idb", [128, 128], BF16)))
        G.oneb = TL(es.enter_context(nc.sbuf_tensor("oneb", [128, 128], BF16)))
        es.enter_context(nc.Block())
        P.dma(G.cst[:, :], I["consts"][:, :], writes=[G.cst])
        P.op(P.dve, lambda e: e.tensor_copy(G.idb[:, :], G.cst[:, C_ID:C_ID + 128]), reads=[G.cst], writes=[G.idb])
        P.op(P.dve, lambda e: e.tensor_copy(G.oneb[:, :], G.cst[:, C_ONE:C_ONE + 128]), reads=[G.cst], writes=[G.oneb])

        from_phases(G, stop_after)

        P.barrier()
    return nc


class V:
    def __init__(s, ap, r):
        s.ap = ap
        s.r = r

    def __getitem__(s, k):
        return s.ap[k]


class Stop(Exception):
    pass


def from_phases(G, stop_after):
    P = G.P
    G.stop = False
    with ExitStack() as es0:
        bigA = sb(G, es0, "bigA", [128, KC * NT], BF16)
        hT = V(bigA.t[:, :].rearrange("p (k n) -> p k n", k=KC), bigA.r)
        h2 = V(bigA.t[:, :].rearrange("p (t n) -> p t n", t=T), bigA.r)

        def ph(name, fn):
            if G.stop:
                return
            fn()
            P.barrier()
            if "hTd" in G.debug and name in ("norm0", "norm1"):
                P.dma(G.S["hTd"].rearrange("k p n -> p k n"), hT[:, :, :], reads=[hT], writes=[G.res["hTd"]])
                P.barrier()
            if stop_after == name:
                G.stop = True

        for l in range(2):
            ph("mods%d" % l, lambda: phase_mods(G, l))
        for l in range(2):
            tiles = list(range(T)) if l == 0 else list(range(2, T))
            ph("norm%d" % l, lambda: phase_norm(G, l, 1, list(range(T)), hT=hT))
            if l == 0:
                ph("inproj0", lambda: phase_inproj0(G, hT))
                ph("attn", lambda: phase_attn(G))
                ph("ret", lambda: phase_ret(G))
            else:
                ph("hgrn", lambda: phase_hgrn(G, hT))
            ph("outproj%d" % l, lambda: phase_outproj(G, l, hT, tiles))
            if G.stop:
                break
            with ExitStack() as esr:
                route = dict(mask=sb(G, esr, "rmask", [128, T, NE], F32), gate=sb(G, esr, "rgate", [128, T, NE], F32),
                             pos=sb(G, esr, "rpos", [128, T, NE], F32), dsel=sb(G, esr, "rdsel", [128, 68, T], F32),
                             gsel=sb(G, esr, "rgsel", [128, 68, T], F32))
                ph("norm%db" % l, lambda: phase_norm(G, l, 2, tiles, h2=h2, route=route))
                ph("moe%d" % l, lambda: phase_moe(G, l, tiles, h2, route, bigA))
        ph("final", lambda: phase_final(G))


_UID = [0]


def sb(G, es, name, shape, dt):
    _UID[0] += 1
    return TL(es.enter_context(G.nc.sbuf_tensor("%s_%d" % (name, _UID[0]), list(shape), dt)))


def phase_mods(G, l):
    P, I, S = G.P, G.I, G.S
    with ExitStack() as es:
        cin = sb(G, es, "cin", [32, 128], F32)
        cT = sb(G, es, "cT", [128, KC, 2], F32)
        wst = [sb(G, es, "mw%d" % i, [128, KC, 512], F32) for i in range(2)]
        bia = [sb(G, es, "mb%d" % i, [2, 512], F32) for i in range(2)]
        mro = [sb(G, es, "mr%d" % i, [2, 512], F32) for i in range(2)]
        P.dma(cin[0:16, :], I["c"].rearrange("o (k p) -> (o k) p", p=128), writes=[cin])
        P.dma(cin[16:32, :], I["c_ctx"].rearrange("o (k p) -> (o k) p", p=128), writes=[cin])
        P.op(P.act, lambda e: e.activation(out=cin[:, :], in_=cin[:, :], func=AF.Silu), reads=[cin], writes=[cin])
        ps = G.ps[0]
        P.mm([lambda e: e.transpose(ps[:, 0:32], cin[:, :], G.cst[0:32, C_ID:C_ID + 32])],
             reads=[cin, G.cst], writes=[ps])
        P.op(P.dve, lambda e: e.tensor_copy(cT[:, :, :].rearrange("p k o -> p o k"),
                                            ps[:, 0:32].rearrange("p (o k) -> p o k", o=2)),
             reads=[ps], writes=[cT])
        for g in range(24):
            w = wst[g % 2]
            b = bia[g % 2]
            m = mro[g % 2]
            pq = G.ps[1 + g % 2]
            P.dma(w[:, :, :], I["ada_w"][l, :, g * 512:(g + 1) * 512].rearrange("(k p) n -> p k n", p=128), writes=[w])
            P.dma(b[:, :], I["ada_b"][l, g * 512:(g + 1) * 512].partition_broadcast(2), writes=[b], E=P.act)
            P.mm([(lambda e, k=k: e.matmul(pq[0:2, :], lhsT=cT[:, k, :], rhs=w[:, k, :], start=(k == 0), stop=(k == KC - 1)))
                  for k in range(KC)], reads=[cT, w], writes=[pq])
            P.op(P.dve, lambda e: e.tensor_tensor(out=m[:, :], in0=pq[0:2, :], in1=b[:, :], op=ALU.add),
                 reads=[pq, b], writes=[m])
            P.dma(S["modD"][l, :, g * 512:(g + 1) * 512], m[:, :], reads=[m], writes=[G.res["modD"]], E=P.pool)


def bc_row(G, es, name, row_ap, n, E=None):
    t = sb(G, es, name, [128, n], F32)
    G.P.dma(t[:, :], row_ap.partition_broadcast(128), writes=[t], E=E)
    return t


def src_rows(G, l, which, t):
    I, S = G.I, G.S
    if l == 0 and which == 1:
        return (I["ctx"][t * 128:(t + 1) * 128, :] if t < 2 else I["x"][(t - 2) * 128:(t - 1) * 128, :]), None
    return S["xres"][t * 128:(t + 1) * 128, :], G.res["xres"]


def rstd_from_ss(G, ss, rs, n, inv_n):
    P = G.P
    P.op(P.act, lambda e: e.activation(out=rs[:, 0:n], in_=ss[:, 0:n], func=AF.Sqrt, scale=inv_n,
                                       bias=G.cst[:, C_COL + 2:C_COL + 3]), reads=[ss, G.cst], writes=[rs])
    P.op(P.dve, lambda e: e.reciprocal(rs[:, 0:n], rs[:, 0:n]), reads=[rs], writes=[rs])


def phase_norm(G, l, which, tiles, hT=None, h2=None, route=None):
    P, I, S = G.P, G.I, G.S
    gain = I["norm_mix"] if which == 1 else I["norm_ffn"]
    m0 = 0 if which == 1 else 3
    with ExitStack() as es:
        gb = bc_row(G, es, "gb", gain[l, :], D)
        Gm, Sh = [], []
        for r in range(2):
            sc = bc_row(G, es, "sc", S["modD"][l, r, (m0 + 1) * D:(m0 + 2) * D], D, E=P.act)
            P.op(P.dve, lambda e, sc=sc: e.scalar_tensor_tensor(out=sc[:, :], in0=sc[:, :], scalar=1.0, in1=gb[:, :],
                                                                op0=ALU.add, op1=ALU.mult), reads=[sc, gb], writes=[sc])
            Gm.append(sc)
            Sh.append(bc_row(G, es, "sh", S["modD"][l, r, m0 * D:(m0 + 1) * D], D, E=P.act))
        xt = [sb(G, es, "xt", [128, D], F32) for _ in range(2)]
        junk = sb(G, es, "junk", [128, D], BF16)
        hf = sb(G, es, "hf", [128, D], F32)
        hb = [sb(G, es, "hb", [128, D], BF16) for _ in range(2)]
        ss = sb(G, es, "ss", [128, 4], F32)
        rs = sb(G, es, "rs", [128, 4], F32)
        if which == 2:
            rw = sb(G, es, "rw", [128, KC, NE], F32)
            P.dma(rw[:, :, :], I["router_w"][l].rearrange("(k p) n -> p k n", p=128), writes=[rw])
            rb = bc_row(G, es, "rb", I["router_b"][l, :], NE)
            hTf = sb(G, es, "hTf", [128, KC, 128], F32)
            lg = sb(G, es, "lg", [128, NE], F32)
            top = sb(G, es, "top", [128, 8], F32)
            nm = sb(G, es, "nm", [128, 1], F32)
            ex = sb(G, es, "ex", [128, NE], F32)
            se = sb(G, es, "se", [128, 1], F32)
        for i, t in enumerate(tiles):
            x = xt[i % 2]
            src, sres = src_rows(G, l, which, t)
            P.dma(x[:, :], src, reads=[sres] if sres else [], writes=[x])
            P.op(P.act, lambda e: e.activation(out=junk[:, :], in_=x[:, :], func=AF.Square, accum_out=ss[:, 0:1]),
                 reads=[x], writes=[junk, ss])
            rstd_from_ss(G, ss, rs, 1, 1.0 / D)
            r = 1 if t < 2 else 0
            P.op(P.dve, lambda e: e.scalar_tensor_tensor(out=hf[:, :], in0=x[:, :], scalar=rs[:, 0:1], in1=Gm[r][:, :],
                                                         op0=ALU.mult, op1=ALU.mult), reads=[x, rs, Gm[r]], writes=[hf])
            if which == 1:
                h = hb[i % 2]
                P.op(P.pool, lambda e: e.tensor_tensor(out=h[:, :], in0=hf[:, :], in1=Sh[r][:, :], op=ALU.add),
                     reads=[hf, Sh[r]], writes=[h])
                for half in range(2):
                    pq = G.ps[(2 * i + half) % 4]
                    pv = pq[:, 0:512].bitcast(BF16)
                    P.mm([(lambda e, k=k: e.transpose(pv[:, (k % 8) * 128:(k % 8 + 1) * 128], h[:, k * 128:(k + 1) * 128], G.idb[:, :]))
                          for k in range(half * 8, half * 8 + 8)], reads=[h, G.idb], writes=[pq])
                    eng = P.act if half == 0 else P.dve
                    P.op(eng, lambda e: e.tensor_copy(hT[:, half * 8:half * 8 + 8, t * 128:(t + 1) * 128],
                                                      pv.rearrange("p (k n) -> p k n", k=8)) if eng is P.dve else
                         e.activation(out=hT[:, half * 8:half * 8 + 8, t * 128:(t + 1) * 128],
                                      in_=pv.rearrange("p (k n) -> p k n", k=8), func=AF.Copy),
                         reads=[pq], writes=[hT])
            else:
                P.op(P.dve, lambda e: e.tensor_tensor(out=hf[:, :], in0=hf[:, :], in1=Sh[r][:, :], op=ALU.add),
                     reads=[hf, Sh[r]], writes=[hf])
                hb_ = hb[i % 2]
                P.op(P.act, lambda e: e.activation(out=hb_[:, :], in_=hf[:, :], func=AF.Copy), reads=[hf], writes=[hb_])
                P.dma(S["h2d"][t * 128:(t + 1) * 128, :], hb_[:, :], reads=[hb_], writes=[G.res["h2d"]], E=P.sp)
                for q4 in range(4):
                    pq = G.ps[q4]
                    P.mm([(lambda e, k=k: e.transpose(pq[:, (k % 4) * 128:(k % 4 + 1) * 128], hf[:, k * 128:(k + 1) * 128],
                                                      G.cst[:, C_ID:C_ID + 128])) for k in range(q4 * 4, q4 * 4 + 4)],
                         reads=[hf, G.cst], writes=[pq])
                    P.op(P.act if q4 % 2 else P.dve,
                         (lambda e: e.activation(out=hTf[:, q4 * 4:q4 * 4 + 4, :], in_=pq[:, :].rearrange("p (k n) -> p k n", k=4), func=AF.Copy))
                         if q4 % 2 else
                         (lambda e: e.tensor_copy(hTf[:, q4 * 4:q4 * 4 + 4, :], pq[:, :].rearrange("p (k n) -> p k n", k=4))),
                         reads=[pq], writes=[hTf])
                pl = G.ps[4]
                P.mm([(lambda e, k=k: e.matmul(pl[:, 0:NE], lhsT=hTf[:, k, :], rhs=rw[:, k, :], start=(k == 0), stop=(k == KC - 1)))
                      for k in range(KC)], reads=[hTf, rw], writes=[pl])
                P.op(P.dve, lambda e: e.tensor_tensor(out=lg[:, :], in0=pl[:, 0:NE], in1=rb[:, :], op=ALU.add),
                     reads=[pl, rb], writes=[lg])
                P.op(P.dve, lambda e: e.max(top[:, :], lg[:, :]), reads=[lg], writes=[top])
                mk, gt = route["mask"], route["gate"]
                P.op(P.dve, lambda e: e.tensor_scalar(out=mk[:, t, :], in0=lg[:, :], scalar1=top[:, 3:4], scalar2=None, op0=ALU.is_ge),
                     reads=[lg, top], writes=[mk])
                P.op(P.dve, lambda e: e.tensor_scalar(out=nm[:, :], in0=top[:, 0:1], scalar1=-1.0, scalar2=None, op0=ALU.mult),
                     reads=[top], writes=[nm])
                P.op(P.act, lambda e: e.activation(out=ex[:, :], in_=lg[:, :], func=AF.Exp, bias=nm[:, 0:1]),
                     reads=[lg, nm], writes=[ex])
                P.op(P.dve, lambda e: e.tensor_tensor(out=ex[:, :], in0=ex[:, :], in1=mk[:, t, :], op=ALU.mult),
                     reads=[ex, mk], writes=[ex])
                P.op(P.dve, lambda e: e.tensor_reduce(out=se[:, :], in_=ex[:, :], axis=AX.X, op=ALU.add), reads=[ex], writes=[se])
                P.op(P.dve, lambda e: e.reciprocal(se[:, :], se[:, :]), reads=[se], writes=[se])
                P.op(P.dve, lambda e: e.tensor_scalar(out=gt[:, t, :], in0=ex[:, :], scalar1=se[:, 0:1], scalar2=None, op0=ALU.mult),
                     reads=[ex, se], writes=[gt])
        if which == 2:
            pos = route["pos"]
            mk = route["mask"]
            for i, t in enumerate(tiles):
                pq = G.ps[5 + i % 2]
                fns = [(lambda e, tp=tp, j=j: e.matmul(pq[:, 0:NE], lhsT=G.cst[:, C_ONE:C_ONE + 128], rhs=mk[:, tp, :],
                                                      start=(j == 0), stop=False)) for j, tp in enumerate(tiles[:i])]
                fns.append(lambda e, i=i, t=t: e.matmul(pq[:, 0:NE], lhsT=G.cst[:, C_US:C_US + 128], rhs=mk[:, t, :],
                                                      start=(i == 0), stop=True))
                P.mm(fns, reads=[mk, G.cst], writes=[pq])
                P.op(P.act, lambda e: e.activation(out=pos[:, t, :], in_=pq[:, 0:NE], func=AF.Copy), reads=[pq], writes=[pos])


def linear_tm(G, es, W, groups, tiles, hT, post):
    P = G.P
    wst = [sb(G, es, "wst", [128, 8, 512], F32) for _ in range(2)]
    wb = [sb(G, es, "wb", [128, KC, 512], BF16) for _ in range(2)]
    cnt = 0
    for gi, (c0, n) in enumerate(groups):
        w = wb[gi % 2]
        for half in range(2):
            st = wst[half]
            P.dma(st[:, :, 0:n], W[half * 1024:(half + 1) * 1024, c0:c0 + n].rearrange("(k p) n -> p k n", p=128),
                  writes=[st], E=(P.sp if half == 0 else P.act))
            if half == 0:
                P.op(P.pool, lambda e: e.tensor_copy(w[:, 0:8, 0:n], st[:, :, 0:n]), reads=[st], writes=[w])
            else:
                P.op(P.act, lambda e: e.activation(out=w[:, 8:16, 0:n], in_=st[:, :, 0:n], func=AF.Copy), reads=[st], writes=[w])
        for t in tiles:
            pq = G.ps[cnt % 3]
            cnt += 1
            P.mm([(lambda e, k=k: e.matmul(pq[:, 0:n], lhsT=hT[:, k, t * 128:(t + 1) * 128], rhs=w[:, k, 0:n],
                                           start=(k == 0), stop=(k == KC - 1))) for k in range(KC)],
                 reads=[hT, w], writes=[pq])
            post(gi, t, pq, n)


def rope_heads(G, src, dst, cs, t, H, tmp):
    P = G.P
    a = src[:, 0:H, 0:64]
    b = src[:, 0:H, 64:128]
    cosb = cs[:, t - 2:t - 1, 0:64].to_broadcast([128, H, 64])
    sinb = cs[:, t - 2:t - 1, 64:128].to_broadcast([128, H, 64])
    t1, t2 = tmp[:, 0:H, 0:64], tmp[:, 0:H, 64:128]
    P.op(P.dve, lambda e: e.tensor_tensor(out=t1, in0=a, in1=cosb, op=ALU.mult), reads=[src, cs], writes=[tmp])
    P.op(P.dve, lambda e: e.tensor_tensor(out=t2, in0=b, in1=sinb, op=ALU.mult), reads=[src, cs], writes=[tmp])
    P.op(P.dve, lambda e: e.tensor_tensor(out=dst[:, 0:H, 0:64], in0=t1, in1=t2, op=ALU.subtract), reads=[tmp], writes=[dst])
    P.op(P.dve, lambda e: e.tensor_tensor(out=t1, in0=a, in1=sinb, op=ALU.mult), reads=[src, cs, dst], writes=[tmp])
    P.op(P.dve, lambda e: e.tensor_tensor(out=t2, in0=b, in1=cosb, op=ALU.mult), reads=[src, cs], writes=[tmp])
    P.op(P.dve, lambda e: e.tensor_tensor(out=dst[:, 0:H, 64:128], in0=t1, in1=t2, op=ALU.add), reads=[tmp], writes=[dst])


def phase_inproj0(G, hT):
    P, I, S = G.P, G.I, G.S
    groups = [(i * 512, 512) for i in range(11)]
    with ExitStack() as es:
        cs = sb(G, es, "cs", [128, 16, 128], F32)
        P.dma(cs[:, :, :], I["cossin"].rearrange("(t p) n -> p t n", p=128), writes=[cs])
        qg = bc_row(G, es, "qg", I["a_q_norm"][0, :], 128)
        kg = bc_row(G, es, "kg", I["a_k_norm"][0, :], 128)
        src = [sb(G, es, "src", [128, 4, 128], F32) for _ in range(2)]
        sq = sb(G, es, "sq", [128, 4, 128], F32)
        tmp = sb(G, es, "tmp", [128, 4, 128], F32)
        ob = [sb(G, es, "ob", [128, 4, 128], BF16) for _ in range(2)]
        oT = [sb(G, es, "oT", [128, 4, 128], BF16) for _ in range(2)]
        ss = sb(G, es, "ss", [128, 4], F32)
        rs = sb(G, es, "rs", [128, 4], F32)
        st = dict(i=0)

        def heads(pq, t, H, c_lo, gain, rope, scale, dT, h0, dTM, tm_c0):
            i = st["i"]
            st["i"] += 1
            s_, o_, oT_ = src[i % 2], ob[i % 2], oT[i % 2]
            pv = pq[:, c_lo:c_lo + H * 128].rearrange("p (h n) -> p h n", h=H)
            P.op(P.act, lambda e: e.activation(out=s_[:, 0:H, :], in_=pv, func=AF.Copy, scale=scale), reads=[pq], writes=[s_])
            if gain is not None:
                P.op(P.act, lambda e: e.activation(out=sq[:, 0:H, :], in_=s_[:, 0:H, :], func=AF.Square), reads=[s_], writes=[sq])
                P.op(P.dve, lambda e: e.tensor_reduce(out=ss[:, 0:H], in_=sq[:, 0:H, :], axis=AX.X, op=ALU.add), reads=[sq], writes=[ss])
                rstd_from_ss(G, ss, rs, H, 1.0 / 128)
                P.op(P.dve, lambda e: e.tensor_tensor(out=s_[:, 0:H, :], in0=s_[:, 0:H, :],
                                                      in1=rs[:, 0:H].unsqueeze(2).to_broadcast([128, H, 128]), op=ALU.mult),
                     reads=[s_, rs], writes=[s_])
                P.op(P.dve, lambda e: e.tensor_tensor(out=s_[:, 0:H, :], in0=s_[:, 0:H, :],
                                                      in1=gain[:, :].unsqueeze(1).to_broadcast([128, H, 128]), op=ALU.mult),
                     reads=[s_, gain], writes=[s_])
            if rope and t >= 2:
                rope_heads(G, s_, o_, cs, t, H, tmp)
            else:
                P.op(P.pool, lambda e: e.tensor_copy(o_[:, 0:H, :], s_[:, 0:H, :]), reads=[s_], writes=[o_])
            if dTM is not None:
                P.dma(dTM[0][t * 128:(t + 1) * 128, tm_c0:tm_c0 + H * 128], o_[:, 0:H, :].rearrange("p h n -> p (h n)"),
                      reads=[o_], writes=[dTM[1]], E=P.pool)
            if dT is not None:
                pt = G.ps[4 + i % 2]
                pvb = pt[:, 0:256].bitcast(BF16)
                P.mm([(lambda e, h=h: e.transpose(pvb[:, h * 128:(h + 1) * 128], o_[:, h, :], G.idb[:, :])) for h in range(H)],
                     reads=[o_, G.idb], writes=[pt])
                P.op(P.act, lambda e: e.activation(out=oT_[:, 0:H, :], in_=pvb[:, 0:H * 128].rearrange("p (h n) -> p h n", h=H), func=AF.Copy),
                     reads=[pt], writes=[oT_])
                P.dma(dT[0][h0:h0 + H, :, t * 128:(t + 1) * 128].rearrange("h d n -> d h n"), oT_[:, 0:H, :],
                      reads=[oT_], writes=[dT[1]], E=P.pool)

        R = G.res
        ksc = 128.0 ** -0.5

        def post(gi, t, pq, n):
            if gi == 0:
                heads(pq, t, 2, 0, kg, True, 1.0, (S["KTa"], R["KTa"]), 0, None, 0)
                heads(pq, t, 2, 256, None, False, 1.0, None, 0, (S["Va"], R["Va"]), 0)
            elif gi in (1, 2):
                heads(pq, t, 4, 0, None, True, ksc, (S["KTb"], R["KTb"]), (gi - 1) * 4, (S["Kb"], R["Kb"]), (gi - 1) * 512)
            elif gi in (3, 4):
                heads(pq, t, 4, 0, None, False, 1.0, None, 0, (S["Vb"], R["Vb"]), (gi - 3) * 512)
            elif gi in (5, 6):
                heads(pq, t, 4, 0, qg, True, 1.0, (S["QTa"], R["QTa"]), (gi - 5) * 4, None, 0)
            elif gi in (7, 8):
                heads(pq, t, 4, 0, None, True, 1.0, (S["QTb"], R["QTb"]), (gi - 7) * 4, None, 0)
            else:
                i = st["i"]
                st["i"] += 1
                o_ = ob[i % 2]
                P.op(P.act, lambda e: e.activation(out=o_[:, :, :], in_=pq[:, 0:512].rearrange("p (h n) -> p h n", h=4), func=AF.Silu),
                     reads=[pq], writes=[o_])
                P.dma(S["Gt"][t * 128:(t + 1) * 128, (gi - 9) * 512:(gi - 8) * 512], o_[:, :, :].rearrange("p h n -> p (h n)"),
                      reads=[o_], writes=[R["Gt"]], E=P.pool)

        linear_tm(G, es, I["ab_w_in"], groups, list(range(T)), hT, post)


def phase_attn(G):
    P, S, R = G.P, G.S, G.res
    sc = 128.0 ** -0.5
    with ExitStack() as es:
        KT = sb(G, es, "KT", [128, NT], BF16)
        V = sb(G, es, "V", [128, T, 128], BF16)
        QT = [sb(G, es, "QT", [128, 512], BF16) for _ in range(2)]
        PT = [sb(G, es, "PT", [128, 512], BF16) for _ in range(3)]
        rz = sb(G, es, "rz", [128, 512], F32)
        OT = [sb(G, es, "OT", [128, 512], BF16) for _ in range(2)]
        it = 0
        for hk in range(2):
            P.dma(KT[:, :], S["KTa"][hk, :, :], reads=[R["KTa"]], writes=[KT])
            P.dma(V[:, :, :], S["Va"][:, hk * 128:(hk + 1) * 128].rearrange("(t p) n -> p t n", p=128), reads=[R["Va"]], writes=[V])
            for h in range(hk * 4, hk * 4 + 4):
                chunks = [(0, 256, [0, 1])] + [(256 + 512 * i, 512, list(range(T))) for i in range(4)]
                for (q0, nq, keys) in chunks:
                    q = QT[it % 2]
                    o = OT[it % 2]
                    it += 1
                    P.dma(q[:, 0:nq], S["QTa"][h, :, q0:q0 + nq], reads=[R["QTa"]], writes=[q], E=P.act)
                    po, pz = G.ps[6], G.ps[7]
                    nk = len(keys)
                    for j, kt in enumerate(keys):
                        pS = G.ps[j % 3]
                        p_ = PT[j % 3]
                        P.mm([lambda e: e.matmul(pS[:, 0:nq], lhsT=KT[:, kt * 128:(kt + 1) * 128], rhs=q[:, 0:nq], start=True, stop=True)],
                             reads=[KT, q], writes=[pS])
                        P.op(P.act, lambda e: e.activation(out=p_[:, 0:nq], in_=pS[:, 0:nq], func=AF.Exp, scale=sc), reads=[pS], writes=[p_])
                        P.mm([lambda e: e.matmul(po[:, 0:nq], lhsT=V[:, kt, :], rhs=p_[:, 0:nq], start=(j == 0), stop=(j == nk - 1)),
                              lambda e: e.matmul(pz[:, 0:nq], lhsT=G.oneb[:, :], rhs=p_[:, 0:nq], start=(j == 0), stop=(j == nk - 1))],
                             reads=[V, p_, G.oneb], writes=[po, pz])
                    P.op(P.dve, lambda e: e.reciprocal(rz[:, 0:nq], pz[:, 0:nq]), reads=[pz], writes=[rz])
                    P.op(P.dve, lambda e: e.tensor_tensor(out=o[:, 0:nq], in0=po[:, 0:nq], in1=rz[:, 0:nq], op=ALU.mult),
                         reads=[po, rz], writes=[o])
                    P.dma(S["mixT"][h, :, q0:q0 + nq], o[:, 0:nq], reads=[o], writes=[R["mixT"]], E=P.pool)


def phase_ret(G):
    P, I, S, R = G.P, G.I, G.S, G.res
    fwd = list(range(T))
    bwd = [1, 0] + list(range(T - 1, 1, -1))
    with ExitStack() as es:
        dexp = bc_row(G, es, "dexp", I["b_decay_exp"][0, :], 16)
        lgm = sb(G, es, "lgm", [128, 16], F32)
        P.op(P.act, lambda e: e.activation(out=lgm[:, :], in_=dexp[:, :], func=AF.Exp, scale=-math.log(2.0)), reads=[dexp], writes=[lgm])
        P.op(P.act, lambda e: e.activation(out=lgm[:, :], in_=lgm[:, :], func=AF.Ln, scale=-1.0, bias=G.cst[:, C_COL + 3:C_COL + 4]),
             reads=[lgm, G.cst], writes=[lgm])
        gnb = bc_row(G, es, "gnb", I["b_gn"][0, :], 1024)
        QT = sb(G, es, "rQT", [128, NT], BF16)
        KT = sb(G, es, "rKT", [128, NT], BF16)
        Kt = sb(G, es, "rK", [128, T, 128], BF16)
        Vt = sb(G, es, "rV", [128, T, 128], BF16)
        Gt = sb(G, es, "rG", [128, T, 128], BF16)
        Dm = sb(G, es, "Dm", [128, 128], F32)
        Dt = sb(G, es, "Dt", [128, 128], F32)
        qdf = sb(G, es, "qdf", [128, 128], F32)
        qdb = sb(G, es, "qdb", [128, 128], F32)
        col = sb(G, es, "col", [128, 4], F32)
        Sb = sb(G, es, "Sb", [128, T, 128], BF16)
        Sf32 = sb(G, es, "Sf32", [128, 128], F32)
        Sfb = [sb(G, es, "Sfb", [128, 128], BF16) for _ in range(2)]
        Kd = [sb(G, es, "Kd", [128, 128], BF16) for _ in range(2)]
        AT = [sb(G, es, "AT", [128, 128], BF16) for _ in range(2)]
        Qf = [sb(G, es, "Qf", [128, 128], BF16) for _ in range(2)]
        Qb = [sb(G, es, "Qb", [128, 128], BF16) for _ in range(2)]
        O = sb(G, es, "O", [128, T, 128], F32)
        sq = sb(G, es, "osq", [128, T, 128], F32)
        st1 = sb(G, es, "st1", [128, T], F32)
        st2 = sb(G, es, "st2", [128, T], F32)
        ob = sb(G, es, "rob", [128, T, 128], BF16)
        oT = [sb(G, es, "roT", [128, 4, 128], BF16) for _ in range(2)]
        cst = G.cst
        for h in range(8):
            P.dma(QT[:, :], S["QTb"][h, :, :], reads=[R["QTb"]], writes=[QT])
            P.dma(KT[:, :], S["KTb"][h, :, :], reads=[R["KTb"]], writes=[KT], E=P.act)
            P.dma(Kt[:, :, :], S["Kb"][:, h * 128:(h + 1) * 128].rearrange("(t p) n -> p t n", p=128), reads=[R["Kb"]], writes=[Kt])
            P.dma(Vt[:, :, :], S["Vb"][:, h * 128:(h + 1) * 128].rearrange("(t p) n -> p t n", p=128), reads=[R["Vb"]], writes=[Vt], E=P.act)
            P.dma(Gt[:, :, :], S["Gt"][:, h * 128:(h + 1) * 128].rearrange("(t p) n -> p t n", p=128), reads=[R["Gt"]], writes=[Gt])
            lf, lb_ = lgm[:, h:h + 1], lgm[:, 8 + h:9 + h]
            P.op(P.act, lambda e: e.activation(out=Dm[:, :], in_=cst[:, C_RGE:C_RGE + 128], func=AF.Exp, scale=lf), reads=[cst, lgm], writes=[Dm])
            P.op(P.dve, lambda e: e.tensor_tensor(out=Dm[:, :], in0=Dm[:, :], in1=cst[:, C_MGE:C_MGE + 128], op=ALU.mult), reads=[Dm, cst], writes=[Dm])
            P.op(P.act, lambda e: e.activation(out=Dt[:, :], in_=cst[:, C_RLE:C_RLE + 128], func=AF.Exp, scale=lb_), reads=[cst, lgm], writes=[Dt])
            P.op(P.dve, lambda e: e.tensor_tensor(out=Dt[:, :], in0=Dt[:, :], in1=cst[:, C_MLE:C_MLE + 128], op=ALU.mult), reads=[Dt, cst], writes=[Dt])
            P.op(P.dve, lambda e: e.tensor_tensor(out=Dm[:, :], in0=Dm[:, :], in1=Dt[:, :], op=ALU.add), reads=[Dm, Dt], writes=[Dm])
            P.op(P.act, lambda e: e.activation(out=qdf[:, :], in_=cst[:, C_JP1:C_JP1 + 128], func=AF.Exp, scale=lf), reads=[cst, lgm], writes=[qdf])
            P.op(P.act, lambda e: e.activation(out=qdb[:, :], in_=cst[:, C_CMJ:C_CMJ + 128], func=AF.Exp, scale=lb_), reads=[cst, lgm], writes=[qdb])
            P.op(P.act, lambda e: e.activation(out=col[:, 0:1], in_=cst[:, C_COL:C_COL + 1], func=AF.Exp, scale=lf), reads=[cst, lgm], writes=[col])
            P.op(P.act, lambda e: e.activation(out=col[:, 1:2], in_=cst[:, C_COL + 1:C_COL + 2], func=AF.Exp, scale=lb_), reads=[cst, lgm], writes=[col])
            P.op(P.act, lambda e: e.activation(out=col[:, 2:3], in_=lf, func=AF.Exp, scale=128.0), reads=[lgm], writes=[col])
            P.op(P.act, lambda e: e.activation(out=col[:, 3:4], in_=lb_, func=AF.Exp, scale=128.0), reads=[lgm], writes=[col])

            def state_step(c, kcol, gcol, i):
                kd = Kd[i % 2]
                pu = G.ps[4 + i % 2]
                P.op(P.pool, lambda e: e.tensor_scalar(out=kd[:, :], in0=Kt[:, c, :], scalar1=col[:, kcol:kcol + 1], scalar2=None, op0=ALU.mult),
                     reads=[Kt, col], writes=[kd])
                P.mm([lambda e: e.matmul(pu[:, 0:128], lhsT=kd[:, :], rhs=Vt[:, c, :], start=True, stop=True)], reads=[kd, Vt], writes=[pu])
                P.op(P.dve, lambda e: e.scalar_tensor_tensor(out=Sf32[:, :], in0=Sf32[:, :], scalar=col[:, gcol:gcol + 1], in1=pu[:, 0:128],
                                                             op0=ALU.mult, op1=ALU.add), reads=[Sf32, col, pu], writes=[Sf32])

            P.op(P.dve, lambda e: e.memset(Sf32[:, :], 0.0), writes=[Sf32])
            for i, c in enumerate(bwd):
                P.op(P.act, lambda e: e.activation(out=Sb[:, c, :], in_=Sf32[:, :], func=AF.Copy), reads=[Sf32], writes=[Sb])
                if i < T - 1:
                    state_step(c, 1, 3, i)
            P.op(P.dve, lambda e: e.memset(Sf32[:, :], 0.0), writes=[Sf32])
            for i, c in enumerate(fwd):
                sfb, at, qf, qb = Sfb[i % 2], AT[i % 2], Qf[i % 2], Qb[i % 2]
                pS, pO = G.ps[i % 2], G.ps[2 + i % 2]
                P.op(P.act, lambda e: e.activation(out=sfb[:, :], in_=Sf32[:, :], func=AF.Copy), reads=[Sf32], writes=[sfb])
                P.mm([lambda e: e.matmul(pS[:, 0:128], lhsT=KT[:, c * 128:(c + 1) * 128], rhs=QT[:, c * 128:(c + 1) * 128], start=True, stop=True)],
                     reads=[KT, QT], writes=[pS])
                P.op(P.dve, lambda e: e.tensor_tensor(out=at[:, :], in0=pS[:, 0:128], in1=Dm[:, :], op=ALU.mult), reads=[pS, Dm], writes=[at])
                P.op(P.pool, lambda e: e.tensor_tensor(out=qf[:, :], in0=QT[:, c * 128:(c + 1) * 128], in1=qdf[:, :], op=ALU.mult), reads=[QT, qdf], writes=[qf])
                P.op(P.pool, lambda e: e.tensor_tensor(out=qb[:, :], in0=QT[:, c * 128:(c + 1) * 128], in1=qdb[:, :], op=ALU.mult), reads=[QT, qdb], writes=[qb])
                P.mm([lambda e: e.matmul(pO[:, 0:128], lhsT=at[:, :], rhs=Vt[:, c, :], start=True, stop=False),
                      lambda e: e.matmul(pO[:, 0:128], lhsT=qf[:, :], rhs=sfb[:, :], start=False, stop=False),
                      lambda e: e.matmul(pO[:, 0:128], lhsT=qb[:, :], rhs=Sb[:, c, :], start=False, stop=True)],
                     reads=[at, Vt, qf, sfb, qb, Sb], writes=[pO])
                P.op(P.act, lambda e: e.activation(out=O[:, c, :], in_=pO[:, 0:128], func=AF.Copy), reads=[pO], writes=[O])
                if i < T - 1:
                    state_step(c, 0, 2, i)
            P.op(P.dve, lambda e: e.tensor_reduce(out=st1[:, :], in_=O[:, :, :], axis=AX.X, op=ALU.add), reads=[O], writes=[st1])
            P.op(P.dve, lambda e: e.tensor_scalar(out=st1[:, :], in0=st1[:, :], scalar1=1.0 / 128, scalar2=None, op0=ALU.mult), reads=[st1], writes=[st1])
            P.op(P.dve, lambda e: e.tensor_tensor(out=O[:, :, :], in0=O[:, :, :], in1=st1[:, :].unsqueeze(2).to_broadcast([128, T, 128]), op=ALU.subtract),
                 reads=[O, st1], writes=[O])
            P.op(P.act, lambda e: e.activation(out=sq[:, :, :], in_=O[:, :, :], func=AF.Square), reads=[O], writes=[sq])
            P.op(P.dve, lambda e: e.tensor_reduce(out=st2[:, :], in_=sq[:, :, :], axis=AX.X, op=ALU.add), reads=[sq], writes=[st2])
            P.op(P.act, lambda e: e.activation(out=st2[:, :], in_=st2[:, :], func=AF.Sqrt, scale=1.0 / 128, bias=cst[:, C_COL + 2:C_COL + 3]),
                 reads=[st2, cst], writes=[st2])
            P.op(P.dve, lambda e: e.reciprocal(st2[:, :], st2[:, :]), reads=[st2], writes=[st2])
            P.op(P.dve, lambda e: e.tensor_tensor(out=O[:, :, :], in0=O[:, :, :], in1=st2[:, :].unsqueeze(2).to_broadcast([128, T, 128]), op=ALU.mult),
                 reads=[O, st2], writes=[O])
            P.op(P.dve, lambda e: e.tensor_tensor(out=O[:, :, :], in0=O[:, :, :], in1=gnb[:, h * 128:(h + 1) * 128].unsqueeze(1).to_broadcast([128, T, 128]), op=ALU.mult),
                 reads=[O, gnb], writes=[O])
            P.op(P.dve, lambda e: e.tensor_tensor(out=ob[:, :, :], in0=O[:, :, :], in1=Gt[:, :, :], op=ALU.mult), reads=[O, Gt], writes=[ob])
            transpose_out(G, ob, oT, S["mixT"], R["mixT"], 8 + h, list(range(T)))


def transpose_out(G, ob, oT, dst, dres, chunk, tiles):
    P = G.P
    for g0 in range(0, len(tiles), 4):
        ts = tiles[g0:g0 + 4]
        n = len(ts)
        pt = G.ps[6 + (g0 // 4) % 2]
        o_ = oT[(g0 // 4) % 2]
        pvb = pt[:, 0:256].bitcast(BF16)
        P.mm([(lambda e, j=j, t=t: e.transpose(pvb[:, j * 128:(j + 1) * 128], ob[:, t, :], G.idb[:, :])) for j, t in enumerate(ts)],
             reads=[ob, G.idb], writes=[pt])
        P.op(P.act, lambda e: e.activation(out=o_[:, 0:n, :], in_=pvb[:, 0:n * 128].rearrange("p (h n) -> p h n", h=n), func=AF.Copy),
             reads=[pt], writes=[o_])
        P.dma(dst[chunk, :, ts[0] * 128:(ts[0] + n) * 128], o_[:, 0:n, :].rearrange("p h n -> p (h n)"), reads=[o_], writes=[dres], E=P.pool)


def phase_outproj(G, l, hT, tiles):
    P, I, S, R = G.P, G.I, G.S, G.res
    W = I["ab_w_out"] if l == 0 else I["c_w_out"]
    with ExitStack() as es:
        P.dma(hT[:, :, :], S["mixT"].rearrange("k p n -> p k n"), reads=[R["mixT"]], writes=[hT])
        gate = [bc_row(G, es, "og", S["modD"][l, r, 2 * D:3 * D], D, E=P.act) for r in range(2)]
        xo = [sb(G, es, "xo", [128, 512], F32) for _ in range(2)]
        xn = [sb(G, es, "xn", [128, 512], F32) for _ in range(2)]
        st = dict(i=0)

        def post(gi, t, pq, n):
            i = st["i"]
            st["i"] += 1
            a, b = xo[i % 2], xn[i % 2]
            if l == 0:
                src = I["ctx"][t * 128:(t + 1) * 128, gi * 512:(gi + 1) * 512] if t < 2 else I["x"][(t - 2) * 128:(t - 1) * 128, gi * 512:(gi + 1) * 512]
                rd = []
            else:
                src = S["xres"][t * 128:(t + 1) * 128, gi * 512:(gi + 1) * 512]
                rd = [R["xres"]]
            P.dma(a[:, :], src, reads=rd, writes=[a], E=P.act)
            r = 1 if t < 2 else 0
            P.op(P.dve, lambda e: e.tensor_tensor(out=b[:, :], in0=pq[:, 0:512], in1=gate[r][:, gi * 512:(gi + 1) * 512], op=ALU.mult),
                 reads=[pq, gate[r]], writes=[b])
            P.op(P.pool, lambda e: e.tensor_tensor(out=b[:, :], in0=b[:, :], in1=a[:, :], op=ALU.add), reads=[b, a], writes=[b])
            P.dma(S["xres"][t * 128:(t + 1) * 128, gi * 512:(gi + 1) * 512], b[:, :], reads=[b], writes=[R["xres"]], E=P.pool)

        linear_tm(G, es, W, [(i * 512, 512) for i in range(4)], tiles, hT, post)


def phase_moe(G, l, tiles, h2, route, bigA):
    P, I, S, R, nc = G.P, G.I, G.S, G.res, G.nc
    nT = len(tiles)
    NB = -(-(4 * nT * 128 + NE * 255) // 256)
    BIG = 1.0e6
    mk, gt, pos = route["mask"], route["gate"], route["pos"]
    dselA, gselA = route["dsel"], route["gsel"]
    cst = G.cst
    t0, t1 = tiles[0], tiles[-1] + 1
    with ExitStack() as es:
        b1T = sb(G, es, "b1T", [128, 2 * KC, NE], F32)
        with ExitStack() as es1:
            b1s = sb(G, es1, "b1s", [NE, 2 * D], F32)
            P.dma(b1s[:, :], I["exp_b1"][l, :, :], writes=[b1s])
            for two in range(2):
                pq = G.ps[two]
                P.mm([(lambda e, c=c: e.transpose(pq[:, c * 32:(c + 1) * 32], b1s[:, c * 256 + two:(c + 1) * 256:2], cst[0:32, C_ID:C_ID + 32]))
                      for c in range(KC)], reads=[b1s, cst], writes=[pq])
                P.op(P.dve, lambda e: e.tensor_copy(b1T[:, two:2 * KC:2, :], pq[:, :].rearrange("p (c n) -> p c n", c=KC)), reads=[pq], writes=[b1T])
            P.barrier()
        cnt = sb(G, es, "cnt", [128, NE], F32)
        nblk = sb(G, es, "nblk", [128, NE], F32)
        bend = sb(G, es, "bend", [128, NE], F32)
        bst = sb(G, es, "bst", [128, NE], F32)
        tmpe = sb(G, es, "tmpe", [128, NE], F32)
        destm = sb(G, es, "destm", [128, T, NE], F32)
        oh = sb(G, es, "oh", [128, NB, NE], F32)
        oh2 = sb(G, es, "oh2", [128, NB, NE], F32)
        bef = sb(G, es, "bef", [128, NB], F32)
        bei = sb(G, es, "bei", [128, NB], mybir.dt.int32)
        tmp3 = sb(G, es, "tmp3", [128, T, NE], F32)
        b1tmp = sb(G, es, "b1tmp", [128, 2 * KC, NE], F32)
        b1sel = [sb(G, es, "b1sel", [128, 2 * KC], F32) for _ in range(2)]
        pq = G.ps[0]
        P.mm([(lambda e, j=j, t=t: e.matmul(pq[:, 0:NE], lhsT=cst[:, C_ONE:C_ONE + 128], rhs=mk[:, t, :], start=(j == 0), stop=(j == nT - 1)))
              for j, t in enumerate(tiles)], reads=[mk, cst], writes=[pq])
        P.op(P.dve, lambda e: e.tensor_copy(cnt[:, :], pq[:, 0:NE]), reads=[pq], writes=[cnt])
        P.op(P.dve, lambda e: e.tensor_scalar(out=nblk[:, :], in0=cnt[:, :], scalar1=0.0, scalar2=None, op0=ALU.is_gt), reads=[cnt], writes=[nblk])
        for j in range(1, 9):
            P.op(P.dve, lambda e: e.tensor_scalar(out=tmpe[:, :], in0=cnt[:, :], scalar1=256.0 * j, scalar2=None, op0=ALU.is_gt), reads=[cnt], writes=[tmpe])
            P.op(P.dve, lambda e: e.tensor_tensor(out=nblk[:, :], in0=nblk[:, :], in1=tmpe[:, :], op=ALU.add), reads=[nblk, tmpe], writes=[nblk])
        P.op(P.dve, lambda e: e.tensor_tensor_scan(out=bend[:, :], data0=cst[:, C_ONE:C_ONE + NE], data1=nblk[:, :], initial=0.0,
                                                   op0=ALU.mult, op1=ALU.add), reads=[cst, nblk], writes=[bend])
        P.op(P.dve, lambda e: e.tensor_tensor(out=bst[:, :], in0=bend[:, :], in1=nblk[:, :], op=ALU.subtract), reads=[bend, nblk], writes=[bst])
        P.op(P.dve, lambda e: e.tensor_scalar(out=tmpe[:, :], in0=bst[:, :], scalar1=256.0, scalar2=BIG, op0=ALU.mult, op1=ALU.add), reads=[bst], writes=[tmpe])
        P.op(P.dve, lambda e: e.tensor_tensor(out=destm[:, t0:t1, :], in0=pos[:, t0:t1, :], in1=tmpe[:, :].unsqueeze(1).to_broadcast([128, nT, NE]), op=ALU.add),
             reads=[pos, tmpe], writes=[destm])
        P.op(P.dve, lambda e: e.tensor_tensor(out=destm[:, t0:t1, :], in0=destm[:, t0:t1, :], in1=mk[:, t0:t1, :], op=ALU.mult), reads=[destm, mk], writes=[destm])
        P.op(P.dve, lambda e: e.tensor_scalar(out=destm[:, t0:t1, :], in0=destm[:, t0:t1, :], scalar1=-BIG, scalar2=None, op0=ALU.add), reads=[destm], writes=[destm])
        iob = cst[:, C_IOTA:C_IOTA + NB].unsqueeze(2).to_broadcast([128, NB, NE])
        P.op(P.dve, lambda e: e.tensor_tensor(out=oh[:, :, :], in0=iob, in1=bst[:, :].unsqueeze(1).to_broadcast([128, NB, NE]), op=ALU.is_ge), reads=[cst, bst], writes=[oh])
        P.op(P.dve, lambda e: e.tensor_tensor(out=oh2[:, :, :], in0=iob, in1=bend[:, :].unsqueeze(1).to_broadcast([128, NB, NE]), op=ALU.is_lt), reads=[cst, bend], writes=[oh2])
        P.op(P.dve, lambda e: e.tensor_tensor(out=oh[:, :, :], in0=oh[:, :, :], in1=oh2[:, :, :], op=ALU.mult), reads=[oh, oh2], writes=[oh])
        P.op(P.dve, lambda e: e.tensor_tensor(out=oh2[:, :, :], in0=oh[:, :, :], in1=cst[:, C_IOTA:C_IOTA + NE].unsqueeze(1).to_broadcast([128, NB, NE]), op=ALU.mult),
             reads=[oh, cst], writes=[oh2])
        P.op(P.dve, lambda e: e.tensor_reduce(out=bef[:, :], in_=oh2[:, :, :], axis=AX.X, op=ALU.add), reads=[oh2], writes=[bef])
        P.op(P.dve, lambda e: e.tensor_copy(bei[:, :], bef[:, :]), reads=[bef], writes=[bei])
        for b in range(NB):
            ohb = oh[:, b:b + 1, :].to_broadcast([128, nT, NE])
            P.op(P.dve, lambda e: e.tensor_tensor(out=tmp3[:, t0:t1, :], in0=destm[:, t0:t1, :], in1=ohb, op=ALU.mult), reads=[destm, oh], writes=[tmp3])
            P.op(P.dve, lambda e: e.tensor_reduce(out=dselA[:, b, t0:t1], in_=tmp3[:, t0:t1, :], axis=AX.X, op=ALU.add), reads=[tmp3], writes=[dselA])
            P.op(P.pool, lambda e: e.tensor_tensor(out=oh2[:, 0:nT, :], in0=gt[:, t0:t1, :], in1=ohb, op=ALU.mult), reads=[gt, oh], writes=[oh2])
            P.op(P.dve, lambda e: e.tensor_reduce(out=gselA[:, b, t0:t1], in_=oh2[:, 0:nT, :], axis=AX.X, op=ALU.add), reads=[oh2], writes=[gselA])
            P.op(P.dve, lambda e: e.tensor_scalar(out=dselA[:, b, t0:t1], in0=dselA[:, b, t0:t1], scalar1=-256.0 * b, scalar2=None, op0=ALU.add),
                 reads=[dselA], writes=[dselA])
        Sall = sb(G, es, "Sall", [128, nT, 256], BF16)
        hgT = sb(G, es, "hgT", [128, KC, 256], BF16)
        actT = sb(G, es, "actT", [128, KC, 256], BF16)
        NWB = 3
        stgv = [V(bigA.t[:, i * 8192:(i + 1) * 8192].bitcast(F32), Res()) for i in range(3)]
        wbb = [V(bigA.t[:, 24576 + i * 4096:24576 + (i + 1) * 4096], Res()) for i in range(NWB)]
        gg = [sb(G, es, "gg", [128, 256], F32) for _ in range(2)]
        sg = [sb(G, es, "sg", [128, 256], F32) for _ in range(2)]
        ll = [sb(G, es, "ll", [128, 256], F32) for _ in range(2)]
        oS = [sb(G, es, "oS", [128, 256], BF16) for _ in range(3)]
        be1 = sb(G, es, "be1", [128, NB], F32)
        be2 = sb(G, es, "be2", [128, NB], F32)
        idx = [sb(G, es, "idx", [128, 48], mybir.dt.int32) for _ in range(2)]
        P.op(P.dve, lambda e: e.tensor_scalar(out=be1[:, :], in0=bef[:, :], scalar1=2048.0, scalar2=None, op0=ALU.mult), reads=[bef], writes=[be1])
        P.op(P.dve, lambda e: e.tensor_scalar(out=be2[:, :], in0=bef[:, :], scalar1=1024.0, scalar2=None, op0=ALU.mult), reads=[bef], writes=[be2])
        wi = dict(i=0)

        class WV:
            def __init__(s_, tl):
                s_.ap = tl.ap.rearrange("p (k n) -> p k n", k=KC)
                s_.r = tl.r

            def __getitem__(s_, k):
                return s_.ap[k]

        def load_block(ix, which, piece):
            i = wi["i"]
            wi["i"] += 1
            w = wbb[i % NWB]
            st = stgv[i % 3]
            W = I["w1t%d" % l] if which == 1 else I["w2t%d" % l]
            j = piece if which == 1 else 32 + piece
            P.dma(None, None, reads=[ix], writes=[st], E=P.pool,
                  fn=lambda e: e.indirect_dma_start(out=st[:, :], out_offset=None, in_=W[:, :],
                                                    in_offset=bass.IndirectOffsetOnAxis(ap=ix[:, j:j + 1], axis=0)))
            if i % 3 != 2:
                P.op(P.act, lambda e: e.activation(out=w[:, :], in_=st[:, :], func=AF.Copy), reads=[st], writes=[w])
            else:
                P.op(P.dve, lambda e: e.tensor_copy(w[:, :], st[:, :]), reads=[st], writes=[w])
            return WV(w)

        hg = [sb(G, es, "hg", [128, 2, D], BF16) for _ in range(2)]
        tokpt = sb(G, es, "tokpt", [128, nT, 2], BF16)
        gidx = [sb(G, es, "gidx", [128, 2], mybir.dt.int32) for _ in range(2)]
        gidf = [sb(G, es, "gidf", [128, 4], F32) for _ in range(2)]
        for j, t in enumerate(tiles):
            P.op(P.dve, lambda e: e.tensor_copy(tokpt[:, j, 0:1], cst[:, C_COL + 1:C_COL + 2]), reads=[cst], writes=[tokpt])
            P.op(P.dve, lambda e: e.memset(tokpt[:, j, 1:2], float(t)), writes=[tokpt])

        def prep(b):
            ix = idx[b % 2]
            P.op(P.dve, lambda e: e.tensor_scalar(out=ix[:, 0:32], in0=cst[:, C_OFF1:C_OFF1 + 32], scalar1=be1[:, b:b + 1], scalar2=None, op0=ALU.add),
                 reads=[cst, be1], writes=[ix])
            P.op(P.dve, lambda e: e.tensor_scalar(out=ix[:, 32:48], in0=cst[:, C_OFF2:C_OFF2 + 16], scalar1=be2[:, b:b + 1], scalar2=None, op0=ALU.add),
                 reads=[cst, be2], writes=[ix])
            bs = b1sel[b % 2]
            P.op(P.dve, lambda e: e.tensor_tensor(out=b1tmp[:, :, :], in0=b1T[:, :, :], in1=oh[:, b:b + 1, :].to_broadcast([128, 2 * KC, NE]), op=ALU.mult),
                 reads=[b1T, oh], writes=[b1tmp])
            P.op(P.dve, lambda e: e.tensor_reduce(out=bs[:, :], in_=b1tmp[:, :, :], axis=AX.X, op=ALU.add), reads=[b1tmp], writes=[bs])
            for j, t in enumerate(tiles):
                P.op(P.dve, lambda e: e.tensor_scalar(out=Sall[:, j, :], in0=cst[:, C_IOTA:C_IOTA + 256], scalar1=dselA[:, b, t:t + 1],
                                                      scalar2=None, op0=ALU.is_equal), reads=[cst, dselA], writes=[Sall])
            pq = G.ps[b % 2]
            P.mm([(lambda e, j=j, s_i=s_i: e.matmul(pq[:, 2 * s_i:2 * s_i + 2], lhsT=Sall[:, j, s_i * 128:(s_i + 1) * 128], rhs=tokpt[:, j, :],
                                                   start=(j == 0), stop=(j == nT - 1))) for s_i in range(2) for j in range(nT)],
                 reads=[Sall, tokpt], writes=[pq])
            gi = gidx[b % 2]
            gf = gidf[b % 2]
            P.op(P.dve, lambda e: e.tensor_copy(gf[:, :], pq[:, 0:4]), reads=[pq], writes=[gf])
            P.op(P.dve, lambda e: e.scalar_tensor_tensor(out=gi[:, :], in0=gf[:, 1:4:2], scalar=128.0, in1=gf[:, 0:4:2], op0=ALU.mult, op1=ALU.add),
                 reads=[gf], writes=[gi])
            g_ = hg[b % 2]
            for s_i in range(2):
                P.dma(None, None, reads=[gi, R["h2d"]], writes=[g_], E=P.pool,
                      fn=lambda e: e.indirect_dma_start(out=g_[:, s_i, :], out_offset=None, in_=S["h2d"][:, :],
                                                        in_offset=bass.IndirectOffsetOnAxis(ap=gi[:, s_i:s_i + 1], axis=0)))

        oi = 0
        prep(0)
        for b in range(NB):
            if b + 1 < NB:
                prep(b + 1)
            ix = idx[b % 2]
            bs = b1sel[b % 2]
            g_ = hg[b % 2]
            for s_i in range(2):
                for half in range(2):
                    pq = G.ps[(2 * s_i + half) % 2]
                    pvb = pq[:, 0:512].bitcast(BF16)
                    P.mm([(lambda e, k=k: e.transpose(pvb[:, (k % 8) * 128:(k % 8 + 1) * 128], g_[:, s_i, k * 128:(k + 1) * 128], G.idb[:, :]))
                          for k in range(half * 8, half * 8 + 8)], reads=[g_, G.idb], writes=[pq])
                    dst = hgT[:, half * 8:half * 8 + 8, s_i * 128:(s_i + 1) * 128]
                    if half:
                        P.op(P.act, lambda e: e.activation(out=dst, in_=pvb.rearrange("p (k n) -> p k n", k=8), func=AF.Copy), reads=[pq], writes=[hgT])
                    else:
                        P.op(P.dve, lambda e: e.tensor_copy(dst, pvb.rearrange("p (k n) -> p k n", k=8)), reads=[pq], writes=[hgT])
            for c in range(KC):
                w = load_block(ix, 1, c)
                pg, pl = G.ps[2 + c % 2], G.ps[4 + c % 2]
                g_, s_, l_ = gg[c % 2], sg[c % 2], ll[c % 2]
                P.mm([(lambda e, k=k: e.matmul(pg[:, 0:256], lhsT=w[:, k, 0:128], rhs=hgT[:, k, :], start=(k == 0), stop=(k == KC - 1)))
                      for k in range(KC)], reads=[w, hgT], writes=[pg])
                P.mm([(lambda e, k=k: e.matmul(pl[:, 0:256], lhsT=w[:, k, 128:256], rhs=hgT[:, k, :], start=(k == 0), stop=(k == KC - 1)))
                      for k in range(KC)], reads=[w, hgT], writes=[pl])
                P.op(P.dve, lambda e: e.tensor_scalar(out=g_[:, :], in0=pg[:, 0:256], scalar1=bs[:, 2 * c:2 * c + 1], scalar2=7.0,
                                                      op0=ALU.add, op1=ALU.min), reads=[pg, bs], writes=[g_])
                P.op(P.act, lambda e: e.activation(out=s_[:, :], in_=g_[:, :], func=AF.Sigmoid, scale=1.702), reads=[g_], writes=[s_])
                P.op(P.dve, lambda e: e.tensor_scalar(out=l_[:, :], in0=pl[:, 0:256], scalar1=bs[:, 2 * c + 1:2 * c + 2], scalar2=7.0,
                                                      op0=ALU.add, op1=ALU.min), reads=[pl, bs], writes=[l_])
                P.op(P.dve, lambda e: e.tensor_scalar(out=l_[:, :], in0=l_[:, :], scalar1=-7.0, scalar2=1.0, op0=ALU.max, op1=ALU.add),
                     reads=[l_], writes=[l_])
                P.op(P.dve, lambda e: e.tensor_tensor(out=g_[:, :], in0=g_[:, :], in1=s_[:, :], op=ALU.mult), reads=[g_, s_], writes=[g_])
                P.op(P.dve, lambda e: e.tensor_tensor(out=actT[:, c, :], in0=g_[:, :], in1=l_[:, :], op=ALU.mult), reads=[g_, l_], writes=[actT])
            for n in range(8):
                w = load_block(ix, 2, n)
                for s_i in range(2):
                    pq = G.ps[6 + oi % 2]
                    o_ = oS[oi % 3]
                    oi += 1
                    P.mm([(lambda e, k=k: e.matmul(pq[:, 0:256], lhsT=actT[:, k, s_i * 128:(s_i + 1) * 128], rhs=w[:, k, :],
                                                   start=(k == 0), stop=(k == KC - 1))) for k in range(KC)], reads=[actT, w], writes=[pq])
                    if oi % 2:
                        P.op(P.act, lambda e: e.activation(out=o_[:, :], in_=pq[:, 0:256], func=AF.Copy), reads=[pq], writes=[o_])
                    else:
                        P.op(P.dve, lambda e: e.tensor_copy(o_[:, :], pq[:, 0:256]), reads=[pq], writes=[o_])
                    P.dma(S["outE"][b, s_i * 128:(s_i + 1) * 128, n * 256:(n + 1) * 256], o_[:, :], reads=[o_], writes=[R["outE"]], E=P.sp)
    P.barrier()
    with ExitStack() as es:
        acc = bigA.t[:, :].bitcast(F32).rearrange("p (t n) -> p t n", n=D)
        accR = bigA.r
        oE = [sb(G, es, "oE", [128, 2, D], BF16) for _ in range(2)]
        Sg = [sb(G, es, "Sg", [128, 256], BF16) for _ in range(2)]
        SgT = [sb(G, es, "SgT", [128, 2, 128], BF16) for _ in range(2)]
        b2s = sb(G, es, "b2s", [NE, D], F32)
        gT = sb(G, es, "gT", [NE, 128], F32)
        xt = [sb(G, es, "mxt", [128, D], F32) for _ in range(2)]
        gate = [bc_row(G, es, "mg", S["modD"][l, r, 5 * D:6 * D], D, E=P.act) for r in range(2)]
        P.dma(b2s[:, :], I["exp_b2"][l, :, :], writes=[b2s])
        for g0 in range(0, nT, 9):
            grp = tiles[g0:g0 + 9]
            for j, t in enumerate(grp):
                pt = G.ps[4]
                P.mm([lambda e: e.transpose(pt[0:NE, 0:128], gt[:, t, :], cst[:, C_ID:C_ID + 128])], reads=[gt, cst], writes=[pt])
                P.op(P.act, lambda e: e.activation(out=gT[:, :], in_=pt[0:NE, 0:128], func=AF.Copy), reads=[pt], writes=[gT])
                for n in range(4):
                    pq = G.ps[n]
                    P.mm([lambda e: e.matmul(pq[:, :], lhsT=gT[:, :], rhs=b2s[:, n * 512:(n + 1) * 512], start=True, stop=True)],
                         reads=[gT, b2s], writes=[pq])
                    P.op(P.dve if n % 2 else P.act,
                         (lambda e: e.tensor_copy(acc[:, j, n * 512:(n + 1) * 512], pq[:, :])) if n % 2 else
                         (lambda e: e.activation(out=acc[:, j, n * 512:(n + 1) * 512], in_=pq[:, :], func=AF.Copy)),
                         reads=[pq], writes=[accR])
            it = 0
            for b in range(NB):
                o = oE[b % 2]
                P.dma(o[:, :, :], S["outE"][b, :, :].rearrange("(s p) n -> p s n", p=128), reads=[R["outE"]], writes=[o])
                for j, t in enumerate(grp):
                    sg_, sgT = Sg[it % 2], SgT[it % 2]
                    it += 1
                    P.op(P.dve, lambda e: e.tensor_scalar(out=sg_[:, :], in0=cst[:, C_IOTA:C_IOTA + 256], scalar1=dselA[:, b, t:t + 1],
                                                           scalar2=gselA[:, b, t:t + 1], op0=ALU.is_equal, op1=ALU.mult),
                         reads=[cst, dselA, gselA], writes=[sg_])
                    pt = G.ps[4 + it % 2]
                    pvb = pt[:, 0:128].bitcast(BF16)
                    P.mm([(lambda e, s_i=s_i: e.transpose(pvb[:, s_i * 128:(s_i + 1) * 128], sg_[:, s_i * 128:(s_i + 1) * 128], G.idb[:, :]))
                          for s_i in range(2)], reads=[sg_, G.idb], writes=[pt])
                    P.op(P.act, lambda e: e.activation(out=sgT[:, :, :], in_=pvb[:, 0:256].rearrange("p (s n) -> p s n", s=2), func=AF.Copy),
                         reads=[pt], writes=[sgT])
                    for n in range(4):
                        pq = G.ps[n]
                        P.mm([(lambda e, s_i=s_i: e.matmul(pq[:, :], lhsT=sgT[:, s_i, :], rhs=o[:, s_i, n * 512:(n + 1) * 512],
                                                          start=(s_i == 0), stop=(s_i == 1))) for s_i in range(2)], reads=[sgT, o], writes=[pq])
                        P.op(P.dve, lambda e: e.tensor_tensor(out=acc[:, j, n * 512:(n + 1) * 512], in0=acc[:, j, n * 512:(n + 1) * 512],
                                                              in1=pq[:, :], op=ALU.add), reads=[pq, accR], writes=[accR])
            for j, t in enumerate(grp):
                x = xt[j % 2]
                r = 1 if t < 2 else 0
                P.dma(x[:, :], S["xres"][t * 128:(t + 1) * 128, :], reads=[R["xres"]], writes=[x])
                P.op(P.pool, lambda e: e.tensor_tensor(out=acc[:, j, :], in0=acc[:, j, :], in1=gate[r][:, :], op=ALU.mult),
                     reads=[accR, gate[r]], writes=[accR])
                P.op(P.dve, lambda e: e.tensor_tensor(out=x[:, :], in0=x[:, :], in1=acc[:, j, :], op=ALU.add), reads=[x, accR], writes=[x])
                P.dma(S["xres"][t * 128:(t + 1) * 128, :], x[:, :], reads=[x], writes=[R["xres"]], E=P.pool)


def phase_hgrn(G, hT):
    P, I, S, R = G.P, G.I, G.S, G.res
    cst = G.cst
    W = I["c_w_in"]
    fwd = list(range(T))
    bwd = [1, 0] + list(range(T - 1, 1, -1))
    NCH = [(i * 512, min(512, NT - i * 512)) for i in range(5)]
    with ExitStack() as es:
        lbT = sb(G, es, "lbT", [128, 16], F32)
        omlT = sb(G, es, "omlT", [128, 16], F32)
        with ExitStack() as es1:
            c0 = sb(G, es1, "c0", [16, 128], F32)
            c1 = sb(G, es1, "c1", [16, 128], F32)
            P.dma(c0[:, :], I["c_lb"][0, :].rearrange("(h p) -> h p", p=128), writes=[c0])
            P.dma(c1[:, :], I["c_lb"][1, :].rearrange("(h p) -> h p", p=128), writes=[c1])
            P.op(P.dve, lambda e: e.tensor_tensor(out=c1[:, :], in0=c1[:, :], in1=c0[:, :], op=ALU.subtract), reads=[c0, c1], writes=[c1])
            P.op(P.act, lambda e: e.activation(out=c1[:, :], in_=c1[:, :], func=AF.Sigmoid), reads=[c1], writes=[c1])
            pq = G.ps[0]
            P.mm([lambda e: e.transpose(pq[:, 0:16], c1[:, :], cst[0:16, C_ID:C_ID + 16])], reads=[c1, cst], writes=[pq])
            P.op(P.dve, lambda e: e.tensor_copy(lbT[:, :], pq[:, 0:16]), reads=[pq], writes=[lbT])
            P.op(P.dve, lambda e: e.tensor_scalar(out=omlT[:, :], in0=lbT[:, :], scalar1=-1.0, scalar2=1.0, op0=ALU.mult, op1=ALU.add),
                 reads=[lbT], writes=[omlT])
            P.barrier()
        gnb = bc_row(G, es, "cgn", I["c_gn"][0, :], D)
        wst = sb(G, es, "hwst", [128, KC, 128], F32)
        W5 = sb(G, es, "W5", [128, KC, 5, 128], BF16)
        lf = sb(G, es, "lf", [128, NT], F32)
        kk = sb(G, es, "kk", [128, NT], F32)
        BX = sb(G, es, "BX", [128, NT], F32)
        qs = sb(G, es, "qs", [128, NT], F32)
        qe = sb(G, es, "qe", [128, NT], BF16)
        ke = sb(G, es, "ke", [128, NT], BF16)
        vt = sb(G, es, "vt", [128, T, 128], BF16)
        gtm = sb(G, es, "gtm", [128, T, 128], BF16)
        O = sb(G, es, "hO", [128, T, 128], F32)
        ones = sb(G, es, "hones", [128, NT], BF16)
        ob = sb(G, es, "hob", [128, T, 128], BF16)
        oT = [sb(G, es, "hoT", [128, 4, 128], BF16) for _ in range(2)]
        sc = sb(G, es, "hsc", [128, 4, T], F32)
        Sst = sb(G, es, "hS", [128, 128], F32)
        Ssc = [sb(G, es, "hSsc", [128, 128], BF16) for _ in range(2)]
        keT = [sb(G, es, "hkeT", [128, 128], BF16) for _ in range(2)]
        AT = [sb(G, es, "hAT", [128, 128], BF16) for _ in range(2)]
        st2 = sb(G, es, "hst2", [128, T], F32)
        P.op(P.pool, lambda e: e.memset(ones[:, :], 1.0), writes=[ones])
        BX3 = BX.t[:, :].rearrange("p (c n) -> p c n", n=128)
        lf3 = lf.t[:, :].rearrange("p (c n) -> p c n", n=128)
        for h in range(16):
            for j in range(5):
                P.dma(wst[:, :, :], W[:, j * D + h * 128:j * D + (h + 1) * 128].rearrange("(k p) n -> p k n", p=128), writes=[wst],
                      E=(P.sp if j % 2 == 0 else P.act))
                if j % 2 == 0:
                    P.op(P.pool, lambda e: e.tensor_copy(W5[:, :, j, :], wst[:, :, :]), reads=[wst], writes=[W5])
                else:
                    P.op(P.act, lambda e: e.activation(out=W5[:, :, j, :], in_=wst[:, :, :], func=AF.Copy), reads=[wst], writes=[W5])

            def proj_fm(j, post):
                for ci, (n0, nn) in enumerate(NCH):
                    pq = G.ps[ci % 2]
                    P.mm([(lambda e, k=k: e.matmul(pq[:, 0:nn], lhsT=W5[:, k, j, :], rhs=hT[:, k, n0:n0 + nn], start=(k == 0), stop=(k == KC - 1)))
                          for k in range(KC)], reads=[W5, hT], writes=[pq])
                    post(pq, n0, nn)

            def proj_tm(j, dst, tiles, func):
                for g0 in range(0, len(tiles), 4):
                    ts = tiles[g0:g0 + 4]
                    pq = G.ps[2 + (g0 // 4) % 2]
                    for jj, t in enumerate(ts):
                        P.mm([(lambda e, k=k: e.matmul(pq[:, jj * 128:(jj + 1) * 128], lhsT=hT[:, k, t * 128:(t + 1) * 128], rhs=W5[:, k, j, :],
                                                       start=(k == 0), stop=(k == KC - 1))) for k in range(KC)], reads=[W5, hT], writes=[pq])
                    n = len(ts)
                    P.op(P.act, lambda e: e.activation(out=dst[:, ts[0]:ts[0] + n, :], in_=pq[:, 0:n * 128].rearrange("p (t n) -> p t n", t=n), func=func),
                         reads=[pq], writes=[dst])

            proj_tm(2, vt, list(range(T)), AF.Copy)
            proj_tm(4, gtm, list(range(2, T)), AF.Silu)
            proj_fm(3, lambda pq, n0, nn: P.op(P.act, lambda e: e.activation(out=qs[:, n0:n0 + nn], in_=pq[:, 0:nn], func=AF.Silu), reads=[pq], writes=[qs]))
            for d in range(2):
                order = fwd if d == 0 else bwd
                proj_fm(d, lambda pq, n0, nn: P.op(P.act, lambda e: e.activation(out=lf[:, n0:n0 + nn], in_=pq[:, 0:nn], func=AF.Sigmoid), reads=[pq], writes=[lf]))
                P.op(P.dve, lambda e: e.tensor_scalar(out=lf[:, :], in0=lf[:, :], scalar1=omlT[:, h:h + 1], scalar2=lbT[:, h:h + 1], op0=ALU.mult, op1=ALU.add),
                     reads=[lf, omlT, lbT], writes=[lf])
                P.op(P.pool, lambda e: e.tensor_scalar(out=kk[:, :], in0=lf[:, :], scalar1=-1.0, scalar2=1.0, op0=ALU.mult, op1=ALU.add), reads=[lf], writes=[kk])
                P.op(P.act, lambda e: e.activation(out=lf[:, :], in_=lf[:, :], func=AF.Ln), reads=[lf], writes=[lf])
                P.op(P.dve, lambda e: e.tensor_tensor_scan(out=BX[:, :], data0=ones[:, :], data1=lf[:, :], initial=0.0, op0=ALU.mult, op1=ALU.add),
                     reads=[ones, lf], writes=[BX])
                if d == 0:
                    P.op(P.dve, lambda e: e.memset(sc[:, 0, 0:1], 0.0), writes=[sc])
                    P.op(P.dve, lambda e: e.tensor_scalar(out=sc[:, 0, 1:T], in0=BX3[:, 0:T - 1, 127], scalar1=-1.0, scalar2=None, op0=ALU.mult), reads=[BX], writes=[sc])
                    edge = 127
                else:
                    P.op(P.dve, lambda e: e.tensor_copy(sc[:, 0, :], BX3[:, :, 127]), reads=[BX], writes=[sc])
                    P.op(P.dve, lambda e: e.tensor_tensor(out=BX[:, :], in0=lf[:, :], in1=BX[:, :], op=ALU.subtract), reads=[lf, BX], writes=[BX])
                    edge = 0
                P.op(P.dve, lambda e: e.tensor_tensor(out=sc[:, 1, :], in0=sc[:, 0, :], in1=BX3[:, :, 64], op=ALU.add), reads=[sc, BX], writes=[sc])
                P.op(P.dve, lambda e: e.tensor_tensor(out=sc[:, 2, :], in0=sc[:, 0, :], in1=BX3[:, :, edge], op=ALU.add), reads=[sc, BX], writes=[sc])
                P.op(P.dve, lambda e: e.tensor_tensor(out=sc[:, 3, :], in0=BX3[:, :, edge], in1=BX3[:, :, 64], op=ALU.subtract), reads=[sc, BX], writes=[sc])
                P.op(P.act, lambda e: e.activation(out=sc[:, 1:4, :], in_=sc[:, 1:4, :], func=AF.Exp), reads=[sc], writes=[sc])
                P.op(P.dve, lambda e: e.tensor_tensor(out=lf3, in0=BX3, in1=BX3[:, :, 64:65].to_broadcast([128, T, 128]), op=ALU.subtract), reads=[BX], writes=[lf])
                P.op(P.act, lambda e: e.activation(out=BX[:, :], in_=lf[:, :], func=AF.Exp), reads=[lf], writes=[BX])
                P.op(P.dve, lambda e: e.tensor_tensor(out=qe[:, :], in0=qs[:, :], in1=BX[:, :], op=ALU.mult), reads=[qs, BX], writes=[qe])
                P.op(P.act, lambda e: e.activation(out=BX[:, :], in_=lf[:, :], func=AF.Exp, scale=-1.0), reads=[lf, qe], writes=[BX])
                P.op(P.dve, lambda e: e.tensor_tensor(out=ke[:, :], in0=kk[:, :], in1=BX[:, :], op=ALU.mult), reads=[kk, BX], writes=[ke])
                mcol = C_MGE if d == 0 else C_MLE
                P.op(P.dve, lambda e: e.memset(Sst[:, :], 0.0), writes=[Sst])
                for i, c in enumerate(order):
                    kt, at, ssc = keT[i % 2], AT[i % 2], Ssc[i % 2]
                    cs_ = slice(c * 128, (c + 1) * 128)
                    pt = G.ps[4 + i % 2]
                    ptb = pt[:, 0:64].bitcast(BF16)
                    P.mm([lambda e: e.transpose(ptb[:, 0:128], ke[:, cs_], G.idb[:, :])], reads=[ke, G.idb], writes=[pt])
                    P.op(P.act, lambda e: e.activation(out=kt[:, :], in_=ptb[:, 0:128], func=AF.Copy), reads=[pt], writes=[kt])
                    if c >= 2:
                        pS, pO = G.ps[6], G.ps[7]
                        P.mm([lambda e: e.matmul(pS[:, 0:128], lhsT=ke[:, cs_], rhs=qe[:, cs_], start=True, stop=True)], reads=[ke, qe], writes=[pS])
                        P.op(P.dve, lambda e: e.tensor_tensor(out=at[:, :], in0=pS[:, 0:128], in1=cst[:, mcol:mcol + 128], op=ALU.mult), reads=[pS, cst], writes=[at])
                        P.op(P.act, lambda e: e.activation(out=ssc[:, :], in_=Sst[:, :], func=AF.Copy, scale=sc[:, 1, c:c + 1]), reads=[Sst, sc], writes=[ssc])
                        P.mm([lambda e: e.matmul(pO[:, 0:128], lhsT=at[:, :], rhs=vt[:, c, :], start=True, stop=False),
                              lambda e: e.matmul(pO[:, 0:128], lhsT=qe[:, cs_], rhs=ssc[:, :], start=False, stop=True)],
                             reads=[at, vt, qe, ssc], writes=[pO])
                        if d == 0:
                            P.op(P.act, lambda e: e.activation(out=O[:, c, :], in_=pO[:, 0:128], func=AF.Copy), reads=[pO], writes=[O])
                        else:
                            P.op(P.dve, lambda e: e.tensor_tensor(out=O[:, c, :], in0=O[:, c, :], in1=pO[:, 0:128], op=ALU.add), reads=[pO, O], writes=[O])
                    if i < T - 1:
                        pU = G.ps[2 + i % 2]
                        P.mm([lambda e: e.matmul(pU[:, 0:128], lhsT=kt[:, :], rhs=vt[:, c, :], start=True, stop=True)], reads=[kt, vt], writes=[pU])
                        P.op(P.dve, lambda e: e.tensor_scalar(out=Sst[:, :], in0=Sst[:, :], scalar1=sc[:, 2, c:c + 1], scalar2=None, op0=ALU.mult),
                             reads=[Sst, sc], writes=[Sst])
                        P.op(P.dve, lambda e: e.scalar_tensor_tensor(out=Sst[:, :], in0=pU[:, 0:128], scalar=sc[:, 3, c:c + 1], in1=Sst[:, :],
                                                                     op0=ALU.mult, op1=ALU.add), reads=[pU, sc, Sst], writes=[Sst])
            Ox = O.t[:, 2:T, :]
            lfx = lf.t[:, :].rearrange("p (c n) -> p c n", n=128)[:, 2:T, :]
            P.op(P.act, lambda e: e.activation(out=lfx, in_=Ox, func=AF.Square), reads=[O], writes=[lf])
            P.op(P.dve, lambda e: e.tensor_reduce(out=st2[:, 2:T], in_=lfx, axis=AX.X, op=ALU.add), reads=[lf], writes=[st2])
            P.op(P.act, lambda e: e.activation(out=st2[:, 2:T], in_=st2[:, 2:T], func=AF.Sqrt, scale=1.0 / 128, bias=cst[:, C_COL + 2:C_COL + 3]),
                 reads=[st2, cst], writes=[st2])
            P.op(P.dve, lambda e: e.reciprocal(st2[:, 2:T], st2[:, 2:T]), reads=[st2], writes=[st2])
            P.op(P.dve, lambda e: e.tensor_tensor(out=Ox, in0=Ox, in1=st2[:, 2:T].unsqueeze(2).to_broadcast([128, T - 2, 128]), op=ALU.mult), reads=[O, st2], writes=[O])
            P.op(P.dve, lambda e: e.tensor_tensor(out=Ox, in0=Ox, in1=gnb[:, h * 128:(h + 1) * 128].unsqueeze(1).to_broadcast([128, T - 2, 128]), op=ALU.mult),
                 reads=[O, gnb], writes=[O])
            P.op(P.dve, lambda e: e.tensor_tensor(out=ob[:, 2:T, :], in0=Ox, in1=gtm[:, 2:T, :], op=ALU.mult), reads=[O, gtm], writes=[ob])
            transpose_out(G, ob, oT, S["mixT"], R["mixT"], h, list(range(2, T)))


def phase_final(G):
    P, I, S, R = G.P, G.I, G.S, G.res
    with ExitStack() as es:
        gb = bc_row(G, es, "fgb", I["norm_final"][0, :], D)
        xt = [sb(G, es, "fxt", [128, D], F32) for _ in range(2)]
        yo = [sb(G, es, "fyo", [128, D], F32) for _ in range(2)]
        junk = sb(G, es, "fjunk", [128, D], BF16)
        ss = sb(G, es, "fss", [128, 4], F32)
        rs = sb(G, es, "frs", [128, 4], F32)
        for i in range(16):
            x, y = xt[i % 2], yo[i % 2]
            P.dma(x[:, :], S["xres"][(i + 2) * 128:(i + 3) * 128, :], reads=[R["xres"]], writes=[x])
            P.op(P.act, lambda e: e.activation(out=junk[:, :], in_=x[:, :], func=AF.Square, accum_out=ss[:, 0:1]), reads=[x], writes=[junk, ss])
            rstd_from_ss(G, ss, rs, 1, 1.0 / D)
            P.op(P.dve, lambda e: e.scalar_tensor_tensor(out=y[:, :], in0=x[:, :], scalar=rs[:, 0:1], in1=gb[:, :], op0=ALU.mult, op1=ALU.mult),
                 reads=[x, rs, gb], writes=[y])
            P.dma(G.out[i * 128:(i + 1) * 128, :], y[:, :], reads=[y], writes=[G.out_res], E=P.act)


_PROG = {}


def _inputs_for_core(inp, b, consts, cossin):
    f = lambda a: np.ascontiguousarray(a, dtype=np.float32)
    return {
        "x": f(inp["x"][b]), "ctx": f(inp["ctx"][b]), "c": f(inp["c"][b:b + 1]), "c_ctx": f(inp["c_ctx"][None, :]),
        "ada_w": f(inp["ada_w"]), "ada_b": f(inp["ada_b"]), "norm_mix": f(inp["norm_mix"]), "norm_ffn": f(inp["norm_ffn"]),
        "ab_w_in": f(inp["ab_w_in"][0]), "ab_w_out": f(inp["ab_w_out"][0]), "a_q_norm": f(inp["a_q_norm"]),
        "a_k_norm": f(inp["a_k_norm"]), "b_decay_exp": f(inp["b_decay_exp"].reshape(1, 16)), "b_gn": f(inp["b_gn"]),
        "c_w_in": f(inp["c_w_in"][0]), "c_w_out": f(inp["c_w_out"][0]), "c_lb": f(inp["c_lb"]), "c_gn": f(inp["c_gn"]),
        "router_w": f(inp["router_w"]), "router_b": f(inp["router_b"]),
        "w1t0": inp["_w1"][0], "w1t1": inp["_w1"][1], "w2t0": inp["_w2"][0], "w2t1": inp["_w2"][1],
        "exp_b1": f(inp["exp_b1"]), "exp_b2": f(inp["exp_b2"]),
        "norm_final": f(inp["norm_final"][None, :]), "consts": consts, "cossin": cossin,
    }


def prep_weights(inp):
    w1, w2 = inp["exp_w1"], inp["exp_w2"]
    if w1 is None:
        z = np.zeros((1, 1), np.float32)
        inp["_w1"] = [z, z]
        inp["_w2"] = [z, z]
        return
    inp["_w1"], inp["_w2"] = [], []
    for l in range(2):
        a = np.asarray(w1[l], dtype=np.float32).reshape(NE, 2, 8, 128, 16, 128, 2)
        inp["_w1"].append(np.ascontiguousarray(a.transpose(0, 4, 3, 1, 2, 6, 5)).reshape(NE * 16 * 128, 4096))
        b_ = np.asarray(w2[l], dtype=np.float32).reshape(NE, 2, 8, 128, 8, 256)
        inp["_w2"].append(np.ascontiguousarray(b_.transpose(0, 4, 3, 1, 2, 5)).reshape(NE * 8 * 128, 4096))


def kernel(**inputs):
    inp = {k: np.asarray(v) for k, v in inputs.items()}
    prep_weights(inp)
    consts, cossin = make_consts(), make_cossin()
    if "nc" not in _PROG:
        _PROG["nc"] = build_program()
    nc = _PROG["nc"]
    in_maps = [_inputs_for_core(inp, b, consts, cossin) for b in range(N_CORES)]
    res = run_bass_kernel_spmd(nc, in_maps, core_ids=list(range(N_CORES)))
    return np.stack([np.asarray(res.results[b]["out"], dtype=np.float32) for b in range(N_CORES)], axis=0)
```
